# Optimizing a Trainium2 kernel written in Bass

```python
import math
import jax
import jax.numpy as jnp
from jax import lax
import numpy as np

D_MODEL = 1024
BATCH = 8
SEQ = 4096
DEPTH = 1

EPS = 1e-6
MLA_HEADS = 8
MLA_NOPE = 64
MLA_ROPE = 32
MLA_V = 64
MLA_QK = MLA_NOPE + MLA_ROPE
MLA_Q_LORA = 384
MLA_KV_LORA = 256
ROPE_THETA = 10000.0
Q_BLOCK = 128
GDN_HEADS = 8
GDN_DK = 64
GDN_DV = 64
CONV_WIDTH = 4
CHUNK = 64
MLA_OUT = MLA_HEADS * MLA_V
GDN_OUT = GDN_HEADS * GDN_DV
D_MIX = MLA_OUT + GDN_OUT
IN_SIZES = (MLA_Q_LORA, MLA_KV_LORA, MLA_ROPE,
            GDN_HEADS * GDN_DK, GDN_HEADS * GDN_DK, GDN_HEADS * GDN_DV,
            GDN_HEADS * GDN_DV, GDN_HEADS, GDN_HEADS)
IN_DIM = sum(IN_SIZES)
CONV_CH = 2 * GDN_HEADS * GDN_DK + GDN_HEADS * GDN_DV
N_EXPERTS = 32
TOP_K = 4
D_EXPERT = D_MODEL
SWIGLU_ALPHA = 1.702
SWIGLU_LIMIT = 7.0
EXPERT_BLOCK = 256

kernel_name = 'hybrid_mla_gdn_moe_adaln_block'


def rms_norm(x, gain):
    xf = x.astype(jnp.float32)
    y = xf * lax.rsqrt(jnp.mean(xf * xf, axis=-1, keepdims=True) + EPS)
    return (y * gain.astype(jnp.float32)).astype(x.dtype)


def l2_normalize(x):
    xf = x.astype(jnp.float32)
    return (xf * lax.rsqrt(jnp.sum(xf * xf, axis=-1, keepdims=True) + EPS)).astype(x.dtype)


def rope_tables(positions):
    half = MLA_ROPE // 2
    inv_freq = ROPE_THETA ** (-jnp.arange(half, dtype=jnp.float32) / half)
    ang = positions.astype(jnp.float32)[..., None] * inv_freq
    return jnp.cos(ang), jnp.sin(ang)


def apply_rope(x, cos, sin):
    half = MLA_ROPE // 2
    xf = x.astype(jnp.float32)
    x1, x2 = xf[..., :half], xf[..., half:]
    return jnp.concatenate([x1 * cos - x2 * sin, x2 * cos + x1 * sin], axis=-1).astype(x.dtype)


def mla_group(q_lat, kv_lat, k_pe, positions, q_norm_g, w_q_b, kv_norm_g, w_kv_b, out_g):
    b, s, _ = q_lat.shape
    n_blocks = s // Q_BLOCK
    q = (rms_norm(q_lat, q_norm_g) @ w_q_b).reshape(b, s, MLA_HEADS, MLA_QK)
    kv = (rms_norm(kv_lat, kv_norm_g) @ w_kv_b).reshape(b, s, MLA_HEADS, MLA_NOPE + MLA_V)
    k_nope, v = kv[..., :MLA_NOPE], kv[..., MLA_NOPE:]
    cos, sin = rope_tables(positions)
    q_pe = apply_rope(q[..., MLA_NOPE:], cos[:, :, None, :], sin[:, :, None, :])
    k_pe = apply_rope(k_pe, cos, sin)
    q = jnp.concatenate([q[..., :MLA_NOPE], q_pe], axis=-1) * (MLA_QK ** -0.5)
    k = jnp.concatenate(
        [k_nope, jnp.broadcast_to(k_pe[:, :, None, :], (b, s, MLA_HEADS, MLA_ROPE))], axis=-1)
    q_blocks = jnp.moveaxis(q.reshape(b, n_blocks, Q_BLOCK, MLA_HEADS, MLA_QK), 1, 0)
    key_pos = jnp.arange(s, dtype=jnp.int32)

    def attend(args):
        q_blk, blk = args
        scores = jnp.einsum('bqhd,bkhd->bhqk', q_blk, k, preferred_element_type=jnp.float32)
        q_pos = blk * Q_BLOCK + jnp.arange(Q_BLOCK, dtype=jnp.int32)
        causal = key_pos[None, :] <= q_pos[:, None]
        probs = jax.nn.softmax(jnp.where(causal, scores, -jnp.inf), axis=-1).astype(v.dtype)
        return jnp.einsum('bhqk,bkhd->bqhd', probs, v)

    o = lax.map(attend, (q_blocks, jnp.arange(n_blocks, dtype=jnp.int32)))
    o = jnp.moveaxis(o, 0, 1).reshape(b, s, MLA_OUT)
    return rms_norm(o, out_g)


def causal_conv(x, w):
    ch = x.shape[-1]
    y = lax.conv_general_dilated(
        x, w[:, None, :].astype(x.dtype), window_strides=(1,),
        padding=((CONV_WIDTH - 1, 0),), dimension_numbers=('NWC', 'WIO', 'NWC'),
        feature_group_count=ch)
    return jax.nn.silu(y)


def gated_delta_rule(q, k, v, g, beta):
    b, s, h, dk = q.shape
    dv = v.shape[-1]
    n = s // CHUNK
    f32 = jnp.float32

    def to_chunks(t):
        return t.astype(f32).reshape(b, n, CHUNK, h, -1).transpose(0, 3, 1, 2, 4)

    qc = to_chunks(q) * (dk ** -0.5)
    kc = to_chunks(k)
    vc = to_chunks(v)
    gc = g.astype(f32).reshape(b, n, CHUNK, h).transpose(0, 3, 1, 2)
    bc = beta.astype(f32).reshape(b, n, CHUNK, h).transpose(0, 3, 1, 2)
    g_cum = jnp.cumsum(gc, axis=-1)
    tri_incl = jnp.tril(jnp.ones((CHUNK, CHUNK), dtype=bool))
    tri_strict = jnp.tril(jnp.ones((CHUNK, CHUNK), dtype=bool), -1)
    diff = g_cum[..., :, None] - g_cum[..., None, :]
    decay = jnp.where(tri_incl, jnp.exp(jnp.where(tri_incl, diff, 0.0)), 0.0)
    k_beta = kc * bc[..., None]
    a_low = jnp.where(tri_strict, jnp.einsum('bhnid,bhnjd->bhnij', k_beta, kc) * decay, 0.0)
    eye = jnp.eye(CHUNK, dtype=f32)
    rhs = jnp.concatenate([vc * bc[..., None], k_beta * jnp.exp(g_cum)[..., None]], axis=-1)
    sol = lax.linalg.triangular_solve(eye + a_low, rhs, left_side=True, lower=True,
                                      unit_diagonal=True)
    u, w = sol[..., :dv], sol[..., dv:]
    qk = jnp.where(tri_incl, jnp.einsum('bhnid,bhnjd->bhnij', qc, kc) * decay, 0.0)
    q_decay = qc * jnp.exp(g_cum)[..., None]
    k_tail = kc * jnp.exp(g_cum[..., -1:] - g_cum)[..., None]
    chunk_decay = jnp.exp(g_cum[..., -1])

    def step(state, xs):
        u_c, w_c, qk_c, qd_c, kt_c, cd_c = xs
        v_new = u_c - jnp.einsum('bhcd,bhde->bhce', w_c, state)
        o_c = (jnp.einsum('bhcd,bhde->bhce', qd_c, state)
               + jnp.einsum('bhij,bhje->bhie', qk_c, v_new))
        state = state * cd_c[..., None, None] + jnp.einsum('bhcd,bhce->bhde', kt_c, v_new)
        return state, o_c

    xs = tuple(jnp.moveaxis(t, 2, 0) for t in (u, w, qk, q_decay, k_tail, chunk_decay))
    _, o = lax.scan(step, jnp.zeros((b, h, dk, dv), f32), xs)
    return o.transpose(1, 0, 3, 2, 4).reshape(b, s, h, dv)


def gdn_group(q_in, k_in, v_in, z, a_in, b_in, conv_w, A_log, dt_bias, norm_g):
    b, s, _ = q_in.shape
    nk = GDN_HEADS * GDN_DK
    qkv = causal_conv(jnp.concatenate([q_in, k_in, v_in], axis=-1), conv_w)
    q = l2_normalize(qkv[..., :nk].reshape(b, s, GDN_HEADS, GDN_DK))
    k = l2_normalize(qkv[..., nk:2 * nk].reshape(b, s, GDN_HEADS, GDN_DK))
    v = qkv[..., 2 * nk:].reshape(b, s, GDN_HEADS, GDN_DV)
    beta = jax.nn.sigmoid(b_in.astype(jnp.float32))
    g = -jnp.exp(A_log.astype(jnp.float32)) * jax.nn.softplus(
        a_in.astype(jnp.float32) + dt_bias.astype(jnp.float32))
    o = gated_delta_rule(q, k, v, g, beta).astype(q_in.dtype)
    o = rms_norm(o, norm_g) * jax.nn.silu(z.reshape(b, s, GDN_HEADS, GDN_DV))
    return o.reshape(b, s, GDN_OUT)


def hybrid_mixer(h, positions, w_in, q_norm_g, w_q_b, kv_norm_g, w_kv_b, mla_out_g,
                 conv_w, A_log, dt_bias, gdn_norm_g, w_out):
    proj = h @ w_in
    cuts = [int(i) for i in np.cumsum(IN_SIZES)[:-1]]
    q_lat, kv_lat, k_pe, g_q, g_k, g_v, g_z, g_a, g_b = jnp.split(proj, cuts, axis=-1)
    mla_o = mla_group(q_lat, kv_lat, k_pe, positions, q_norm_g, w_q_b, kv_norm_g, w_kv_b,
                      mla_out_g)
    gdn_o = gdn_group(g_q, g_k, g_v, g_z, g_a, g_b, conv_w, A_log, dt_bias, gdn_norm_g)
    return jnp.concatenate([mla_o, gdn_o], axis=-1) @ w_out


def moe_ffn(h, router_w, router_b, w_gate_up, b_gate_up, w_down, b_down):
    b, s, d = h.shape
    t = b * s
    m = t * TOP_K
    xf = h.reshape(t, d)
    logits = (xf @ router_w + router_b).astype(jnp.float32)
    top_val, top_idx = lax.top_k(logits, TOP_K)
    weights = jax.nn.softmax(top_val, axis=-1)
    e_flat = top_idx.reshape(m).astype(jnp.int32)
    tok_flat = jnp.arange(m, dtype=jnp.int32) // TOP_K
    w_flat = weights.reshape(m)
    order = jnp.argsort(e_flat)
    e_sorted = e_flat[order]
    counts = jnp.zeros((N_EXPERTS,), jnp.int32).at[e_flat].add(1)
    padded = ((counts + EXPERT_BLOCK - 1) // EXPERT_BLOCK) * EXPERT_BLOCK
    start = jnp.cumsum(counts) - counts
    pstart = jnp.cumsum(padded) - padded
    dest = pstart[e_sorted] + (jnp.arange(m, dtype=jnp.int32) - start[e_sorted])
    n_blocks = (m + N_EXPERTS * (EXPERT_BLOCK - 1) + EXPERT_BLOCK - 1) // EXPERT_BLOCK
    rows = n_blocks * EXPERT_BLOCK
    row_tok = jnp.full((rows,), t, jnp.int32).at[dest].set(tok_flat[order])
    row_w = jnp.zeros((rows,), jnp.float32).at[dest].set(w_flat[order])
    row_e = jnp.full((rows,), N_EXPERTS - 1, jnp.int32).at[dest].set(e_sorted)
    blk_tok = row_tok.reshape(n_blocks, EXPERT_BLOCK)
    blk_e = row_e[::EXPERT_BLOCK]
    x_pad = jnp.concatenate([xf, jnp.zeros((1, d), xf.dtype)], axis=0)

    def expert_block(args):
        tok, e = args
        xb = x_pad[tok]
        gu = xb @ w_gate_up[e] + b_gate_up[e]
        gate = jnp.minimum(gu[:, :D_EXPERT], SWIGLU_LIMIT)
        up = jnp.clip(gu[:, D_EXPERT:], -SWIGLU_LIMIT, SWIGLU_LIMIT)
        act = (up + 1.0) * (gate * jax.nn.sigmoid(SWIGLU_ALPHA * gate))
        return act @ w_down[e] + b_down[e]

    y_rows = lax.map(expert_block, (blk_tok, blk_e)).reshape(rows, d)
    y_rows = y_rows * row_w[:, None].astype(y_rows.dtype)
    y = jnp.zeros((t + 1, d), y_rows.dtype).at[row_tok].add(y_rows)
    return y[:t].reshape(b, s, d)


def setup_inputs(seed: int = 0) -> dict:
    key = jax.random.key(seed)
    ks = jax.random.split(key, 24)

    def nrm(k, shape, scale):
        return jax.random.normal(k, shape, jnp.float32) * scale

    x = nrm(ks[0], (BATCH, SEQ, D_MODEL), 1.0)
    c = nrm(ks[1], (BATCH, D_MODEL), 1.0)
    positions = jnp.broadcast_to(jnp.arange(SEQ, dtype=jnp.int32), (BATCH, SEQ))
    ada_w = nrm(ks[2], (DEPTH, D_MODEL, 6 * D_MODEL), 0.5 * D_MODEL ** -0.5)
    ada_b = nrm(ks[3], (DEPTH, 6 * D_MODEL), 0.02)
    norm1_g = 1.0 + nrm(ks[4], (DEPTH, D_MODEL), 0.1)
    w_in = nrm(ks[5], (DEPTH, D_MODEL, IN_DIM), D_MODEL ** -0.5)
    q_norm_g = 1.0 + nrm(ks[6], (DEPTH, MLA_Q_LORA), 0.1)
    w_q_b = nrm(ks[7], (DEPTH, MLA_Q_LORA, MLA_HEADS * MLA_QK), MLA_Q_LORA ** -0.5)
    kv_norm_g = 1.0 + nrm(ks[8], (DEPTH, MLA_KV_LORA), 0.1)
    w_kv_b = nrm(ks[9], (DEPTH, MLA_KV_LORA, MLA_HEADS * (MLA_NOPE + MLA_V)), MLA_KV_LORA ** -0.5)
    mla_out_g = 1.0 + nrm(ks[10], (DEPTH, MLA_OUT), 0.1)
    conv_w = nrm(ks[11], (DEPTH, CONV_WIDTH, CONV_CH), CONV_WIDTH ** -0.5)
    A_log = jnp.log(jax.random.uniform(ks[12], (DEPTH, GDN_HEADS), jnp.float32, 1.0, 16.0))
    dt = jnp.exp(jax.random.uniform(ks[13], (DEPTH, GDN_HEADS), jnp.float32,
                                    math.log(1e-3), math.log(1e-1)))
    dt_bias = dt + jnp.log(-jnp.expm1(-dt))
    gdn_norm_g = 1.0 + nrm(ks[14], (DEPTH, GDN_DV), 0.1)
    w_out = nrm(ks[15], (DEPTH, D_MIX, D_MODEL), D_MIX ** -0.5)
    norm2_g = 1.0 + nrm(ks[16], (DEPTH, D_MODEL), 0.1)
    router_w = nrm(ks[17], (DEPTH, D_MODEL, N_EXPERTS), D_MODEL ** -0.5)
    router_b = nrm(ks[18], (DEPTH, N_EXPERTS), 0.01)
    w_gate_up = nrm(ks[19], (DEPTH, N_EXPERTS, D_MODEL, 2 * D_EXPERT), D_MODEL ** -0.5)
    b_gate_up = nrm(ks[20], (DEPTH, N_EXPERTS, 2 * D_EXPERT), 0.01)
    w_down = nrm(ks[21], (DEPTH, N_EXPERTS, D_EXPERT, D_MODEL), D_EXPERT ** -0.5)
    b_down = nrm(ks[22], (DEPTH, N_EXPERTS, D_MODEL), 0.01)
    final_g = 1.0 + nrm(ks[23], (D_MODEL,), 0.1)
    return {'x': x, 'c': c, 'positions': positions, 'ada_w': ada_w, 'ada_b': ada_b,
            'norm1_g': norm1_g, 'w_in': w_in, 'q_norm_g': q_norm_g, 'w_q_b': w_q_b,
            'kv_norm_g': kv_norm_g, 'w_kv_b': w_kv_b, 'mla_out_g': mla_out_g,
            'conv_w': conv_w, 'A_log': A_log, 'dt_bias': dt_bias, 'gdn_norm_g': gdn_norm_g,
            'w_out': w_out, 'norm2_g': norm2_g, 'router_w': router_w, 'router_b': router_b,
            'w_gate_up': w_gate_up, 'b_gate_up': b_gate_up, 'w_down': w_down,
            'b_down': b_down, 'final_g': final_g}


def reference(x, c, positions, ada_w, ada_b, norm1_g, w_in, q_norm_g, w_q_b, kv_norm_g,
              w_kv_b, mla_out_g, conv_w, A_log, dt_bias, gdn_norm_g, w_out, norm2_g,
              router_w, router_b, w_gate_up, b_gate_up, w_down, b_down, final_g):
    c_act = jax.nn.silu(c)
    for l in range(DEPTH):
        mod = (c_act @ ada_w[l] + ada_b[l])[:, None, :]
        sh1, sc1, gt1, sh2, sc2, gt2 = jnp.split(mod, 6, axis=-1)
        h = rms_norm(x, norm1_g[l]) * (1.0 + sc1) + sh1
        mix = hybrid_mixer(h, positions, w_in[l], q_norm_g[l], w_q_b[l], kv_norm_g[l],
                           w_kv_b[l], mla_out_g[l], conv_w[l], A_log[l], dt_bias[l],
                           gdn_norm_g[l], w_out[l])
        x = x + gt1 * mix
        h = rms_norm(x, norm2_g[l]) * (1.0 + sc2) + sh2
        ffn = moe_ffn(h, router_w[l], router_b[l], w_gate_up[l], b_gate_up[l], w_down[l],
                      b_down[l])
        x = x + gt2 * ffn
    return rms_norm(x, final_g)
```

```python
import contextlib
import numpy as np
import concourse.bass as bass
import concourse.mybir as mybir
from concourse.bass_utils import run_bass_kernel_spmd

F32 = mybir.dt.float32
BF16 = mybir.dt.bfloat16
AF = mybir.ActivationFunctionType
ALU = mybir.AluOpType
AX = mybir.AxisListType

S = 4096
D = 1024
NT = S // 128
NB = S // 512
IN_DIM = 2736
EPS = 1e-6
NE = 32


class KB:
    def __init__(self, nc):
        self.nc = nc
        self.es = contextlib.ExitStack()
        self.E = {}
        for name, eng in [("pe", nc.tensor), ("act", nc.scalar), ("dve", nc.vector),
                          ("pool", nc.gpsimd), ("sp", nc.sync)]:
            sem = self.es.enter_context(nc.semaphore("sem_" + name))
            self.E[name] = dict(eng=eng, sem=sem, cnt=0, seen={})
        self.sems = {n: e["sem"] for n, e in self.E.items()}
        self.ndma = 24
        self.dval = []
        for i in range(self.ndma):
            self.sems[("d", i)] = self.es.enter_context(nc.semaphore("sem_d%d" % i))
            self.dval.append(0)
        self.dptr = 0
        self.nw = 8
        for i in range(self.nw):
            self.sems[("d", self.ndma + i)] = self.es.enter_context(nc.semaphore("sem_w%d" % i))
            self.dval.append(0)
        self.wptr = 0
        self.nsw = 40
        self.swptr = 0
        for i in range(self.nsw):
            self.sems[("s", i)] = self.es.enter_context(nc.semaphore("sem_s%d" % i))
        self.state = {}
        self.n_ins = 0
        for sk, sem in self.sems.items():
            nc.gpsimd.sem_clear(sem)
        nc.all_engine_barrier()

    def sb(self, name, shape, dt, es=None):
        return (es or self.es).enter_context(self.nc.sbuf_tensor(name, list(shape), dt))

    def ps(self, name, shape, dt, es=None):
        return (es or self.es).enter_context(self.nc.psum_tensor(name, list(shape), dt))

    def _new(self):
        return {"w": None, "r": {}}

    def _sts(self, key):
        if not isinstance(key, tuple):
            key = (key, None)
        t, sub = key
        d = self.state.setdefault(id(t), {})
        if sub is None:
            if None not in d:
                d[None] = self._new()
            return list(d.values())
        if sub not in d:
            d[sub] = self._new()
        res = [d[sub]]
        if None in d:
            res.append(d[None])
        return res

    def _collect(self, ename, reads, writes):
        need = {}

        def add(sk, val):
            if sk == "pe" and ename == "pe":
                return
            if need.get(sk, 0) < val:
                need[sk] = val
        for k in reads:
            for st in self._sts(k):
                if st["w"] is not None:
                    add(*st["w"])
        for k in writes:
            for st in self._sts(k):
                if st["w"] is not None:
                    add(*st["w"])
                for sk, v in st["r"].items():
                    add(sk, v)
        return need

    def _wait(self, ename, need):
        e = self.E[ename]
        for sk, val in need.items():
            if e["seen"].get(sk, 0) < val:
                e["eng"].wait_ge(self.sems[sk], val)
                e["seen"][sk] = val

    def _update(self, reads, writes, sk, val):
        for k in reads:
            if not isinstance(k, tuple):
                k = (k, None)
            sts = self._sts(k)
            if k[1] is None:
                for st in sts:
                    st["r"][sk] = max(st["r"].get(sk, 0), val)
            else:
                sts[0]["r"][sk] = max(sts[0]["r"].get(sk, 0), val)
        for k in writes:
            if not isinstance(k, tuple):
                k = (k, None)
            sts = self._sts(k)
            if k[1] is None:
                for st in sts:
                    st["w"] = (sk, val)
                    st["r"] = {}
            else:
                sts[0]["w"] = (sk, val)
                sts[0]["r"] = {}

    def op(self, ename, fn, reads=(), writes=(), inc=True):
        e = self.E[ename]
        self._wait(ename, self._collect(ename, reads, writes))
        ins = fn(e["eng"])
        val = e["cnt"] + 1
        if inc:
            ins.then_inc(e["sem"], 1)
            e["cnt"] = val
        self._update(reads, writes, ename, val)
        self.n_ins += 1
        return ins

    def dma(self, ename, out, in_, reads=(), writes=(), wpool=False, **kw):
        e = self.E[ename]
        if ename == "pool":
            sk = ("s", self.swptr)
            self.swptr += 1
            assert self.swptr <= self.nsw, "out of single-use SW-DMA semaphores"
            self._wait(ename, self._collect(ename, reads, writes))
            ins = e["eng"].dma_start(out=out, in_=in_, **kw)
            ins.then_inc(self.sems[sk], 16)
            self._update(reads, writes, sk, 16)
            self.n_ins += 1
            return sk, 16
        if wpool:
            i = self.ndma + self.wptr
            self.wptr = (self.wptr + 1) % self.nw
        else:
            i = self.dptr
            self.dptr = (self.dptr + 1) % self.ndma
        sk = ("d", i)
        need = self._collect(ename, reads, writes)
        if self.dval[i] > 0:
            need[sk] = max(need.get(sk, 0), self.dval[i])
        self._wait(ename, need)
        ins = e["eng"].dma_start(out=out, in_=in_, **kw)
        self.dval[i] += 16
        ins.then_inc(self.sems[sk], 16)
        self._update(reads, writes, sk, self.dval[i])
        self.n_ins += 1
        return sk, self.dval[i]

    def barrier(self):
        for ename, e in self.E.items():
            for sk in self.sems:
                if isinstance(sk, tuple) and sk[0] == "s":
                    val = 16 if sk[1] < self.swptr else 0
                else:
                    val = self.E[sk]["cnt"] if sk in self.E else self.dval[sk[1]]
                if val > 0 and e["seen"].get(sk, 0) < val and sk != ename:
                    e["eng"].wait_ge(self.sems[sk], val)
                    e["seen"][sk] = val

    def finish(self, ename, keys):
        need = self._collect(ename, keys, ())
        self._wait(ename, need)


class Ctx:
    pass


class PV(tuple):
    def __new__(cls, tile, q):
        return super().__new__(cls, (tile, ("q", q)))

    def __getitem__(self, idx):
        if isinstance(idx, int):
            return tuple.__getitem__(self, idx)
        tile = tuple.__getitem__(self, 0)
        q = tuple.__getitem__(self, 1)[1]
        p_, c_ = idx
        return tile[p_, q * 128 + c_.start:q * 128 + c_.stop]


def build(cfg):
    nc = bass.Bass("TRN2", target_bir_lowering=False)
    k = KB(nc)
    dbg = cfg.get("dbg", ())
    phases = cfg.get("phases", ("p0", "p1", "p2", "p3", "p4", "p5"))
    g = Ctx()
    g.nc, g.k, g.dbg, g.cfg = nc, k, dbg, cfg

    def din(name, shape, dt=F32):
        return nc.dram_tensor(name, list(shape), dt, kind="ExternalInput").ap()

    def dout(name, shape, dt=F32):
        return nc.dram_tensor(name, list(shape), dt, kind="ExternalOutput").ap()

    def dscr(name, shape, dt=F32):
        if name in dbg:
            return dout(name, shape, dt)
        if name in cfg.get("as_input", ()):
            return din(name, shape, dt)
        return nc.dram_tensor(name, list(shape), dt, kind="Internal").ap()
    g.din, g.dout, g.dscr = din, dout, dscr

    g.x = din("x", [S, D])
    g.consts = din("consts", [4, 128, 128])
    g.gvec = din("gvec", [3, 128, D])
    g.modscr = dscr("modscr", [6, 128, D])
    g.projT = dscr("projT", [IN_DIM, S])
    g.oT = dscr("oT", [D, S])
    g.x1 = dscr("x1", [S, D])
    g.h2T = dscr("h2T", [D, S], BF16)
    g.out = dout("out", [S, D])
    if "p5" in phases:
        g.w_gu = din("w_gate_up", [NE, D, 2 * D])
        g.w_dn = din("w_down", [NE, D, D])
        g.wgu_bf = dscr("wgu_bf", [NE, D, 2 * D], BF16)
        g.wdn_bf = dscr("wdn_bf", [NE, D, D], BF16)
    g.cast_next = 0 if "p5" in phases else 12

    def cast_some(n):
        for _ in range(n):
            i = g.cast_next
            if i >= 12:
                return
            g.cast_next += 1
            order = [("g", 0), ("g", 1), ("d", 0), ("g", 2), ("g", 3), ("d", 1), ("g", 4), ("g", 5), ("d", 2), ("g", 6), ("g", 7), ("d", 3)]
            kind, j = order[i]
            if kind == "g":
                k.dma("pool", g.wgu_bf[4 * j:4 * j + 4], g.w_gu[4 * j:4 * j + 4], writes=[(g.wgu_bf.tensor, e_) for e_ in range(4 * j, 4 * j + 4)])
            else:
                k.dma("pool", g.wdn_bf[8 * j:8 * j + 8], g.w_dn[8 * j:8 * j + 8], writes=[(g.wdn_bf.tensor, e_) for e_ in range(8 * j, 8 * j + 8)])
    g.cast_some = cast_some
    g.outputs = [g.out.tensor]
    for n in dbg:
        pass

    with k.es:
        g.ident = k.sb("ident", [128, 128], F32)
        g.ones = k.sb("ones", [128, 128], F32)
        k.dma("sp", g.ident[:], g.consts[0], writes=[g.ident])
        k.dma("sp", g.ones[:], g.consts[1], writes=[g.ones])
        g.wr = k.sb("wr", [128, NT, NE], F32)
        if "p0" in phases:
            phase0(g)
        if "p1" in phases:
            phase1(g)
        if "p2" in phases:
            phase2(g)
        if "p3" in phases:
            phase3(g)
        if "p4" in phases:
            phase4(g)
        if "p5" in phases:
            phase5(g)
        fin = list(g.outputs)
        for n in dbg:
            fin.append(getattr(g, n).tensor)
        k.finish("sp", fin)
        k.barrier()
        nc.all_engine_barrier()
    return nc, k


def phase0(g):
    nc, k = g.nc, g.k
    ada_w = g.din("ada_w", [D, 6 * D])
    ada_b = g.din("ada_b", [1, 6 * D])
    c_t = g.din("c_t", [128, 8])
    ones = g.ones
    with contextlib.ExitStack() as p0:
        mod_bc = k.sb("mod_bc", [128, 6 * D], F32, p0)
        ct = k.sb("ct", [128, 8], F32, p0)
        cact = k.sb("cact", [128, 8], F32, p0)
        cbc = k.sb("cbc", [128, 8, 128], F32, p0)
        adab = k.sb("adab", [1, 6 * D], F32, p0)
        g1 = k.sb("g1", [128, D], F32, p0)
        g2 = k.sb("g2", [128, D], F32, p0)
        awb = [k.sb("awb%d" % i, [128, 8, 512], F32, p0) for i in range(2)]
        pm = [k.ps("pm%d" % i, [128, 512], F32, p0) for i in range(2)]
        k.dma("sp", ct[:], c_t, writes=[ct])
        k.dma("sp", adab[:], ada_b, writes=[adab])
        k.dma("sp", g1[:], g.gvec[0], writes=[g1])
        k.dma("sp", g2[:], g.gvec[1], writes=[g2])
        k.op("act", lambda e: e.activation(out=cact[:], in_=ct[:], func=AF.Silu), [ct], [cact])
        for kc in range(8):
            k.op("dve", lambda e: e.tensor_scalar(out=cbc[:, kc, :], in0=ones[:], scalar1=cact[:, kc:kc + 1],
                                                  scalar2=None, op0=ALU.mult), [ones, cact], [(cbc, kc)])
        aw_v = ada_w.rearrange("(c p) n -> p c n", p=128)
        for blk in range(12):
            wb = awb[blk % 2]
            k.dma("sp", wb[:], aw_v[:, :, blk * 512:(blk + 1) * 512], writes=[wb])
            pp = pm[blk % 2]
            for kc in range(8):
                k.op("pe", lambda e: e.matmul(pp[:], lhsT=cbc[:, kc, :], rhs=wb[:, kc, :], start=(kc == 0), stop=False),
                     [(cbc, kc), wb], [pp], inc=False)
            k.op("pe", lambda e: e.matmul(pp[:], lhsT=ones[0:1, :], rhs=adab[0:1, blk * 512:(blk + 1) * 512], start=False, stop=True),
                 [ones, adab], [pp])
            k.op("act", lambda e: e.copy(out=mod_bc[:, blk * 512:(blk + 1) * 512], in_=pp[:]), [pp], [(mod_bc, blk)])
        k.op("dve", lambda e: e.scalar_tensor_tensor(out=mod_bc[:, D:2 * D], in0=mod_bc[:, D:2 * D], scalar=1.0, in1=g1[:],
                                                     op0=ALU.add, op1=ALU.mult), [mod_bc, g1], [mod_bc])
        k.op("dve", lambda e: e.scalar_tensor_tensor(out=mod_bc[:, 4 * D:5 * D], in0=mod_bc[:, 4 * D:5 * D], scalar=1.0, in1=g2[:],
                                                     op0=ALU.add, op1=ALU.mult), [mod_bc, g2], [mod_bc])
        for j in range(6):
            k.dma("sp", g.modscr[j], mod_bc[:, j * D:(j + 1) * D], reads=[mod_bc], writes=[(g.modscr.tensor, j)])
        k.barrier()


def rms_tile(g, xt, stat, col, junk, scale):
    k = g.k
    ss = stat[:, 0, col:col + 1]
    sd = stat[:, 1, col:col + 1]
    rs = stat[:, 2, col:col + 1]
    k.op("act", lambda e: e.activation(out=junk[:], in_=xt[:], func=AF.Square, accum_out=ss), [xt], [junk, (stat, (col, 0))])
    k.op("act", lambda e: e.activation(out=sd, in_=ss, func=AF.Sqrt, scale=scale, bias=EPS), [(stat, (col, 0))], [(stat, (col, 1))])
    k.op("dve", lambda e: e.reciprocal(out=rs, in_=sd), [(stat, (col, 1))], [(stat, (col, 2))])
    return rs, (stat, (col, 2))


def phase1(g):
    nc, k = g.nc, g.k
    w_in = g.din("w_in", [D, IN_DIM])
    x, ident, projT = g.x, g.ident, g.projT
    with contextlib.ExitStack() as p1:
        winb = k.sb("winb", [128, 8, IN_DIM], BF16, p1)
        wv = w_in.rearrange("(c p) n -> p c n", p=128)
        for hh in range(2):
            k.dma("pool", winb[:, :, hh * 1368:(hh + 1) * 1368], wv[:, :, hh * 1368:(hh + 1) * 1368], writes=[(winb, hh)])
        s1_bc = k.sb("s1_bc", [128, D], F32, p1)
        sh1_bc = k.sb("sh1_bc", [128, D], F32, p1)
        k.dma("sp", sh1_bc[:], g.modscr[0], reads=[(g.modscr.tensor, 0)], writes=[sh1_bc])
        k.dma("sp", s1_bc[:], g.modscr[1], reads=[(g.modscr.tensor, 1)], writes=[s1_bc])
        xts = [k.sb("xt%d" % i, [128, D], F32, p1) for i in range(3)]
        junk = k.sb("junk", [128, D], BF16, p1)
        hts = [k.sb("ht%d" % i, [128, D], F32, p1) for i in range(2)]
        stat = k.sb("stat", [128, 4, NT], F32, p1)
        h1T = [k.sb("h1T%d" % i, [128, 8, 512], BF16, p1) for i in range(2)]
        stg = [k.sb("stg%d" % i, [128, 512], F32, p1) for i in range(4)]
        ptr = [k.ps("ptr%d" % i, [128, 4, 128], F32, p1) for i in range(4)]
        pmm = [k.ps("pmm%d" % i, [128, 512], F32, p1) for i in range(4)]
        nchunks = (IN_DIM + 127) // 128
        mmi = 0
        for t in range(NT):
            xt = xts[t % 3]
            ht = hts[t % 2]
            k.dma("sp", xt[:], x[t * 128:(t + 1) * 128, :], writes=[xt])
            if t % 2 == 0:
                g.cast_some(1)
            rs, rskey = rms_tile(g, xt, stat, t, junk, 1.0 / D)
            k.op("dve", lambda e: e.scalar_tensor_tensor(out=ht[:], in0=xt[:], scalar=rs, in1=s1_bc[:], op0=ALU.mult, op1=ALU.mult),
                 [xt, rskey, s1_bc], [ht])
            k.op("dve", lambda e: e.tensor_tensor(out=ht[:], in0=ht[:], in1=sh1_bc[:], op=ALU.add), [ht, sh1_bc], [ht])
            hb = h1T[(t // 4) % 2]
            tt = t % 4
            for half in range(2):
                pt = ptr[(2 * t + half) % 4]
                for j in range(4):
                    kc = half * 4 + j
                    k.op("pe", lambda e: e.transpose(out=pt[:, j, :], in_=ht[:, kc * 128:(kc + 1) * 128], identity=ident[:]),
                         [ht, ident], [pt], inc=(j == 3))
                if half == 0:
                    k.op("act", lambda e: e.copy(out=hb[:, half * 4:half * 4 + 4, tt * 128:(tt + 1) * 128], in_=pt[:]), [pt], [(hb, (half, tt))])
                else:
                    k.op("dve", lambda e: e.tensor_copy(out=hb[:, half * 4:half * 4 + 4, tt * 128:(tt + 1) * 128], in_=pt[:]), [pt], [(hb, (half, tt))])
            if tt == 3:
                blk = t // 4
                for ci in range(nchunks):
                    c0 = ci * 128
                    cw = min(128, IN_DIM - c0)
                    pp = pmm[mmi % 4]
                    sg = stg[mmi % 4]
                    for kc in range(8):
                        k.op("pe", lambda e: e.matmul(pp[0:cw, :], lhsT=winb[:, kc, c0:c0 + cw], rhs=hb[:, kc, :],
                                                      start=(kc == 0), stop=(kc == 7)), [winb, hb], [pp], inc=(kc == 7))
                    if mmi % 2 == 0:
                        k.op("act", lambda e: e.copy(out=sg[0:cw, :], in_=pp[0:cw, :]), [pp], [sg])
                    else:
                        k.op("dve", lambda e: e.tensor_copy(out=sg[0:cw, :], in_=pp[0:cw, :]), [pp], [sg])
                    k.dma("sp", projT[c0:c0 + cw, blk * 512:(blk + 1) * 512], sg[0:cw, :], reads=[sg], writes=[(projT.tensor, (ci, blk))])
                    mmi += 1
        k.barrier()


def phase2(g):
    nc, k = g.nc, g.k
    HQ = 96
    wq_d = g.din("w_q_b", [384, 768])
    wkv_d = g.din("w_kv_b", [256, 1024])
    mvec = g.din("mla_vec", [128, 16])
    pos_d = g.din("pos_bc", [128, S], mybir.dt.int32)
    tri_d = g.consts[2]
    ident, ones, projT = g.ident, g.ones, g.projT
    heads = g.cfg.get("mla_heads", 8)
    TWO_PI = 2.0 * np.pi
    with contextlib.ExitStack() as p2:
        mv = k.sb("mv", [128, 16], F32, p2)
        k.dma("sp", mv[:], mvec, writes=[mv])
        tri = k.sb("tri", [128, 128], BF16, p2)
        k.dma("pool", tri[:], tri_d, writes=[tri])
        wqb = k.sb("wqb", [128, 3, 768], BF16, p2)
        wqr = k.sb("wqr", [128, 3, 768], BF16, p2)
        wkvb = k.sb("wkvb", [128, 2, 1024], BF16, p2)
        qlatn = k.sb("qlatn", [128, 3, S], BF16, p2)
        kvlatn = k.sb("kvlatn", [128, 2, S], BF16, p2)
        cosT = k.sb("cosT", [128, S], F32, p2)
        sinT = k.sb("sinT", [128, S], F32, p2)
        kper = k.sb("kper", [128, S], BF16, p2)
        mo = k.sb("mo", [128, NT, 512], BF16, p2)
        psA = [k.ps("psA%d" % i, [128, 512], F32, p2) for i in range(2)]
        psS = [k.ps("psS%d" % i, [128, 512], F32, p2) for i in range(2)]
        psO = [k.ps("psO%d" % i, [128, 512], F32, p2) for i in range(4)]
        with contextlib.ExitStack() as pa:
            wtmp = k.sb("wtmp", [128, 3, 1024], F32, pa)
            k.dma("sp", wtmp[:, :, 0:768], wq_d.rearrange("(c p) n -> p c n", p=128), writes=[wtmp])
            for c in range(3):
                k.op("dve", lambda e: e.tensor_scalar(out=wtmp[:, c, 0:768], in0=wtmp[:, c, 0:768], scalar1=mv[:, c:c + 1], scalar2=float(HQ ** -0.5),
                                                      op0=ALU.mult, op1=ALU.mult), [wtmp, mv], [wtmp])
            k.op("act", lambda e: e.copy(out=wqb[:], in_=wtmp[:, :, 0:768]), [wtmp], [wqb])
            k.op("pool", lambda e: e.memset(wqr[:], 0.0), [], [wqr])
            w4 = wtmp[:, :, 0:768].rearrange("p c (h d) -> p c h d", d=HQ)
            r4 = wqr[:].rearrange("p c (h d) -> p c h d", d=HQ)
            for c in range(3):
                k.op("dve", lambda e: e.tensor_scalar(out=r4[:, c, :, 64:80], in0=w4[:, c, :, 80:96], scalar1=-1.0, scalar2=None, op0=ALU.mult), [wtmp, wqr], [wqr])
                k.op("dve", lambda e: e.tensor_copy(out=r4[:, c, :, 80:96], in_=w4[:, c, :, 64:80]), [wtmp, wqr], [wqr])
            wtmp2 = k.sb("wtmp2", [128, 2, 1024], F32, pa)
            k.dma("sp", wtmp2[:], wkv_d.rearrange("(c p) n -> p c n", p=128), writes=[wtmp2])
            for c in range(2):
                k.op("dve", lambda e: e.tensor_scalar(out=wkvb[:, c, :], in0=wtmp2[:, c, :], scalar1=mv[:, 3 + c:4 + c], scalar2=None, op0=ALU.mult),
                     [wtmp2, mv], [wkvb])
            pi_ = k.sb("pi_", [128, 1024], mybir.dt.int32, pa)
            pf = k.sb("pf", [128, 1024], F32, pa)
            kf = k.sb("kf", [128, 1024], F32, pa)
            ki = k.sb("ki", [128, 1024], mybir.dt.int32, pa)
            m1 = k.sb("m1", [128, 1024], F32, pa)
            rc = k.sb("rc", [128, 1024], F32, pa)

            def wrap(r):
                k.op("dve", lambda e: e.tensor_scalar(out=m1[:], in0=r[:], scalar1=float(np.pi), scalar2=-TWO_PI, op0=ALU.is_gt, op1=ALU.mult), [r], [m1])
                k.op("dve", lambda e: e.tensor_tensor(out=r[:], in0=r[:], in1=m1[:], op=ALU.add), [r, m1], [r])
                k.op("dve", lambda e: e.tensor_scalar(out=m1[:], in0=r[:], scalar1=float(-np.pi), scalar2=TWO_PI, op0=ALU.is_lt, op1=ALU.mult), [r], [m1])
                k.op("dve", lambda e: e.tensor_tensor(out=r[:], in0=r[:], in1=m1[:], op=ALU.add), [r, m1], [r])
            for q4 in range(4):
                cs = slice(q4 * 1024, (q4 + 1) * 1024)
                k.dma("sp", pi_[:], pos_d[:, cs], writes=[pi_])
                k.op("dve", lambda e: e.tensor_copy(out=pf[:], in_=pi_[:]), [pi_], [pf])
                k.op("dve", lambda e: e.tensor_scalar(out=pf[:], in0=pf[:], scalar1=mv[:, 9:10], scalar2=None, op0=ALU.mult), [pf, mv], [pf])
                k.op("dve", lambda e: e.tensor_scalar(out=kf[:], in0=pf[:], scalar1=float(1.0 / TWO_PI), scalar2=None, op0=ALU.mult), [pf], [kf])
                k.op("dve", lambda e: e.tensor_copy(out=ki[:], in_=kf[:]), [kf], [ki])
                k.op("dve", lambda e: e.tensor_copy(out=kf[:], in_=ki[:]), [ki], [kf])
                k.op("dve", lambda e: e.scalar_tensor_tensor(out=pf[:], in0=kf[:], scalar=-TWO_PI, in1=pf[:], op0=ALU.mult, op1=ALU.add), [kf, pf], [pf])
                wrap(pf)
                k.op("act", lambda e: e.activation(out=sinT[:, cs], in_=pf[:], func=AF.Sin), [pf], [(sinT, q4)])
                k.op("dve", lambda e: e.tensor_scalar(out=rc[:], in0=pf[:], scalar1=float(np.pi / 2), scalar2=None, op0=ALU.add), [pf], [rc])
                wrap(rc)
                k.op("act", lambda e: e.activation(out=cosT[:, cs], in_=rc[:], func=AF.Sin), [rc], [(cosT, q4)])
            kp = k.sb("kp", [128, S], F32, pa)
            ksw = k.sb("ksw", [128, S], F32, pa)
            k.dma("sp", kp[64:96, :], projT[640:672, :], reads=[projT.tensor], writes=[kp])
            k.dma("sp", ksw[64:80, :], projT[656:672, :], reads=[projT.tensor], writes=[(ksw, 0)])
            k.dma("sp", ksw[80:96, :], projT[640:656, :], reads=[projT.tensor], writes=[(ksw, 1)])
            k.op("dve", lambda e: e.tensor_tensor(out=kp[64:96, :], in0=kp[64:96, :], in1=cosT[64:96, :], op=ALU.mult), [kp, cosT], [kp])
            k.op("dve", lambda e: e.scalar_tensor_tensor(out=ksw[64:96, :], in0=ksw[64:96, :], scalar=mv[64:96, 10:11], in1=sinT[64:96, :], op0=ALU.mult, op1=ALU.mult),
                 [ksw, mv, sinT], [ksw])
            k.op("pool", lambda e: e.tensor_tensor(out=kper[64:96, :], in0=kp[64:96, :], in1=ksw[64:96, :], op=ALU.add), [kp, ksw], [kper])
            k.barrier()
        with contextlib.ExitStack() as pb:
            lat = [k.sb("lat%d" % i, [128, 5, 512], F32, pb) for i in range(2)]
            sq = k.sb("sq", [128, 5, 512], F32, pb)
            rst = [k.sb("rst%d" % i, [128, 512], F32, pb) for i in range(2)]
            pv = projT[0:640, :].rearrange("(c p) s -> p c s", p=128)
            for b in range(NB):
                cs = slice(b * 512, (b + 1) * 512)
                lt = lat[b % 2]
                k.dma("sp", lt[:], pv[:, 0:5, cs], reads=[projT.tensor], writes=[lt])
                k.op("act", lambda e: e.activation(out=sq[:], in_=lt[:], func=AF.Square), [lt], [sq])
                for (c0, c1, n, dst, pp, rs_) in ((0, 3, 384.0, qlatn, psA[0], rst[0]), (3, 5, 256.0, kvlatn, psA[1], rst[1])):
                    for c in range(c0, c1):
                        k.op("pe", lambda e: e.matmul(pp[:], lhsT=ones[:], rhs=sq[:, c, :], start=(c == c0), stop=(c == c1 - 1)), [ones, sq], [pp], inc=(c == c1 - 1))
                    k.op("act", lambda e: e.activation(out=rs_[:], in_=pp[:], func=AF.Sqrt, scale=1.0 / n, bias=EPS), [pp], [rs_])
                    k.op("dve", lambda e: e.reciprocal(out=rs_[:], in_=rs_[:]), [rs_], [rs_])
                    for c in range(c0, c1):
                        k.op("dve", lambda e: e.tensor_tensor(out=dst[:, c - c0, cs], in0=lt[:, c, :], in1=rs_[:], op=ALU.mult), [lt, rs_], [(dst, (c - c0, b))])
            k.barrier()
        with contextlib.ExitStack() as pc:
            qh = [k.sb("qh%d" % i, [128, S], BF16, pc) for i in range(2)]
            kh = [k.sb("kh%d" % i, [128, S], BF16, pc) for i in range(2)]
            vh = [k.sb("vh%d" % i, [128, NT, 65], BF16, pc) for i in range(2)]
            pT = [k.sb("pT%d" % i, [128, 512], BF16, pc) for i in range(3)]
            t1 = [k.sb("t1_%d" % i, [128, 512], F32, pc) for i in range(2)]
            t2 = [k.sb("t2_%d" % i, [128, 512], F32, pc) for i in range(2)]
            rec = k.sb("rec", [128, 8, NT], F32, pc)
            for i in range(2):
                k.op("pool", lambda e: e.memset(vh[i][:, :, 64:65], 1.0), [], [vh[i]])
            pti = 0
            for h in range(heads):
                q_, k_, v_ = qh[h % 2], kh[h % 2], vh[h % 2]
                for b in range(NB):
                    cs = slice(b * 512, (b + 1) * 512)
                    p1, p2_ = psA[0], psA[1]
                    for c in range(3):
                        k.op("pe", lambda e: e.matmul(p1[0:HQ, :], lhsT=wqb[:, c, h * HQ:(h + 1) * HQ], rhs=qlatn[:, c, cs], start=(c == 0), stop=(c == 2)),
                             [wqb, (qlatn, (c, b))], [p1], inc=(c == 2))
                    k.op("act", lambda e: e.copy(out=q_[0:64, cs], in_=p1[0:64, :]), [p1], [(q_, (0, b))])
                    k.op("dve", lambda e: e.tensor_tensor(out=t1[b % 2][64:96, :], in0=p1[64:96, :], in1=cosT[64:96, cs], op=ALU.mult), [p1, cosT], [t1[b % 2]])
                    for c in range(3):
                        k.op("pe", lambda e: e.matmul(p2_[0:HQ, :], lhsT=wqr[:, c, h * HQ:(h + 1) * HQ], rhs=qlatn[:, c, cs], start=(c == 0), stop=(c == 2)),
                             [wqr, (qlatn, (c, b))], [p2_], inc=(c == 2))
                    k.op("dve", lambda e: e.tensor_tensor(out=t2[b % 2][64:96, :], in0=p2_[64:96, :], in1=sinT[64:96, cs], op=ALU.mult), [p2_, sinT], [t2[b % 2]])
                    k.op("dve", lambda e: e.tensor_tensor(out=q_[64:96, cs], in0=t1[b % 2][64:96, :], in1=t2[b % 2][64:96, :], op=ALU.add),
                         [t1[b % 2], t2[b % 2]], [(q_, (1, b))])
                    for c in range(2):
                        k.op("pe", lambda e: e.matmul(p1[0:64, :], lhsT=wkvb[:, c, h * 128:h * 128 + 64], rhs=kvlatn[:, c, cs], start=(c == 0), stop=(c == 1)),
                             [wkvb, (kvlatn, (c, b))], [p1], inc=(c == 1))
                    k.op("act", lambda e: e.copy(out=k_[0:64, cs], in_=p1[0:64, :]), [p1], [(k_, (0, b))])
                    k.op("act", lambda e: e.copy(out=k_[64:96, cs], in_=kper[64:96, cs]), [kper], [(k_, (1, b))])
                for g8 in range(NT // 8):
                    pp = psA[g8 % 2]
                    for tl in range(8):
                        t = g8 * 8 + tl
                        for c in range(2):
                            k.op("pe", lambda e: e.matmul(pp[:, tl * 64:(tl + 1) * 64], lhsT=kvlatn[:, c, t * 128:(t + 1) * 128], rhs=wkvb[:, c, h * 128 + 64:h * 128 + 128],
                                                          start=(c == 0), stop=(c == 1)), [kvlatn, wkvb], [pp], inc=(c == 1 and tl == 7))
                    k.op("act", lambda e: e.copy(out=v_[:, g8 * 8:(g8 + 1) * 8, 0:64], in_=pp[:].rearrange("p (t d) -> p t d", d=64)), [pp], [(v_, g8)])
                steps = [(qb, j) for qb in range(NB) for j in range(4 * qb + 4)]

                def emit_scores(i):
                    qb, j = steps[i]
                    c0 = max(0, j - 4 * qb) * 128
                    ps_ = psS[i % 2]
                    k.op("pe", lambda e: e.matmul(ps_[:, c0:512], lhsT=k_[0:HQ, j * 128:(j + 1) * 128], rhs=q_[0:HQ, qb * 512 + c0:(qb + 1) * 512], start=True, stop=True),
                         [k_, q_], [ps_])
                emit_scores(0)
                for i, (qb, j) in enumerate(steps):
                    if i + 1 < len(steps):
                        emit_scores(i + 1)
                    r = j - 4 * qb
                    c0 = max(0, r) * 128
                    ps_ = psS[i % 2]
                    pt_ = pT[i % 3]
                    k.op("act", lambda e: e.activation(out=pt_[:, c0:512], in_=ps_[:, c0:512], func=AF.Exp), [ps_], [pt_])
                    if r >= 0:
                        k.op("dve", lambda e: e.tensor_tensor(out=pt_[:, c0:c0 + 128], in0=pt_[:, c0:c0 + 128], in1=tri[:], op=ALU.mult), [pt_, tri], [pt_])
                    for s_ in range(c0 // 128, 4):
                        k.op("pe", lambda e: e.matmul(psO[s_][:, 0:65], lhsT=pt_[:, s_ * 128:(s_ + 1) * 128], rhs=v_[:, j, 0:65], start=(j == 0), stop=(j == 4 * qb + s_)),
                             [pt_, v_], [psO[s_]], inc=(s_ == 3))
                    if j == 4 * qb + 3:
                        for s_ in range(4):
                            t = qb * 4 + s_
                            k.op("dve", lambda e: e.reciprocal(out=rec[:, h, t:t + 1], in_=psO[s_][:, 64:65]), [psO[s_]], [(rec, (h, t))])
                            k.op("dve", lambda e: e.tensor_scalar(out=mo[:, t, h * 64:(h + 1) * 64], in0=psO[s_][:, 0:64], scalar1=rec[:, h, t:t + 1], scalar2=None, op0=ALU.mult),
                                 [psO[s_], (rec, (h, t))], [(mo, (t, h))])
            k.barrier()
        with contextlib.ExitStack() as pd_:
            stat = k.sb("stat2", [128, 4, NT], F32, pd_)
            junk = k.sb("junk2", [128, 512], BF16, pd_)
            mn = [k.sb("mn%d" % i, [128, 512], F32, pd_) for i in range(2)]
            stg = [k.sb("stg2_%d" % i, [128, 4, 512], F32, pd_) for i in range(2)]
            for t in range(NT):
                mt = mo[:, t, :]
                ss, sd, rs = stat[:, 0, t:t + 1], stat[:, 1, t:t + 1], stat[:, 2, t:t + 1]
                k.op("act", lambda e: e.activation(out=junk[:], in_=mt, func=AF.Square, accum_out=ss), [mo], [junk, (stat, (t, 0))])
                k.op("act", lambda e: e.activation(out=sd, in_=ss, func=AF.Sqrt, scale=1.0 / 512, bias=EPS), [(stat, (t, 0))], [(stat, (t, 1))])
                k.op("dve", lambda e: e.reciprocal(out=rs, in_=sd), [(stat, (t, 1))], [(stat, (t, 2))])
                m_ = mn[t % 2]
                k.op("dve", lambda e: e.tensor_scalar(out=m_[:], in0=mt, scalar1=rs, scalar2=None, op0=ALU.mult), [mo, (stat, (t, 2))], [m_])
                pp = psA[t % 2].rearrange("p (c s) -> p c s", s=128)
                for c in range(4):
                    k.op("pe", lambda e: e.transpose(out=pp[:, c, :], in_=m_[:, c * 128:(c + 1) * 128], identity=ident[:]), [m_, ident], [psA[t % 2]], inc=(c == 3))
                sg = stg[(t // 4) % 2]
                for c in range(4):
                    k.op("act", lambda e: e.activation(out=sg[:, c, (t % 4) * 128:(t % 4 + 1) * 128], in_=pp[:, c, :], func=AF.Copy, scale=mv[:, 5 + c:6 + c]),
                         [psA[t % 2], mv], [(sg, (c, t % 4))])
                if t % 4 == 3:
                    b = t // 4
                    for c in range(4):
                        k.dma("sp", g.oT[c * 128:(c + 1) * 128, b * 512:(b + 1) * 512], sg[:, c, :], reads=[sg], writes=[(g.oT.tensor, ("m", c, b))])
            k.barrier()


def phase3(g):
    nc, k = g.nc, g.k
    gvd = g.din("gdn_vec", [128, 32])
    cwd = g.din("conv_wt", [64, 24, 4])
    ident, ones, projT = g.ident, g.ones, g.projT
    heads = g.cfg.get("gdn_heads", 8)
    NCH = S // 64
    BIG = 30000.0
    P = 64
    with contextlib.ExitStack() as p3:
        gv = k.sb("gv", [128, 32], F32, p3)
        k.dma("sp", gv[:], gvd, writes=[gv])
        cw = k.sb("cw", [P, 24, 4], F32, p3)
        k.dma("sp", cw[:], cwd, writes=[cw])
        trif = k.sb("trif", [128, 128], F32, p3)
        k.dma("sp", trif[:], g.consts[2], writes=[trif])
        bigm1 = k.sb("bigm1", [P, P], F32, p3)
        negb2 = k.sb("negb2", [P, P], F32, p3)
        k.op("dve", lambda e: e.tensor_scalar(out=bigm1[:], in0=trif[0:P, 0:P], scalar1=BIG, scalar2=None, op0=ALU.mult), [trif], [bigm1])
        k.op("dve", lambda e: e.tensor_scalar(out=negb2[:], in0=trif[0:P, 0:P], scalar1=-1.0, scalar2=BIG, op0=ALU.add, op1=ALU.mult), [trif], [negb2])
        beta = k.sb("beta", [P, NCH, 8], F32, p3)
        gc = k.sb("gc", [P, NCH, 8], F32, p3)
        ngc = k.sb("ngc", [P, NCH, 8], F32, p3)
        bgam = k.sb("bgam", [P, NCH, 8], F32, p3)
        ktl = k.sb("ktl", [P, NCH, 8], F32, p3)
        cdb = k.sb("cdb", [P, NCH, 8], F32, p3)
        ps = [k.ps("pgd%d" % i, [128, 512], F32, p3) for i in range(8)]
        psq = ps
        with contextlib.ExitStack() as pa:
            ab = k.sb("ab", [16, S], F32, pa)
            k.dma("sp", ab[:], projT[2720:2736, :], reads=[projT.tensor], writes=[ab])
            abt = k.sb("abt", [P, NCH, 16], F32, pa)
            for q4 in range(2):
                pp = ps[q4]
                for j in range(32):
                    n = q4 * 32 + j
                    k.op("pe", lambda e: e.transpose(out=pp[0:P, j * 16:(j + 1) * 16], in_=ab[0:16, n * 64:(n + 1) * 64], identity=ident[0:16, 0:16]),
                         [ab, ident], [pp], inc=(j == 31))
                k.op("act", lambda e: e.copy(out=abt[:, q4 * 32:(q4 + 1) * 32, :], in_=pp[0:P, :].rearrange("p (n f) -> p n f", f=16)), [pp], [(abt, q4)])
            xa = k.sb("xa_", [P, NCH, 8], F32, pa)
            ax = k.sb("ax_", [P, NCH, 8], F32, pa)
            gg = k.sb("gg_", [P, NCH, 8], F32, pa)
            ea = k.sb("ea_", [P, 8], F32, pa)
            gt = k.sb("gt_", [P, NCH, 8], F32, pa)
            k.op("act", lambda e: e.activation(out=beta[:], in_=abt[:, :, 8:16], func=AF.Sigmoid), [abt], [beta])
            for n in range(NCH):
                k.op("pool", lambda e: e.tensor_tensor(out=xa[:, n, :], in0=abt[:, n, 0:8], in1=gv[0:P, 8:16], op=ALU.add), [abt, gv], [(xa, n)])
            k.op("act", lambda e: e.activation(out=ax[:], in_=xa[:], func=AF.Abs), [xa], [ax])
            k.op("act", lambda e: e.activation(out=ax[:], in_=ax[:], func=AF.Exp, scale=-1.0), [ax], [ax])
            k.op("act", lambda e: e.activation(out=ax[:], in_=ax[:], func=AF.Ln, bias=1.0), [ax], [ax])
            k.op("dve", lambda e: e.scalar_tensor_tensor(out=xa[:], in0=xa[:], scalar=0.0, in1=ax[:], op0=ALU.max, op1=ALU.add), [xa, ax], [xa])
            k.op("act", lambda e: e.activation(out=ea[:], in_=gv[0:P, 0:8], func=AF.Exp), [gv], [ea])
            for n in range(NCH):
                k.op("pool", lambda e: e.tensor_tensor(out=gg[:, n, :], in0=xa[:, n, :], in1=ea[:], op=ALU.mult), [xa, ea], [(gg, n)])
            k.op("dve", lambda e: e.tensor_scalar(out=gg[:], in0=gg[:], scalar1=-1.0, scalar2=None, op0=ALU.mult), [gg], [gg])
            ggf = gg[:].rearrange("p n h -> p (n h)")
            pc_, pt_ = ps[2], ps[3]
            k.op("pe", lambda e: e.matmul(pc_[0:P, :], lhsT=trif[0:P, 0:P], rhs=ggf, start=True, stop=True), [trif, gg], [pc_])
            k.op("pe", lambda e: e.matmul(pt_[0:P, :], lhsT=ones[0:P, 0:P], rhs=ggf, start=True, stop=True), [ones, gg], [pt_])
            f2 = lambda t_: t_[:].rearrange("p n h -> p (n h)")
            k.op("act", lambda e: e.copy(out=f2(gc), in_=pc_[0:P, :]), [pc_], [gc])
            k.op("act", lambda e: e.copy(out=f2(ax), in_=pt_[0:P, :]), [pt_], [ax])
            k.op("act", lambda e: e.activation(out=f2(cdb), in_=f2(ax), func=AF.Exp), [ax], [cdb])
            k.op("dve", lambda e: e.tensor_tensor(out=f2(gt), in0=f2(ax), in1=f2(gc), op=ALU.subtract), [ax, gc], [gt])
            k.op("act", lambda e: e.activation(out=f2(ktl), in_=f2(gt), func=AF.Exp), [gt], [ktl])
            k.op("dve", lambda e: e.tensor_scalar(out=f2(ngc), in0=f2(gc), scalar1=-1.0, scalar2=None, op0=ALU.mult), [gc], [ngc])
            k.op("act", lambda e: e.activation(out=f2(gt), in_=f2(gc), func=AF.Exp), [gc], [gt])
            k.op("dve", lambda e: e.tensor_tensor(out=f2(bgam), in0=f2(gt), in1=f2(beta), op=ALU.mult), [gt, beta], [bgam])
            k.barrier()
        with contextlib.ExitStack() as pb:
            xp = k.sb("xp", [P, S + 4], F32, pb)
            cv = k.sb("cv", [P, S], F32, pb)
            cvo = k.sb("cvo", [P, S], F32, pb)
            u_all = k.sb("u_all", [P, NCH, 64], F32, pb)
            wT_all = k.sb("wT_all", [P, S], BF16, pb)
            qg_all = k.sb("qg_all", [P, S], BF16, pb)
            qk_all = k.sb("qk_all", [P, NCH, 64], BF16, pb)
            kt_all = k.sb("kt_all", [P, NCH, 64], BF16, pb)
            kTb2 = [k.sb("kTb%d" % i, [P, S], BF16, pb) for i in range(2)]
            qTb2 = [k.sb("qTb%d" % i, [P, S], BF16, pb) for i in range(2)]
            vTb2 = [k.sb("vTb%d" % i, [P, S], BF16, pb) for i in range(2)]
            identb = k.sb("identb", [P, P], BF16, pb)
            k.op("act", lambda e: e.copy(out=identb[:], in_=ident[0:P, 0:P]), [ident], [identb])
            o_all = u_all
            Xs = [k.sb("Xs%d" % i, [P, P], F32, pb) for i in range(2)]
            Gb = [k.sb("Gb%d" % i, [P, P], F32, pb) for i in range(2)]
            D1 = [k.sb("D1_%d" % i, [P, P], F32, pb) for i in range(2)]
            DT = [k.sb("DT_%d" % i, [P, P], F32, pb) for i in range(2)]
            Pm = [[k.sb("Pm%d_%d" % (i, j), [P, P], BF16, pb) for j in range(2)] for i in range(16)]
            Qm = [[k.sb("Qm%d_%d" % (i, j), [P, P], BF16, pb) for j in range(2)] for i in range(16)]
            Wm = [[k.sb("Wm%d_%d" % (i, j), [P, P], BF16, pb) for j in range(2)] for i in range(16)]
            kbg = [k.sb("kbg%d" % i, [P, P], BF16, pb) for i in range(2)]
            bv = [k.sb("bv%d" % i, [P, P], BF16, pb) for i in range(2)]
            Sst = [k.sb("Sst%d" % i, [P, P], F32, pb) for i in range(2)]
            Sbf = [k.sb("Sbf%d" % i, [P, P], BF16, pb) for i in range(2)]
            vn = [k.sb("vn%d" % i, [P, P], BF16, pb) for i in range(2)]
            rsd = k.sb("rsd", [P, 4, NCH], F32, pb)
            k.op("pool", lambda e: e.memset(xp[:, 0:4], 0.0), [], [(xp, "pad")])
            epsb = k.sb("epsb", [P, 1], F32, pb)
            k.op("pool", lambda e: e.memset(epsb[:], EPS), [], [epsb])
            psi = 0

            psf = 0

            psd = 0

            def nps(kind):
                nonlocal psi, psd
                if kind == "act":
                    psi += 1
                    return psq[psi % 4]
                psd += 1
                return psq[4 + psd % 4]

            def npsf(kind="act"):
                return nps(kind)
            def conv_gen(hh):
                for ti, (dstb, row0) in enumerate(((qTb2[hh % 2], 672), (kTb2[hh % 2], 1184), (vTb2[hh % 2], 1696))):
                    k.dma("sp", xp[:, 4:S + 4], projT[row0 + hh * 64:row0 + (hh + 1) * 64, :], reads=[projT.tensor], writes=[(xp, "x")])
                    ci = ti * 8 + hh
                    k.op("dve", lambda e: e.tensor_scalar(out=cv[:], in0=xp[:, 1:S + 1], scalar1=cw[:, ci, 0:1], scalar2=None, op0=ALU.mult), [xp, cw], [cv])
                    yield
                    for j in range(1, 4):
                        k.op("dve", lambda e: e.scalar_tensor_tensor(out=cv[:], in0=xp[:, 1 + j:S + 1 + j], scalar=cw[:, ci, j:j + 1], in1=cv[:], op0=ALU.mult, op1=ALU.add),
                             [xp, cw, cv], [cv])
                        yield
                    if ti == 2:
                        k.op("act", lambda e: e.activation(out=dstb[:], in_=cv[:], func=AF.Silu), [cv], [dstb])
                        yield
                        continue
                    k.op("act", lambda e: e.activation(out=cvo[:], in_=cv[:], func=AF.Silu), [cv], [cvo])
                    k.op("dve", lambda e: e.tensor_tensor(out=cv[:], in0=cvo[:], in1=cvo[:], op=ALU.mult), [cvo], [cv])
                    yield
                    for b in range(NB):
                        cs = slice(b * 512, (b + 1) * 512)
                        pp = npsf()
                        k.op("pe", lambda e: e.matmul(pp[0:P, :], lhsT=ones[0:P, 0:P], rhs=cv[:, cs], start=True, stop=True), [ones, cv], [pp])
                        k.op("act", lambda e: e.activation(out=cv[:, cs], in_=pp[0:P, :], func=AF.Ln, bias=epsb[0:P, 0:1]), [pp, epsb], [(cv, b)])
                        k.op("act", lambda e: e.activation(out=cv[:, cs], in_=cv[:, cs], func=AF.Exp, scale=-0.5), [(cv, b)], [(cv, b)])
                        sc_ = 0.125 if ti == 0 else 1.0
                        k.op("dve", lambda e: e.scalar_tensor_tensor(out=dstb[:, cs], in0=cvo[:, cs], scalar=sc_, in1=cv[:, cs], op0=ALU.mult, op1=ALU.mult),
                             [cvo, (cv, b)], [(dstb, b)])
                        yield

            ei = 0
            for h in range(heads):
                if h == 0:
                    for _ in conv_gen(0):
                        pass
                kTb, qTb, vTb = kTb2[h % 2], qTb2[h % 2], vTb2[h % 2]
                nxt_conv = conv_gen(h + 1) if h + 1 < heads else iter(())
                G = 8
                St = Sst[0]
                k.op("pool", lambda e: e.memset(St[:], 0.0), [], [St])
                k.op("pool", lambda e: e.memset(Sbf[0][:], 0.0), [], [Sbf[0]])

                def scan_step(n, St):
                    cs = slice(n * 64, (n + 1) * 64)
                    p1_, p2_, p3_ = nps("dve"), nps("act"), nps("dve")
                    v_ = vn[n % 2]
                    Sb = Sbf[n % 2]
                    k.op("pe", lambda e: e.matmul(p1_[0:P, 0:P], lhsT=wT_all[:, cs], rhs=Sb[:], start=True, stop=True), [(wT_all, n), Sb], [p1_])
                    k.op("dve", lambda e: e.tensor_tensor(out=v_[:], in0=u_all[:, n, :], in1=p1_[0:P, 0:P], op=ALU.subtract), [(u_all, n), p1_], [v_])
                    k.op("pe", lambda e: e.matmul(p2_[0:P, 0:P], lhsT=qg_all[:, cs], rhs=Sb[:], start=True, stop=False), [(qg_all, n), Sb], [p2_], inc=False)
                    k.op("pe", lambda e: e.matmul(p2_[0:P, 0:P], lhsT=qk_all[:, n, :], rhs=v_[:], start=False, stop=True), [(qk_all, n), v_], [p2_])
                    k.op("pe", lambda e: e.matmul(p3_[0:P, 0:P], lhsT=kt_all[:, n, :], rhs=v_[:], start=True, stop=True), [(kt_all, n), v_], [p3_])
                    Sn = Sst[(n + 1) % 2]
                    k.op("dve", lambda e: e.scalar_tensor_tensor(out=Sn[:], in0=St[:], scalar=cdb[:, n, h:h + 1], in1=p3_[0:P, 0:P], op0=ALU.mult, op1=ALU.add),
                         [St, cdb, p3_], [Sn])
                    k.op("act", lambda e: e.copy(out=Sbf[(n + 1) % 2][:], in_=Sn[:]), [Sn], [Sbf[(n + 1) % 2]])
                    k.op("act", lambda e: e.copy(out=o_all[:, n, :], in_=p2_[0:P, 0:P]), [p2_], [(o_all, n)])
                    return Sn

                def stage_a(n, sl):
                    cs = slice(n * 64, (n + 1) * 64)
                    kTn, qTn = kTb[:, cs], qTb[:, cs]
                    gcn, ngcn, btn = gc[:, n, h:h + 1], ngc[:, n, h:h + 1], beta[:, n, h:h + 1]
                    X = Xs[n % 2]
                    k.op("dve", lambda e: e.tensor_scalar(out=X[:], in0=ident[0:P, 0:P], scalar1=gcn, scalar2=None, op0=ALU.mult), [ident, gc], [X])
                    pR, pR2, pR3 = nps("act"), nps("act"), nps("act")
                    k.op("pe", lambda e: e.matmul(pR[0:P, 0:P], lhsT=ones[0:P, 0:P], rhs=X[:], start=True, stop=False), [ones, X], [pR], inc=False)
                    k.op("pe", lambda e: e.matmul(pR[0:P, 0:P], lhsT=ident[0:P, 0:P], rhs=bigm1[:], start=False, stop=True), [ident, bigm1], [pR])
                    k.op("pe", lambda e: e.matmul(pR2[0:P, 0:P], lhsT=ones[0:P, 0:P], rhs=X[:], start=True, stop=False), [ones, X], [pR2], inc=False)
                    k.op("pe", lambda e: e.matmul(pR2[0:P, 0:P], lhsT=ident[0:P, 0:P], rhs=negb2[:], start=False, stop=True), [ident, negb2], [pR2])
                    k.op("pe", lambda e: e.matmul(pR3[0:P, 0:P], lhsT=ones[0:P, 0:P], rhs=X[:], start=True, stop=True), [ones, X], [pR3])
                    d1, dt_, gb = D1[n % 2], DT[n % 2], Gb[n % 2]
                    k.op("act", lambda e: e.activation(out=d1[:], in_=pR[0:P, 0:P], func=AF.Exp, scale=-1.0, bias=gcn), [pR, gc], [d1])
                    k.op("act", lambda e: e.activation(out=dt_[:], in_=pR2[0:P, 0:P], func=AF.Exp, scale=1.0, bias=ngcn), [pR2, ngc], [dt_])
                    k.op("act", lambda e: e.activation(out=gb[:], in_=pR3[0:P, 0:P], func=AF.Exp), [pR3], [gb])
                    pK, pQ = nps("dve"), nps("dve")
                    k.op("pe", lambda e: e.matmul(pK[0:P, 0:P], lhsT=kTn, rhs=kTn, start=True, stop=True), [kTb], [pK])
                    k.op("pe", lambda e: e.matmul(pQ[0:P, 0:P], lhsT=kTn, rhs=qTn, start=True, stop=True), [kTb, qTb], [pQ])
                    A, Q0, W = Pm[sl][0], Qm[sl][0], Wm[sl][0]
                    k.op("dve", lambda e: e.scalar_tensor_tensor(out=A[:], in0=pK[0:P, 0:P], scalar=btn, in1=d1[:], op0=ALU.mult, op1=ALU.mult), [pK, beta, d1], [A])
                    k.op("dve", lambda e: e.tensor_tensor(out=qk_all[:, n, :], in0=pQ[0:P, 0:P], in1=dt_[:], op=ALU.mult), [pQ, dt_], [(qk_all, n)])
                    k.op("pool", lambda e: e.tensor_tensor(out=qg_all[:, cs], in0=qTn, in1=gb[:], op=ALU.mult), [qTb, gb], [(qg_all, n)])
                    pT_ = nps("act")
                    k.op("pe", lambda e: e.matmul(pT_[0:P, 0:P], lhsT=A[:], rhs=identb[:], start=True, stop=True), [A, identb], [pT_])
                    k.op("act", lambda e: e.copy(out=Q0[:], in_=pT_[0:P, 0:P]), [pT_], [Q0])
                    k.op("pool", lambda e: e.tensor_tensor(out=W[:], in0=ident[0:P, 0:P], in1=Q0[:], op=ALU.subtract), [ident, Q0], [W])

                def stage_b(sl, lv):
                    Pk, Qk, W = Pm[sl][lv % 2], Qm[sl][lv % 2], Wm[sl][lv % 2]
                    Pn, Qn, Wn = Pm[sl][(lv + 1) % 2], Qm[sl][(lv + 1) % 2], Wm[sl][(lv + 1) % 2]
                    pP = nps("act")
                    k.op("pe", lambda e: e.matmul(pP[0:P, 0:P], lhsT=Qk[:], rhs=Pk[:], start=True, stop=True), [Qk, Pk], [pP])
                    if lv < 4:
                        pQ2 = nps("dve")
                        k.op("pe", lambda e: e.matmul(pQ2[0:P, 0:P], lhsT=Pk[:], rhs=Qk[:], start=True, stop=True), [Pk, Qk], [pQ2])
                    k.op("act", lambda e: e.copy(out=Pn[:], in_=pP[0:P, 0:P]), [pP], [Pn])
                    if lv < 4:
                        k.op("dve", lambda e: e.tensor_copy(out=Qn[:], in_=pQ2[0:P, 0:P]), [pQ2], [Qn])
                    pW = nps("dve")
                    k.op("pe", lambda e: e.matmul(pW[0:P, 0:P], lhsT=Pn[:], rhs=W[:], start=True, stop=True), [Pn, W], [pW])
                    k.op("dve", lambda e: e.tensor_tensor(out=Wn[:], in0=pW[0:P, 0:P], in1=W[:], op=ALU.add), [pW, W], [Wn])

                def stage_c(n, sl):
                    cs = slice(n * 64, (n + 1) * 64)
                    kTn, vTn = kTb[:, cs], vTb[:, cs]
                    btn = beta[:, n, h:h + 1]
                    W = Wm[sl][1]
                    pk_, pv_ = nps("dve"), nps("act")
                    k.op("pe", lambda e: e.matmul(pk_[0:P, 0:P], lhsT=kTn, rhs=identb[:], start=True, stop=True), [kTb, identb], [pk_])
                    k.op("pe", lambda e: e.matmul(pv_[0:P, 0:P], lhsT=vTn, rhs=identb[:], start=True, stop=True), [vTb, identb], [pv_])
                    kb_, bv_ = kbg[n % 2], bv[n % 2]
                    k.op("dve", lambda e: e.tensor_scalar(out=kb_[:], in0=pk_[0:P, 0:P], scalar1=bgam[:, n, h:h + 1], scalar2=None, op0=ALU.mult), [pk_, bgam], [kb_])
                    k.op("dve", lambda e: e.tensor_scalar(out=kt_all[:, n, :], in0=pk_[0:P, 0:P], scalar1=ktl[:, n, h:h + 1], scalar2=None, op0=ALU.mult), [pk_, ktl], [(kt_all, n)])
                    k.op("act", lambda e: e.activation(out=bv_[:], in_=pv_[0:P, 0:P], func=AF.Copy, scale=btn), [pv_, beta], [bv_])
                    pu, pw = nps("act"), nps("dve")
                    k.op("pe", lambda e: e.matmul(pu[0:P, 0:P], lhsT=W[:], rhs=bv_[:], start=True, stop=True), [W, bv_], [pu])
                    k.op("pe", lambda e: e.matmul(pw[0:P, 0:P], lhsT=kb_[:], rhs=W[:], start=True, stop=True), [kb_, W], [pw])
                    k.op("act", lambda e: e.copy(out=u_all[:, n, :], in_=pu[0:P, 0:P]), [pu], [(u_all, n)])
                    k.op("dve", lambda e: e.tensor_copy(out=wT_all[:, cs], in_=pw[0:P, 0:P]), [pw], [(wT_all, n)])

                ngrp = NCH // G
                for gi in range(ngrp + 1):
                    for _ in range(7):
                        next(nxt_conv, None)
                    base = (gi % 2) * G
                    if gi < ngrp:
                        for c in range(G):
                            stage_a(gi * G + c, base + c)
                    for lv in range(5):
                        if gi < ngrp:
                            for c in range(G):
                                stage_b(base + c, lv)
                        if gi >= 1:
                            for sj in range(lv * G // 5, (lv + 1) * G // 5):
                                St = scan_step((gi - 1) * G + sj, St)
                    if gi < ngrp:
                        for c in range(G):
                            stage_c(gi * G + c, base + c)
                k.op("dve", lambda e: e.tensor_tensor(out=cv[:].rearrange("p (n d) -> p n d", d=64), in0=o_all[:], in1=o_all[:], op=ALU.mult), [o_all], [cv])
                k.op("dve", lambda e: e.tensor_reduce(out=rsd[:, 0, :], in_=cv[:].rearrange("p (n d) -> p n d", d=64), axis=AX.X, op=ALU.add), [cv], [(rsd, 0)])
                k.op("act", lambda e: e.activation(out=rsd[:, 1, :], in_=rsd[:, 0, :], func=AF.Sqrt, scale=1.0 / 64, bias=EPS), [(rsd, 0)], [(rsd, 1)])
                k.op("dve", lambda e: e.reciprocal(out=rsd[:, 2, :], in_=rsd[:, 1, :]), [(rsd, 1)], [(rsd, 2)])
                k.dma("sp", xp[:, 4:S + 4], projT[2208 + h * 64:2208 + (h + 1) * 64, :], reads=[projT.tensor], writes=[(xp, "x")])
                k.op("act", lambda e: e.activation(out=xp[:, 4:S + 4], in_=xp[:, 4:S + 4], func=AF.Silu), [(xp, "x")], [(xp, "x")])
                for b in range(NB):
                    pp = npsf("dve")
                    for j in range(8):
                        n = b * 8 + j
                        k.op("dve", lambda e: e.tensor_scalar(out=o_all[:, n, :], in0=o_all[:, n, :], scalar1=rsd[:, 2, n:n + 1], scalar2=None, op0=ALU.mult),
                             [(o_all, n), (rsd, 2)], [(o_all, n)])
                        k.op("pe", lambda e: e.transpose(out=pp[0:P, j * 64:(j + 1) * 64], in_=o_all[:, n, :], identity=ident[0:P, 0:P]), [(o_all, n), ident], [pp], inc=(j == 7))
                    cs = slice(b * 512, (b + 1) * 512)
                    k.op("dve", lambda e: e.scalar_tensor_tensor(out=cv[:, cs], in0=pp[0:P, :], scalar=gv[0:P, 16:17], in1=xp[:, 4 + b * 512:4 + (b + 1) * 512], op0=ALU.mult, op1=ALU.mult),
                         [pp, gv, (xp, "x")], [(cv, b)])
                k.dma("sp", g.oT[512 + h * 64:512 + (h + 1) * 64, :], cv[:], reads=[cv], writes=[(g.oT.tensor, ("g", h))])
            k.barrier()


def phase4(g):
    nc, k = g.nc, g.k
    w_out = g.din("w_out", [D, D])
    router_w = g.din("router_w", [D, NE])
    router_b = g.din("router_b", [1, NE])
    x, ident, ones = g.x, g.ident, g.ones
    with contextlib.ExitStack() as p4:
        woutb = k.sb("woutb", [128, 8, D], BF16, p4)
        wov = w_out.rearrange("(c p) n -> p c n", p=128)
        k.dma("pool", woutb[:], wov, writes=[woutb])
        rw = k.sb("rw", [128, 8, NE], F32, p4)
        k.dma("sp", rw[:], router_w.rearrange("(c p) n -> p c n", p=128), writes=[rw])
        rb = k.sb("rb", [1, NE], F32, p4)
        k.dma("sp", rb[:], router_b, writes=[rb])
        bcs = {}
        for nm, slot in (("gt1", 2), ("sh2", 3), ("s2", 4)):
            bcs[nm] = k.sb(nm + "_bc", [128, D], F32, p4)
            k.dma("sp", bcs[nm][:], g.modscr[slot], reads=[(g.modscr.tensor, slot)], writes=[bcs[nm]])
        oTb = [k.sb("oTb%d" % i, [128, 8, 512], BF16, p4) for i in range(2)]
        xts = [k.sb("xq%d" % i, [128, D], F32, p4) for i in range(2)]
        x1s = [k.sb("x1s%d" % i, [128, D], F32, p4) for i in range(2)]
        hts = [k.sb("h2t%d" % i, [128, D], F32, p4) for i in range(2)]
        junk = k.sb("junk4", [128, D], BF16, p4)
        stat = k.sb("stat4", [128, 4, NT], F32, p4)
        h2Tf = [k.sb("h2Tf%d" % i, [128, 8, 128], F32, p4) for i in range(2)]
        h2Tb = [k.sb("h2Tb%d" % i, [128, 8, 512], BF16, p4) for i in range(2)]
        lg = k.sb("lg", [128, NT, NE], F32, p4)
        m8 = k.sb("m8", [128, NT, 8], F32, p4)
        rt = k.sb("rt", [128, 4, NT], F32, p4)
        msk = [k.sb("msk%d" % i, [128, NE], F32, p4) for i in range(2)]
        ex = [k.sb("ex%d" % i, [128, NE], F32, p4) for i in range(2)]
        pmx = [k.ps("pmx%d" % i, [128, 512], F32, p4) for i in range(4)]
        ptr = [k.ps("ptr4_%d" % i, [128, 4, 128], F32, p4) for i in range(2)]
        plg = [k.ps("plg%d" % i, [128, NE], F32, p4) for i in range(2)]
        oTv = g.oT.rearrange("(c p) s -> p c s", p=128)
        h2Tv = g.h2T.rearrange("(c p) s -> p c s", p=128)
        for blk in range(NB):
            ob = oTb[blk % 2]
            k.dma("pool", ob[:], oTv[:, :, blk * 512:(blk + 1) * 512], reads=[g.oT.tensor], writes=[ob])
            for tt in range(4):
                t = blk * 4 + tt
                xt = xts[t % 2]
                x1t = x1s[t % 2]
                ht = hts[t % 2]
                k.dma("sp", xt[:], x[t * 128:(t + 1) * 128, :], writes=[xt])
                for half in range(2):
                    pp = pmx[(2 * t + half) % 4]
                    for kc in range(8):
                        k.op("pe", lambda e: e.matmul(pp[:], lhsT=ob[:, kc, tt * 128:(tt + 1) * 128], rhs=woutb[:, kc, half * 512:(half + 1) * 512],
                                                      start=(kc == 0), stop=(kc == 7)), [ob, woutb], [pp], inc=(kc == 7))
                    hs = slice(half * 512, (half + 1) * 512)
                    k.op("dve", lambda e: e.tensor_tensor(out=x1t[:, hs], in0=pp[:], in1=bcs["gt1"][:, hs], op=ALU.mult), [pp, bcs["gt1"]], [(x1t, half)])
                    k.op("dve", lambda e: e.tensor_tensor(out=x1t[:, hs], in0=x1t[:, hs], in1=xt[:, hs], op=ALU.add), [(x1t, half), xt], [(x1t, half)])
                k.dma("sp", g.x1[t * 128:(t + 1) * 128, :], x1t[:], reads=[x1t], writes=[(g.x1.tensor, t)])
                if g.cfg.get("p4_level", 9) < 0.2:
                    continue
                rs, rskey = rms_tile(g, x1t, stat, t, junk, 1.0 / D)
                if g.cfg.get("p4_level", 9) < 0.27:
                    continue
                k.op("dve", lambda e: e.scalar_tensor_tensor(out=ht[:], in0=x1t[:], scalar=rs, in1=bcs["s2"][:], op0=ALU.mult, op1=ALU.mult),
                     [x1t, rskey, bcs["s2"]], [ht])
                if g.cfg.get("p4_level", 9) < 0.29:
                    continue
                k.op("dve", lambda e: e.tensor_tensor(out=ht[:], in0=ht[:], in1=bcs["sh2"][:], op=ALU.add), [ht, bcs["sh2"]], [ht])
                if g.cfg.get("p4_level", 9) < 0.5:
                    continue
                hf = h2Tf[t % 2]
                hb = h2Tb[blk % 2]
                for half in range(2):
                    pt = ptr[half]
                    for j in range(4):
                        kc = half * 4 + j
                        k.op("pe", lambda e: e.transpose(out=pt[:, j, :], in_=ht[:, kc * 128:(kc + 1) * 128], identity=ident[:]),
                             [ht, ident], [pt], inc=(j == 3))
                    k.op("act", lambda e: e.copy(out=hf[:, half * 4:half * 4 + 4, :], in_=pt[:]), [pt], [(hf, half)])
                    k.op("dve", lambda e: e.tensor_copy(out=hb[:, half * 4:half * 4 + 4, tt * 128:(tt + 1) * 128], in_=hf[:, half * 4:half * 4 + 4, :]),
                         [(hf, half)], [(hb, (half, tt))])
                if tt == 3 and g.cfg.get("p4_level", 9) >= 0.7:
                    for kc in range(8):
                        k.dma("sp", g.h2T[kc * 128:(kc + 1) * 128, blk * 512:(blk + 1) * 512], hb[:, kc, :], reads=[hb], writes=[(g.h2T.tensor, (blk, kc))])
                if g.cfg.get("p4_level", 9) < 2:
                    continue
                pl = plg[t % 2]
                for kc in range(8):
                    k.op("pe", lambda e: e.matmul(pl[:], lhsT=hf[:, kc, :], rhs=rw[:, kc, :], start=(kc == 0), stop=False), [hf, rw], [pl], inc=False)
                k.op("pe", lambda e: e.matmul(pl[:], lhsT=ones[0:1, :], rhs=rb[0:1, :], start=False, stop=True), [ones, rb], [pl])
                lgt = lg[:, t, :]
                k.op("act", lambda e: e.copy(out=lgt, in_=pl[:]), [pl], [(lg, t)])
                k.op("dve", lambda e: e.max(out=m8[:, t, :], in_=lgt), [(lg, t)], [(m8, t)])
                mk = msk[t % 2]
                et = ex[t % 2]
                k.op("dve", lambda e: e.tensor_scalar(out=mk[:], in0=lgt, scalar1=m8[:, t, 3:4], scalar2=None, op0=ALU.is_ge), [(lg, t), (m8, t)], [mk])
                k.op("dve", lambda e: e.tensor_scalar(out=rt[:, 0, t:t + 1], in0=m8[:, t, 0:1], scalar1=-1.0, scalar2=None, op0=ALU.mult), [(m8, t)], [(rt, (t, 0))])
                k.op("act", lambda e: e.activation(out=et[:], in_=lgt, func=AF.Exp, bias=rt[:, 0, t:t + 1], scale=1.0), [(lg, t), (rt, (t, 0))], [et])
                k.op("dve", lambda e: e.tensor_tensor(out=et[:], in0=et[:], in1=mk[:], op=ALU.mult), [et, mk], [et])
                k.op("dve", lambda e: e.reduce_sum(out=rt[:, 1, t:t + 1], in_=et[:], axis=AX.X), [et], [(rt, (t, 1))])
                k.op("dve", lambda e: e.reciprocal(out=rt[:, 2, t:t + 1], in_=rt[:, 1, t:t + 1]), [(rt, (t, 1))], [(rt, (t, 2))])
                k.op("dve", lambda e: e.tensor_scalar(out=g.wr[:, t, :], in0=et[:], scalar1=rt[:, 2, t:t + 1], scalar2=None, op0=ALU.mult),
                     [et, (rt, (t, 2))], [(g.wr, t)])
        k.barrier()


def phase5(g):
    nc, k = g.nc, g.k
    cfg = g.cfg
    n_exp = cfg.get("n_exp", NE)
    g.cast_some(12)
    bgu_t = g.din("bgu_t", [128, NE, 16])
    b_dn = g.din("b_down", [NE, D])
    ident = g.ident
    PT = 1024
    npass = S // PT
    with contextlib.ExitStack() as p5:
        gt2 = k.sb("gt2_bc", [128, D], F32, p5)
        fg = k.sb("fg_bc", [128, D], F32, p5)
        k.dma("sp", gt2[:], g.modscr[5], reads=[(g.modscr.tensor, 5)], writes=[gt2])
        k.dma("sp", fg[:], g.gvec[2], writes=[fg])
        bgu = k.sb("bgu", [128, NE, 16], F32, p5)
        k.dma("sp", bgu[:], bgu_t, writes=[bgu])
        bgu1 = k.sb("bgu1", [128, NE, 16], F32, p5)
        k.op("dve", lambda e: e.tensor_scalar(out=bgu1[:], in0=bgu[:], scalar1=1.0, scalar2=None, op0=ALU.add), [bgu], [bgu1])
        bdn = k.sb("bdn", [NE, D], F32, p5)
        k.dma("sp", bdn[:], b_dn, writes=[bdn])
        h2s = k.sb("h2s", [128, 8, PT], BF16, p5)
        yacc = k.sb("yacc", [128, PT // 128, D], F32, p5)
        wgu = [k.sb("wgu%d" % i, [128, 8, 2 * D], BF16, p5) for i in range(2)]
        wdn = [k.sb("wdn%d" % i, [128, 8, D], BF16, p5) for i in range(2)]
        actT = [k.sb("actT%d" % i, [128, 8, 512], BF16, p5) for i in range(2)]
        gS = [k.sb("gS%d" % i, [128, 512], F32, p5) for i in range(2)]
        sS = [k.sb("sS%d" % i, [128, 512], F32, p5) for i in range(2)]
        uS = [k.sb("uS%d" % i, [128, 512], F32, p5) for i in range(2)]
        wrT = k.sb("wrT", [NE, PT], F32, p5)
        xa = [k.sb("xa%d" % i, [128, D], F32, p5) for i in range(2)]
        junk = k.sb("junk5", [128, D], BF16, p5)
        stat = k.sb("stat5", [128, 4, NT], F32, p5)
        pg = [k.ps("pg%d" % i, [128, 512], F32, p5) for i in range(2)]
        pu = [k.ps("pu%d" % i, [128, 512], F32, p5) for i in range(2)]
        pd = [k.ps("pd%d" % i, [128, 512], F32, p5) for i in range(4)]
        h2Tv = g.h2T.rearrange("(c p) s -> p c s", p=128)
        wguv = g.wgu_bf.rearrange("e (c p) n -> e p c n", p=128)
        wdnv = g.wdn_bf.rearrange("e (c p) n -> e p c n", p=128)

        n_tot = npass * n_exp

        def load_wg(ei):
            if ei >= n_tot:
                return
            wg = wgu[ei % 2]
            e_ = ei % n_exp
            for kc in range(0, 8, 4):
                k.dma("sp", wg[:, kc:kc + 4, :], wguv[e_, :, kc:kc + 4, :], reads=[(g.wgu_bf.tensor, e_)], writes=[(wg, kc + j) for j in range(4)])

        def load_wd(ei):
            if ei >= n_tot:
                return
            wd = wdn[ei % 2]
            e_ = ei % n_exp
            k.dma("sp", wd[:], wdnv[e_], reads=[(g.wdn_bf.tensor, e_)], writes=[(wd, kc) for kc in range(8)])

        fci = 0
        pdi = 0
        NBK = PT // 512

        def emit_gu(ei, bk):
            nonlocal fci
            e_ = ei % n_exp
            wg = wgu[ei % 2]
            aT = actT[(ei * NBK + bk) % 2]
            rhs_cols = slice(bk * 512, (bk + 1) * 512)
            for fc in range(8):
                pgt = pg[fci % 2]
                put = pu[fci % 2]
                g_ = gS[fci % 2]
                s_ = sS[fci % 2]
                u_ = uS[fci % 2]
                fci += 1
                for kc in range(8):
                    k.op("pe", lambda e: e.matmul(pgt[:], lhsT=wg[:, kc, fc * 128:(fc + 1) * 128], rhs=h2s[:, kc, rhs_cols],
                                                  start=(kc == 0), stop=(kc == 7)), [(wg, kc), h2s], [pgt], inc=(kc == 7))
                for kc in range(8):
                    k.op("pe", lambda e: e.matmul(put[:], lhsT=wg[:, kc, D + fc * 128:D + (fc + 1) * 128], rhs=h2s[:, kc, rhs_cols],
                                                  start=(kc == 0), stop=(kc == 7)), [(wg, kc), h2s], [put], inc=(kc == 7))
                k.op("dve", lambda e: e.tensor_scalar(out=g_[:], in0=pgt[:], scalar1=bgu[:, e_, fc:fc + 1], scalar2=7.0, op0=ALU.add, op1=ALU.min),
                     [pgt, bgu], [g_])
                k.op("act", lambda e: e.activation(out=s_[:], in_=g_[:], func=AF.Sigmoid, scale=1.702), [g_], [s_])
                k.op("dve", lambda e: e.tensor_scalar(out=u_[:], in0=put[:], scalar1=bgu1[:, e_, 8 + fc:9 + fc], scalar2=8.0, op0=ALU.add, op1=ALU.min),
                     [put, bgu1], [u_])
                k.op("dve", lambda e: e.tensor_tensor(out=g_[:], in0=g_[:], in1=s_[:], op=ALU.mult), [g_, s_], [g_])
                k.op("dve", lambda e: e.scalar_tensor_tensor(out=aT[:, fc, :], in0=u_[:], scalar=-6.0, in1=g_[:], op0=ALU.max, op1=ALU.mult), [g_, u_], [(aT, fc)])
            if bk == NBK - 1:
                load_wg(ei + 2)

        def emit_down(ei, bk, t0):
            nonlocal pdi
            e_ = ei % n_exp
            wd = wdn[ei % 2]
            aT = actT[(ei * NBK + bk) % 2]
            for tt in range(4):
                tl = bk * 4 + tt
                for half in range(2):
                    pp = pd[pdi % 4]
                    pdi += 1
                    for fc in range(8):
                        k.op("pe", lambda e: e.matmul(pp[:], lhsT=aT[:, fc, tt * 128:(tt + 1) * 128], rhs=wd[:, fc, half * 512:(half + 1) * 512],
                                                      start=(fc == 0), stop=(fc == 7)), [(aT, fc), (wd, fc)], [pp], inc=(fc == 7))
                    ya = yacc[:, tl, half * 512:(half + 1) * 512]
                    k.op("dve", lambda e: e.scalar_tensor_tensor(out=ya, in0=pp[:], scalar=g.wr[:, t0 + tl, e_:e_ + 1], in1=ya, op0=ALU.mult, op1=ALU.add),
                         [pp, (g.wr, t0 + tl), (yacc, (tl, half))], [(yacc, (tl, half))])
            if bk == NBK - 1:
                load_wd(ei + 2)

        for i in range(2):
            load_wg(i)
            load_wd(i)
        for ps_ in range(npass):
            t0 = ps_ * (PT // 128)
            k.dma("sp", h2s[:], h2Tv[:, :, ps_ * PT:(ps_ + 1) * PT], reads=[g.h2T.tensor], writes=[h2s])
            for tl in range(PT // 128):
                pt = pd[pdi % 4]
                pdi += 1
                k.op("pe", lambda e: e.transpose(out=pt[0:NE, 0:128], in_=g.wr[:, t0 + tl, :], identity=ident[:]), [(g.wr, t0 + tl), ident], [pt])
                k.op("act", lambda e: e.copy(out=wrT[:, tl * 128:(tl + 1) * 128], in_=pt[0:NE, 0:128]), [pt], [(wrT, tl)])
                for half in range(2):
                    pp = pd[pdi % 4]
                    pdi += 1
                    k.op("pe", lambda e: e.matmul(pp[:], lhsT=wrT[:, tl * 128:(tl + 1) * 128], rhs=bdn[:, half * 512:(half + 1) * 512], start=True, stop=True),
                         [(wrT, tl), bdn], [pp])
                    k.op("dve", lambda e: e.tensor_copy(out=yacc[:, tl, half * 512:(half + 1) * 512], in_=pp[:]), [pp], [(yacc, (tl, half))])
            units = [(ps_ * n_exp + e_, bk) for e_ in range(n_exp) for bk in range(NBK)]
            emit_gu(*units[0])
            for i, (ei_, bk_) in enumerate(units):
                if i + 1 < len(units):
                    emit_gu(*units[i + 1])
                emit_down(ei_, bk_, t0)
            for tl in range(PT // 128):
                t = t0 + tl
                xt = xa[t % 2]
                k.dma("sp", xt[:], g.x1[t * 128:(t + 1) * 128, :], reads=[(g.x1.tensor, t)], writes=[xt])
                ya = yacc[:, tl, :]
                k.op("dve", lambda e: e.tensor_tensor(out=ya, in0=ya, in1=gt2[:], op=ALU.mult), [(yacc, (tl, 0)), (yacc, (tl, 1)), gt2], [(yacc, (tl, 0)), (yacc, (tl, 1))])
                k.op("dve", lambda e: e.tensor_tensor(out=xt[:], in0=xt[:], in1=ya, op=ALU.add), [xt, (yacc, (tl, 0)), (yacc, (tl, 1))], [xt])
                rs, rskey = rms_tile(g, xt, stat, t, junk, 1.0 / D)
                k.op("dve", lambda e: e.scalar_tensor_tensor(out=xt[:], in0=xt[:], scalar=rs, in1=fg[:], op0=ALU.mult, op1=ALU.mult),
                     [xt, rskey, fg], [xt])
                k.dma("sp", g.out[t * 128:(t + 1) * 128, :], xt[:], reads=[xt], writes=[(g.out.tensor, t)])
        k.barrier()


def _consts():
    c = np.zeros((4, 128, 128), np.float32)
    c[0] = np.eye(128, dtype=np.float32)
    c[1] = 1.0
    c[2] = np.triu(np.ones((128, 128), np.float32))
    return c


def make_in_map(inp, b, names=None):
    f = lambda a: np.ascontiguousarray(np.asarray(a, dtype=np.float32))
    m = {}
    m["x"] = f(inp["x"][b])
    m["c_t"] = f(np.asarray(inp["c"][b]).reshape(8, 128).T)
    m["ada_w"] = f(inp["ada_w"][0])
    m["ada_b"] = f(inp["ada_b"][0]).reshape(1, -1)
    gv = np.stack([np.broadcast_to(np.asarray(inp[n]).reshape(-1), (128, D)) for n in ("norm1_g", "norm2_g", "final_g")])
    m["gvec"] = f(gv)
    m["w_in"] = f(inp["w_in"][0])
    m["consts"] = _consts()
    m["w_out"] = f(inp["w_out"][0])
    m["router_w"] = f(inp["router_w"][0])
    m["router_b"] = f(inp["router_b"][0]).reshape(1, -1)
    m["w_gate_up"] = f(inp["w_gate_up"][0])
    m["w_down"] = f(inp["w_down"][0])
    m["bgu_t"] = f(np.asarray(inp["b_gate_up"][0]).reshape(NE, 16, 128).transpose(2, 0, 1))
    m["b_down"] = f(inp["b_down"][0])
    m["w_q_b"] = f(inp["w_q_b"][0])
    m["w_kv_b"] = f(inp["w_kv_b"][0])
    mv = np.zeros((128, 16), np.float32)
    mv[:, 0:3] = np.asarray(inp["q_norm_g"][0]).reshape(3, 128).T
    mv[:, 3:5] = np.asarray(inp["kv_norm_g"][0]).reshape(2, 128).T
    mv[:, 5:9] = np.asarray(inp["mla_out_g"][0]).reshape(4, 128).T
    pidx = np.arange(128)
    mv[:, 9] = (np.float32(10000.0) ** (-(pidx % 16).astype(np.float32) / np.float32(16))).astype(np.float32)
    mv[:, 10] = np.where((pidx % 32) < 16, -1.0, 1.0)
    m["mla_vec"] = mv
    m["pos_bc"] = np.ascontiguousarray(np.broadcast_to(np.asarray(inp["positions"][b]).astype(np.int32).reshape(1, S), (128, S)))
    gvv = np.zeros((128, 32), np.float32)
    gvv[:, 0:8] = np.asarray(inp["A_log"][0]).reshape(1, 8)
    gvv[:, 8:16] = np.asarray(inp["dt_bias"][0]).reshape(1, 8)
    gvv[0:64, 16] = np.asarray(inp["gdn_norm_g"][0]).reshape(64)
    gvv[64:128, 16] = np.asarray(inp["gdn_norm_g"][0]).reshape(64)
    m["gdn_vec"] = gvv
    m["conv_wt"] = f(np.asarray(inp["conv_w"][0]).reshape(4, 24, 64).transpose(2, 1, 0))
    if names is not None:
        m = {n: v for n, v in m.items() if n in names}
    return m


def input_names(nc):
    return None


_CACHE = {}


def kernel(**inputs):
    if "nc" not in _CACHE:
        _CACHE["nc"] = build({})
    nc, kb = _CACHE["nc"]
    in_maps = [make_in_map(inputs, b) for b in range(8)]
    res = run_bass_kernel_spmd(nc, in_maps, core_ids=list(range(8)))
    out = np.stack([np.asarray(r["out"], dtype=np.float32) for r in res.results], axis=0)
    return out
```

```python
import contextlib
import numpy as np
import concourse.bass as bass
import concourse.mybir as mybir
from concourse.bass_utils import run_bass_kernel_spmd

F32 = mybir.dt.float32
BF16 = mybir.dt.bfloat16
AF = mybir.ActivationFunctionType
ALU = mybir.AluOpType
AX = mybir.AxisListType

S = 4096
D = 1024
NT = S // 128
NB = S // 512
IN_DIM = 2736
EPS = 1e-6
NE = 32


class KB:
    def __init__(self, nc):
        self.nc = nc
        self.es = contextlib.ExitStack()
        self.E = {}
        for name, eng in [("pe", nc.tensor), ("act", nc.scalar), ("dve", nc.vector),
                          ("pool", nc.gpsimd), ("sp", nc.sync)]:
            sem = self.es.enter_context(nc.semaphore("sem_" + name))
            self.E[name] = dict(eng=eng, sem=sem, cnt=0, seen={})
        self.sems = {n: e["sem"] for n, e in self.E.items()}
        self.ndma = 24
        self.dval = []
        for i in range(self.ndma):
            self.sems[("d", i)] = self.es.enter_context(nc.semaphore("sem_d%d" % i))
            self.dval.append(0)
        self.dptr = 0
        self.nw = 8
        for i in range(self.nw):
            self.sems[("d", self.ndma + i)] = self.es.enter_context(nc.semaphore("sem_w%d" % i))
            self.dval.append(0)
        self.wptr = 0
        self.nsw = 40
        self.swptr = 0
        for i in range(self.nsw):
            self.sems[("s", i)] = self.es.enter_context(nc.semaphore("sem_s%d" % i))
        self.state = {}
        self.n_ins = 0
        for sk, sem in self.sems.items():
            nc.gpsimd.sem_clear(sem)
        nc.all_engine_barrier()

    def sb(self, name, shape, dt, es=None):
        return (es or self.es).enter_context(self.nc.sbuf_tensor(name, list(shape), dt))

    def ps(self, name, shape, dt, es=None):
        return (es or self.es).enter_context(self.nc.psum_tensor(name, list(shape), dt))

    def _new(self):
        return {"w": None, "r": {}}

    def _sts(self, key):
        if not isinstance(key, tuple):
            key = (key, None)
        t, sub = key
        d = self.state.setdefault(id(t), {})
        if sub is None:
            if None not in d:
                d[None] = self._new()
            return list(d.values())
        if sub not in d:
            d[sub] = self._new()
        res = [d[sub]]
        if None in d:
            res.append(d[None])
        return res

    def _collect(self, ename, reads, writes):
        need = {}

        def add(sk, val):
            if sk == "pe" and ename == "pe":
                return
            if need.get(sk, 0) < val:
                need[sk] = val
        for k in reads:
            for st in self._sts(k):
                if st["w"] is not None:
                    add(*st["w"])
        for k in writes:
            for st in self._sts(k):
                if st["w"] is not None:
                    add(*st["w"])
                for sk, v in st["r"].items():
                    add(sk, v)
        return need

    def _wait(self, ename, need):
        e = self.E[ename]
        for sk, val in need.items():
            if e["seen"].get(sk, 0) < val:
                e["eng"].wait_ge(self.sems[sk], val)
                e["seen"][sk] = val

    def _update(self, reads, writes, sk, val):
        for k in reads:
            if not isinstance(k, tuple):
                k = (k, None)
            sts = self._sts(k)
            if k[1] is None:
                for st in sts:
                    st["r"][sk] = max(st["r"].get(sk, 0), val)
            else:
                sts[0]["r"][sk] = max(sts[0]["r"].get(sk, 0), val)
        for k in writes:
            if not isinstance(k, tuple):
                k = (k, None)
            sts = self._sts(k)
            if k[1] is None:
                for st in sts:
                    st["w"] = (sk, val)
                    st["r"] = {}
            else:
                sts[0]["w"] = (sk, val)
                sts[0]["r"] = {}

    def op(self, ename, fn, reads=(), writes=(), inc=True):
        e = self.E[ename]
        self._wait(ename, self._collect(ename, reads, writes))
        ins = fn(e["eng"])
        val = e["cnt"] + 1
        if inc:
            ins.then_inc(e["sem"], 1)
            e["cnt"] = val
        self._update(reads, writes, ename, val)
        self.n_ins += 1
        return ins

    def dma(self, ename, out, in_, reads=(), writes=(), wpool=False, **kw):
        e = self.E[ename]
        if ename == "pool":
            sk = ("s", self.swptr)
            self.swptr += 1
            assert self.swptr <= self.nsw, "out of single-use SW-DMA semaphores"
            self._wait(ename, self._collect(ename, reads, writes))
            ins = e["eng"].dma_start(out=out, in_=in_, **kw)
            ins.then_inc(self.sems[sk], 16)
            self._update(reads, writes, sk, 16)
            self.n_ins += 1
            return sk, 16
        if wpool:
            i = self.ndma + self.wptr
            self.wptr = (self.wptr + 1) % self.nw
        else:
            i = self.dptr
            self.dptr = (self.dptr + 1) % self.ndma
        sk = ("d", i)
        need = self._collect(ename, reads, writes)
        if self.dval[i] > 0:
            need[sk] = max(need.get(sk, 0), self.dval[i])
        self._wait(ename, need)
        ins = e["eng"].dma_start(out=out, in_=in_, **kw)
        self.dval[i] += 16
        ins.then_inc(self.sems[sk], 16)
        self._update(reads, writes, sk, self.dval[i])
        self.n_ins += 1
        return sk, self.dval[i]

    def barrier(self):
        for ename, e in self.E.items():
            for sk in self.sems:
                if isinstance(sk, tuple) and sk[0] == "s":
                    val = 16 if sk[1] < self.swptr else 0
                else:
                    val = self.E[sk]["cnt"] if sk in self.E else self.dval[sk[1]]
                if val > 0 and e["seen"].get(sk, 0) < val and sk != ename:
                    e["eng"].wait_ge(self.sems[sk], val)
                    e["seen"][sk] = val

    def finish(self, ename, keys):
        need = self._collect(ename, keys, ())
        self._wait(ename, need)


class Ctx:
    pass


class PV(tuple):
    def __new__(cls, tile, q):
        return super().__new__(cls, (tile, ("q", q)))

    def __getitem__(self, idx):
        if isinstance(idx, int):
            return tuple.__getitem__(self, idx)
        tile = tuple.__getitem__(self, 0)
        q = tuple.__getitem__(self, 1)[1]
        p_, c_ = idx
        return tile[p_, q * 128 + c_.start:q * 128 + c_.stop]


def build(cfg):
    nc = bass.Bass("TRN2", target_bir_lowering=False)
    k = KB(nc)
    dbg = cfg.get("dbg", ())
    phases = cfg.get("phases", ("p0", "p1", "p2", "p3", "p4", "p5"))
    g = Ctx()
    g.nc, g.k, g.dbg, g.cfg = nc, k, dbg, cfg

    def din(name, shape, dt=F32):
        return nc.dram_tensor(name, list(shape), dt, kind="ExternalInput").ap()

    def dout(name, shape, dt=F32):
        return nc.dram_tensor(name, list(shape), dt, kind="ExternalOutput").ap()

    def dscr(name, shape, dt=F32):
        if name in dbg:
            return dout(name, shape, dt)
        if name in cfg.get("as_input", ()):
            return din(name, shape, dt)
        return nc.dram_tensor(name, list(shape), dt, kind="Internal").ap()
    g.din, g.dout, g.dscr = din, dout, dscr

    g.x = din("x", [S, D])
    g.consts = din("consts", [4, 128, 128])
    g.gvec = din("gvec", [3, 128, D])
    g.modscr = dscr("modscr", [6, 128, D])
    g.projT = dscr("projT", [IN_DIM, S])
    g.oT = dscr("oT", [D, S])
    g.x1 = dscr("x1", [S, D])
    g.h2T = dscr("h2T", [D, S], BF16)
    g.out = dout("out", [S, D])
    if "p5" in phases:
        g.w_gu = din("w_gate_up", [NE, D, 2 * D])
        g.w_dn = din("w_down", [NE, D, D])
        g.wgu_bf = dscr("wgu_bf", [NE, D, 2 * D], BF16)
        g.wdn_bf = dscr("wdn_bf", [NE, D, D], BF16)
    g.cast_next = 0 if "p5" in phases else 12

    def cast_some(n):
        for _ in range(n):
            i = g.cast_next
            if i >= 12:
                return
            g.cast_next += 1
            order = [("g", 0), ("g", 1), ("d", 0), ("g", 2), ("g", 3), ("d", 1), ("g", 4), ("g", 5), ("d", 2), ("g", 6), ("g", 7), ("d", 3)]
            kind, j = order[i]
            if kind == "g":
                k.dma("pool", g.wgu_bf[4 * j:4 * j + 4], g.w_gu[4 * j:4 * j + 4], writes=[(g.wgu_bf.tensor, e_) for e_ in range(4 * j, 4 * j + 4)])
            else:
                k.dma("pool", g.wdn_bf[8 * j:8 * j + 8], g.w_dn[8 * j:8 * j + 8], writes=[(g.wdn_bf.tensor, e_) for e_ in range(8 * j, 8 * j + 8)])
    g.cast_some = cast_some
    g.outputs = [g.out.tensor]
    for n in dbg:
        pass

    with k.es:
        g.ident = k.sb("ident", [128, 128], F32)
        g.ones = k.sb("ones", [128, 128], F32)
        k.dma("sp", g.ident[:], g.consts[0], writes=[g.ident])
        k.dma("sp", g.ones[:], g.consts[1], writes=[g.ones])
        g.wr = k.sb("wr", [128, NT, NE], F32)
        if "p0" in phases:
            phase0(g)
        if "p1" in phases:
            phase1(g)
        if "p2" in phases:
            phase2(g)
        if "p3" in phases:
            phase3(g)
        if "p4" in phases:
            phase4(g)
        if "p5" in phases:
            phase5(g)
        fin = list(g.outputs)
        for n in dbg:
            fin.append(getattr(g, n).tensor)
        k.finish("sp", fin)
        k.barrier()
        nc.all_engine_barrier()
    return nc, k


def phase0(g):
    nc, k = g.nc, g.k
    ada_w = g.din("ada_w", [D, 6 * D])
    ada_b = g.din("ada_b", [1, 6 * D])
    c_t = g.din("c_t", [128, 8])
    ones = g.ones
    with contextlib.ExitStack() as p0:
        mod_bc = k.sb("mod_bc", [128, 6 * D], F32, p0)
        ct = k.sb("ct", [128, 8], F32, p0)
        cact = k.sb("cact", [128, 8], F32, p0)
        cbc = k.sb("cbc", [128, 8, 128], F32, p0)
        adab = k.sb("adab", [1, 6 * D], F32, p0)
        g1 = k.sb("g1", [128, D], F32, p0)
        g2 = k.sb("g2", [128, D], F32, p0)
        awb = [k.sb("awb%d" % i, [128, 8, 512], F32, p0) for i in range(2)]
        pm = [k.ps("pm%d" % i, [128, 512], F32, p0) for i in range(2)]
        k.dma("sp", ct[:], c_t, writes=[ct])
        k.dma("sp", adab[:], ada_b, writes=[adab])
        k.dma("sp", g1[:], g.gvec[0], writes=[g1])
        k.dma("sp", g2[:], g.gvec[1], writes=[g2])
        k.op("act", lambda e: e.activation(out=cact[:], in_=ct[:], func=AF.Silu), [ct], [cact])
        for kc in range(8):
            k.op("dve", lambda e: e.tensor_scalar(out=cbc[:, kc, :], in0=ones[:], scalar1=cact[:, kc:kc + 1],
                                                  scalar2=None, op0=ALU.mult), [ones, cact], [(cbc, kc)])
        aw_v = ada_w.rearrange("(c p) n -> p c n", p=128)
        for blk in range(12):
            wb = awb[blk % 2]
            k.dma("sp", wb[:], aw_v[:, :, blk * 512:(blk + 1) * 512], writes=[wb])
            pp = pm[blk % 2]
            for kc in range(8):
                k.op("pe", lambda e: e.matmul(pp[:], lhsT=cbc[:, kc, :], rhs=wb[:, kc, :], start=(kc == 0), stop=False),
                     [(cbc, kc), wb], [pp], inc=False)
            k.op("pe", lambda e: e.matmul(pp[:], lhsT=ones[0:1, :], rhs=adab[0:1, blk * 512:(blk + 1) * 512], start=False, stop=True),
                 [ones, adab], [pp])
            k.op("act", lambda e: e.copy(out=mod_bc[:, blk * 512:(blk + 1) * 512], in_=pp[:]), [pp], [(mod_bc, blk)])
        k.op("dve", lambda e: e.scalar_tensor_tensor(out=mod_bc[:, D:2 * D], in0=mod_bc[:, D:2 * D], scalar=1.0, in1=g1[:],
                                                     op0=ALU.add, op1=ALU.mult), [mod_bc, g1], [mod_bc])
        k.op("dve", lambda e: e.scalar_tensor_tensor(out=mod_bc[:, 4 * D:5 * D], in0=mod_bc[:, 4 * D:5 * D], scalar=1.0, in1=g2[:],
                                                     op0=ALU.add, op1=ALU.mult), [mod_bc, g2], [mod_bc])
        for j in range(6):
            k.dma("sp", g.modscr[j], mod_bc[:, j * D:(j + 1) * D], reads=[mod_bc], writes=[(g.modscr.tensor, j)])
        k.barrier()


def rms_tile(g, xt, stat, col, junk, scale):
    k = g.k
    ss = stat[:, 0, col:col + 1]
    sd = stat[:, 1, col:col + 1]
    rs = stat[:, 2, col:col + 1]
    k.op("act", lambda e: e.activation(out=junk[:], in_=xt[:], func=AF.Square, accum_out=ss), [xt], [junk, (stat, (col, 0))])
    k.op("act", lambda e: e.activation(out=sd, in_=ss, func=AF.Sqrt, scale=scale, bias=EPS), [(stat, (col, 0))], [(stat, (col, 1))])
    k.op("dve", lambda e: e.reciprocal(out=rs, in_=sd), [(stat, (col, 1))], [(stat, (col, 2))])
    return rs, (stat, (col, 2))


def phase1(g):
    nc, k = g.nc, g.k
    w_in = g.din("w_in", [D, IN_DIM])
    x, ident, projT = g.x, g.ident, g.projT
    with contextlib.ExitStack() as p1:
        winb = k.sb("winb", [128, 8, IN_DIM], BF16, p1)
        wv = w_in.rearrange("(c p) n -> p c n", p=128)
        for hh in range(2):
            k.dma("pool", winb[:, :, hh * 1368:(hh + 1) * 1368], wv[:, :, hh * 1368:(hh + 1) * 1368], writes=[(winb, hh)])
        g.cast_some(12)
        s1_bc = k.sb("s1_bc", [128, D], F32, p1)
        sh1_bc = k.sb("sh1_bc", [128, D], F32, p1)
        k.dma("sp", sh1_bc[:], g.modscr[0], reads=[(g.modscr.tensor, 0)], writes=[sh1_bc])
        k.dma("sp", s1_bc[:], g.modscr[1], reads=[(g.modscr.tensor, 1)], writes=[s1_bc])
        xts = [k.sb("xt%d" % i, [128, D], F32, p1) for i in range(3)]
        junk = k.sb("junk", [128, D], BF16, p1)
        hts = [k.sb("ht%d" % i, [128, D], F32, p1) for i in range(2)]
        stat = k.sb("stat", [128, 4, NT], F32, p1)
        h1T = [k.sb("h1T%d" % i, [128, 8, 512], BF16, p1) for i in range(2)]
        stg = [k.sb("stg%d" % i, [128, 512], F32, p1) for i in range(4)]
        ptr = [k.ps("ptr%d" % i, [128, 4, 128], F32, p1) for i in range(4)]
        pmm = [k.ps("pmm%d" % i, [128, 512], F32, p1) for i in range(4)]
        nchunks = (IN_DIM + 127) // 128
        mmi = 0
        for t in range(NT):
            xt = xts[t % 3]
            ht = hts[t % 2]
            k.dma("sp", xt[:], x[t * 128:(t + 1) * 128, :], writes=[xt])
            rs, rskey = rms_tile(g, xt, stat, t, junk, 1.0 / D)
            k.op("dve", lambda e: e.scalar_tensor_tensor(out=ht[:], in0=xt[:], scalar=rs, in1=s1_bc[:], op0=ALU.mult, op1=ALU.mult),
                 [xt, rskey, s1_bc], [ht])
            k.op("dve", lambda e: e.tensor_tensor(out=ht[:], in0=ht[:], in1=sh1_bc[:], op=ALU.add), [ht, sh1_bc], [ht])
            hb = h1T[(t // 4) % 2]
            tt = t % 4
            for half in range(2):
                pt = ptr[(2 * t + half) % 4]
                for j in range(4):
                    kc = half * 4 + j
                    k.op("pe", lambda e: e.transpose(out=pt[:, j, :], in_=ht[:, kc * 128:(kc + 1) * 128], identity=ident[:]),
                         [ht, ident], [pt], inc=(j == 3))
                if half == 0:
                    k.op("act", lambda e: e.copy(out=hb[:, half * 4:half * 4 + 4, tt * 128:(tt + 1) * 128], in_=pt[:]), [pt], [(hb, (half, tt))])
                else:
                    k.op("dve", lambda e: e.tensor_copy(out=hb[:, half * 4:half * 4 + 4, tt * 128:(tt + 1) * 128], in_=pt[:]), [pt], [(hb, (half, tt))])
            if tt == 3:
                blk = t // 4
                for ci in range(nchunks):
                    c0 = ci * 128
                    cw = min(128, IN_DIM - c0)
                    pp = pmm[mmi % 4]
                    sg = stg[mmi % 4]
                    for kc in range(8):
                        k.op("pe", lambda e: e.matmul(pp[0:cw, :], lhsT=winb[:, kc, c0:c0 + cw], rhs=hb[:, kc, :],
                                                      start=(kc == 0), stop=(kc == 7)), [winb, hb], [pp], inc=(kc == 7))
                    if mmi % 2 == 0:
                        k.op("act", lambda e: e.copy(out=sg[0:cw, :], in_=pp[0:cw, :]), [pp], [sg])
                    else:
                        k.op("dve", lambda e: e.tensor_copy(out=sg[0:cw, :], in_=pp[0:cw, :]), [pp], [sg])
                    k.dma("sp", projT[c0:c0 + cw, blk * 512:(blk + 1) * 512], sg[0:cw, :], reads=[sg], writes=[(projT.tensor, (ci, blk))])
                    mmi += 1
        k.barrier()


def phase2(g):
    nc, k = g.nc, g.k
    HQ = 96
    wq_d = g.din("w_q_b", [384, 768])
    wkv_d = g.din("w_kv_b", [256, 1024])
    mvec = g.din("mla_vec", [128, 16])
    pos_d = g.din("pos_bc", [128, S], mybir.dt.int32)
    tri_d = g.consts[2]
    ident, ones, projT = g.ident, g.ones, g.projT
    heads = g.cfg.get("mla_heads", 8)
    TWO_PI = 2.0 * np.pi
    with contextlib.ExitStack() as p2:
        mv = k.sb("mv", [128, 16], F32, p2)
        k.dma("sp", mv[:], mvec, writes=[mv])
        tri = k.sb("tri", [128, 128], BF16, p2)
        trf = k.sb("trf", [128, 128], F32, p2)
        k.dma("sp", trf[:], tri_d, writes=[trf])
        k.op("dve", lambda e: e.tensor_copy(out=tri[:], in_=trf[:]), [trf], [tri])
        wqb = k.sb("wqb", [128, 3, 768], BF16, p2)
        wqr = k.sb("wqr", [128, 3, 768], BF16, p2)
        wkvb = k.sb("wkvb", [128, 2, 1024], BF16, p2)
        qlatn = k.sb("qlatn", [128, 3, S], BF16, p2)
        kvlatn = k.sb("kvlatn", [128, 2, S], BF16, p2)
        cosT = k.sb("cosT", [128, S], F32, p2)
        sinT = k.sb("sinT", [128, S], F32, p2)
        kper = k.sb("kper", [128, S], BF16, p2)
        mo = k.sb("mo", [128, NT, 512], BF16, p2)
        psA = [k.ps("psA%d" % i, [128, 512], F32, p2) for i in range(2)]
        psS = [k.ps("psS%d" % i, [128, 512], F32, p2) for i in range(2)]
        psO = [k.ps("psO%d" % i, [128, 512], F32, p2) for i in range(4)]
        with contextlib.ExitStack() as pa:
            wtmp = k.sb("wtmp", [128, 3, 1024], F32, pa)
            k.dma("sp", wtmp[:, :, 0:768], wq_d.rearrange("(c p) n -> p c n", p=128), writes=[wtmp])
            for c in range(3):
                k.op("dve", lambda e: e.tensor_scalar(out=wtmp[:, c, 0:768], in0=wtmp[:, c, 0:768], scalar1=mv[:, c:c + 1], scalar2=float(HQ ** -0.5),
                                                      op0=ALU.mult, op1=ALU.mult), [wtmp, mv], [wtmp])
            k.op("act", lambda e: e.copy(out=wqb[:], in_=wtmp[:, :, 0:768]), [wtmp], [wqb])
            k.op("dve", lambda e: e.memset(wqr[:], 0.0), [], [wqr])
            w4 = wtmp[:, :, 0:768].rearrange("p c (h d) -> p c h d", d=HQ)
            r4 = wqr[:].rearrange("p c (h d) -> p c h d", d=HQ)
            for c in range(3):
                k.op("dve", lambda e: e.tensor_scalar(out=r4[:, c, :, 64:80], in0=w4[:, c, :, 80:96], scalar1=-1.0, scalar2=None, op0=ALU.mult), [wtmp, wqr], [wqr])
                k.op("dve", lambda e: e.tensor_copy(out=r4[:, c, :, 80:96], in_=w4[:, c, :, 64:80]), [wtmp, wqr], [wqr])
            wtmp2 = k.sb("wtmp2", [128, 2, 1024], F32, pa)
            k.dma("sp", wtmp2[:], wkv_d.rearrange("(c p) n -> p c n", p=128), writes=[wtmp2])
            for c in range(2):
                k.op("dve", lambda e: e.tensor_scalar(out=wkvb[:, c, :], in0=wtmp2[:, c, :], scalar1=mv[:, 3 + c:4 + c], scalar2=None, op0=ALU.mult),
                     [wtmp2, mv], [wkvb])
            pi_ = k.sb("pi_", [128, 1024], mybir.dt.int32, pa)
            pf = k.sb("pf", [128, 1024], F32, pa)
            kf = k.sb("kf", [128, 1024], F32, pa)
            ki = k.sb("ki", [128, 1024], mybir.dt.int32, pa)
            m1 = k.sb("m1", [128, 1024], F32, pa)
            rc = k.sb("rc", [128, 1024], F32, pa)

            def wrap(r):
                k.op("dve", lambda e: e.tensor_scalar(out=m1[:], in0=r[:], scalar1=float(np.pi), scalar2=-TWO_PI, op0=ALU.is_gt, op1=ALU.mult), [r], [m1])
                k.op("dve", lambda e: e.tensor_tensor(out=r[:], in0=r[:], in1=m1[:], op=ALU.add), [r, m1], [r])
                k.op("dve", lambda e: e.tensor_scalar(out=m1[:], in0=r[:], scalar1=float(-np.pi), scalar2=TWO_PI, op0=ALU.is_lt, op1=ALU.mult), [r], [m1])
                k.op("dve", lambda e: e.tensor_tensor(out=r[:], in0=r[:], in1=m1[:], op=ALU.add), [r, m1], [r])
            for q4 in range(4):
                cs = slice(q4 * 1024, (q4 + 1) * 1024)
                k.dma("sp", pi_[:], pos_d[:, cs], writes=[pi_])
                k.op("dve", lambda e: e.tensor_copy(out=pf[:], in_=pi_[:]), [pi_], [pf])
                k.op("dve", lambda e: e.tensor_scalar(out=pf[:], in0=pf[:], scalar1=mv[:, 9:10], scalar2=None, op0=ALU.mult), [pf, mv], [pf])
                k.op("dve", lambda e: e.tensor_scalar(out=kf[:], in0=pf[:], scalar1=float(1.0 / TWO_PI), scalar2=None, op0=ALU.mult), [pf], [kf])
                k.op("dve", lambda e: e.tensor_copy(out=ki[:], in_=kf[:]), [kf], [ki])
                k.op("dve", lambda e: e.tensor_copy(out=kf[:], in_=ki[:]), [ki], [kf])
                k.op("dve", lambda e: e.scalar_tensor_tensor(out=pf[:], in0=kf[:], scalar=-TWO_PI, in1=pf[:], op0=ALU.mult, op1=ALU.add), [kf, pf], [pf])
                wrap(pf)
                k.op("act", lambda e: e.activation(out=sinT[:, cs], in_=pf[:], func=AF.Sin), [pf], [(sinT, q4)])
                k.op("dve", lambda e: e.tensor_scalar(out=rc[:], in0=pf[:], scalar1=float(np.pi / 2), scalar2=None, op0=ALU.add), [pf], [rc])
                wrap(rc)
                k.op("act", lambda e: e.activation(out=cosT[:, cs], in_=rc[:], func=AF.Sin), [rc], [(cosT, q4)])
            kp = k.sb("kp", [128, S], F32, pa)
            ksw = k.sb("ksw", [128, S], F32, pa)
            k.dma("sp", kp[64:96, :], projT[640:672, :], reads=[projT.tensor], writes=[kp])
            k.dma("sp", ksw[64:80, :], projT[656:672, :], reads=[projT.tensor], writes=[(ksw, 0)])
            k.dma("sp", ksw[80:96, :], projT[640:656, :], reads=[projT.tensor], writes=[(ksw, 1)])
            k.op("dve", lambda e: e.tensor_tensor(out=kp[64:96, :], in0=kp[64:96, :], in1=cosT[64:96, :], op=ALU.mult), [kp, cosT], [kp])
            k.op("dve", lambda e: e.scalar_tensor_tensor(out=ksw[64:96, :], in0=ksw[64:96, :], scalar=mv[64:96, 10:11], in1=sinT[64:96, :], op0=ALU.mult, op1=ALU.mult),
                 [ksw, mv, sinT], [ksw])
            k.op("dve", lambda e: e.tensor_tensor(out=kper[64:96, :], in0=kp[64:96, :], in1=ksw[64:96, :], op=ALU.add), [kp, ksw], [kper])
            k.barrier()
        with contextlib.ExitStack() as pb:
            lat = [k.sb("lat%d" % i, [128, 5, 512], F32, pb) for i in range(2)]
            sq = k.sb("sq", [128, 5, 512], F32, pb)
            rst = [k.sb("rst%d" % i, [128, 512], F32, pb) for i in range(2)]
            pv = projT[0:640, :].rearrange("(c p) s -> p c s", p=128)
            for b in range(NB):
                cs = slice(b * 512, (b + 1) * 512)
                lt = lat[b % 2]
                k.dma("sp", lt[:], pv[:, 0:5, cs], reads=[projT.tensor], writes=[lt])
                k.op("act", lambda e: e.activation(out=sq[:], in_=lt[:], func=AF.Square), [lt], [sq])
                for (c0, c1, n, dst, pp, rs_) in ((0, 3, 384.0, qlatn, psA[0], rst[0]), (3, 5, 256.0, kvlatn, psA[1], rst[1])):
                    for c in range(c0, c1):
                        k.op("pe", lambda e: e.matmul(pp[:], lhsT=ones[:], rhs=sq[:, c, :], start=(c == c0), stop=(c == c1 - 1)), [ones, sq], [pp], inc=(c == c1 - 1))
                    k.op("act", lambda e: e.activation(out=rs_[:], in_=pp[:], func=AF.Sqrt, scale=1.0 / n, bias=EPS), [pp], [rs_])
                    k.op("dve", lambda e: e.reciprocal(out=rs_[:], in_=rs_[:]), [rs_], [rs_])
                    for c in range(c0, c1):
                        k.op("dve", lambda e: e.tensor_tensor(out=dst[:, c - c0, cs], in0=lt[:, c, :], in1=rs_[:], op=ALU.mult), [lt, rs_], [(dst, (c - c0, b))])
            k.barrier()
        with contextlib.ExitStack() as pc:
            qh = [k.sb("qh%d" % i, [128, S], BF16, pc) for i in range(2)]
            kh = [k.sb("kh%d" % i, [128, S], BF16, pc) for i in range(2)]
            vh = [k.sb("vh%d" % i, [128, NT, 65], BF16, pc) for i in range(2)]
            pT = [k.sb("pT%d" % i, [128, 512], BF16, pc) for i in range(3)]
            t1 = [k.sb("t1_%d" % i, [128, 512], F32, pc) for i in range(2)]
            t2 = [k.sb("t2_%d" % i, [128, 512], F32, pc) for i in range(2)]
            rec = k.sb("rec", [128, 8, NT], F32, pc)
            for i in range(2):
                k.op("dve", lambda e: e.memset(vh[i][:, :, 64:65], 1.0), [], [vh[i]])
            pti = 0
            for h in range(heads):
                q_, k_, v_ = qh[h % 2], kh[h % 2], vh[h % 2]
                for b in range(NB):
                    cs = slice(b * 512, (b + 1) * 512)
                    p1, p2_ = psA[0], psA[1]
                    for c in range(3):
                        k.op("pe", lambda e: e.matmul(p1[0:HQ, :], lhsT=wqb[:, c, h * HQ:(h + 1) * HQ], rhs=qlatn[:, c, cs], start=(c == 0), stop=(c == 2)),
                             [wqb, (qlatn, (c, b))], [p1], inc=(c == 2))
                    k.op("act", lambda e: e.copy(out=q_[0:64, cs], in_=p1[0:64, :]), [p1], [(q_, (0, b))])
                    k.op("dve", lambda e: e.tensor_tensor(out=t1[b % 2][64:96, :], in0=p1[64:96, :], in1=cosT[64:96, cs], op=ALU.mult), [p1, cosT], [t1[b % 2]])
                    for c in range(3):
                        k.op("pe", lambda e: e.matmul(p2_[0:HQ, :], lhsT=wqr[:, c, h * HQ:(h + 1) * HQ], rhs=qlatn[:, c, cs], start=(c == 0), stop=(c == 2)),
                             [wqr, (qlatn, (c, b))], [p2_], inc=(c == 2))
                    k.op("dve", lambda e: e.tensor_tensor(out=t2[b % 2][64:96, :], in0=p2_[64:96, :], in1=sinT[64:96, cs], op=ALU.mult), [p2_, sinT], [t2[b % 2]])
                    k.op("dve", lambda e: e.tensor_tensor(out=q_[64:96, cs], in0=t1[b % 2][64:96, :], in1=t2[b % 2][64:96, :], op=ALU.add),
                         [t1[b % 2], t2[b % 2]], [(q_, (1, b))])
                    for c in range(2):
                        k.op("pe", lambda e: e.matmul(p1[0:64, :], lhsT=wkvb[:, c, h * 128:h * 128 + 64], rhs=kvlatn[:, c, cs], start=(c == 0), stop=(c == 1)),
                             [wkvb, (kvlatn, (c, b))], [p1], inc=(c == 1))
                    k.op("act", lambda e: e.copy(out=k_[0:64, cs], in_=p1[0:64, :]), [p1], [(k_, (0, b))])
                    k.op("act", lambda e: e.copy(out=k_[64:96, cs], in_=kper[64:96, cs]), [kper], [(k_, (1, b))])
                for g8 in range(NT // 8):
                    pp = psA[g8 % 2]
                    for tl in range(8):
                        t = g8 * 8 + tl
                        for c in range(2):
                            k.op("pe", lambda e: e.matmul(pp[:, tl * 64:(tl + 1) * 64], lhsT=kvlatn[:, c, t * 128:(t + 1) * 128], rhs=wkvb[:, c, h * 128 + 64:h * 128 + 128],
                                                          start=(c == 0), stop=(c == 1)), [kvlatn, wkvb], [pp], inc=(c == 1 and tl == 7))
                    k.op("act", lambda e: e.copy(out=v_[:, g8 * 8:(g8 + 1) * 8, 0:64], in_=pp[:].rearrange("p (t d) -> p t d", d=64)), [pp], [(v_, g8)])
                steps = [(qb, j) for qb in range(NB) for j in range(4 * qb + 4)]

                def emit_scores(i):
                    qb, j = steps[i]
                    c0 = max(0, j - 4 * qb) * 128
                    ps_ = psS[i % 2]
                    k.op("pe", lambda e: e.matmul(ps_[:, c0:512], lhsT=k_[0:HQ, j * 128:(j + 1) * 128], rhs=q_[0:HQ, qb * 512 + c0:(qb + 1) * 512], start=True, stop=True),
                         [k_, q_], [ps_])
                emit_scores(0)
                for i, (qb, j) in enumerate(steps):
                    if i + 1 < len(steps):
                        emit_scores(i + 1)
                    r = j - 4 * qb
                    c0 = max(0, r) * 128
                    ps_ = psS[i % 2]
                    pt_ = pT[i % 3]
                    k.op("act", lambda e: e.activation(out=pt_[:, c0:512], in_=ps_[:, c0:512], func=AF.Exp), [ps_], [pt_])
                    if r >= 0:
                        k.op("dve", lambda e: e.tensor_tensor(out=pt_[:, c0:c0 + 128], in0=pt_[:, c0:c0 + 128], in1=tri[:], op=ALU.mult), [pt_, tri], [pt_])
                    for s_ in range(c0 // 128, 4):
                        k.op("pe", lambda e: e.matmul(psO[s_][:, 0:65], lhsT=pt_[:, s_ * 128:(s_ + 1) * 128], rhs=v_[:, j, 0:65], start=(j == 0), stop=(j == 4 * qb + s_)),
                             [pt_, v_], [psO[s_]], inc=(s_ == 3))
                    if j == 4 * qb + 3:
                        for s_ in range(4):
                            t = qb * 4 + s_
                            k.op("dve", lambda e: e.reciprocal(out=rec[:, h, t:t + 1], in_=psO[s_][:, 64:65]), [psO[s_]], [(rec, (h, t))])
                            k.op("dve", lambda e: e.tensor_scalar(out=mo[:, t, h * 64:(h + 1) * 64], in0=psO[s_][:, 0:64], scalar1=rec[:, h, t:t + 1], scalar2=None, op0=ALU.mult),
                                 [psO[s_], (rec, (h, t))], [(mo, (t, h))])
            k.barrier()
        with contextlib.ExitStack() as pd_:
            stat = k.sb("stat2", [128, 4, NT], F32, pd_)
            junk = k.sb("junk2", [128, 512], BF16, pd_)
            mn = [k.sb("mn%d" % i, [128, 512], F32, pd_) for i in range(2)]
            stg = [k.sb("stg2_%d" % i, [128, 4, 512], F32, pd_) for i in range(2)]
            for t in range(NT):
                mt = mo[:, t, :]
                ss, sd, rs = stat[:, 0, t:t + 1], stat[:, 1, t:t + 1], stat[:, 2, t:t + 1]
                k.op("act", lambda e: e.activation(out=junk[:], in_=mt, func=AF.Square, accum_out=ss), [mo], [junk, (stat, (t, 0))])
                k.op("act", lambda e: e.activation(out=sd, in_=ss, func=AF.Sqrt, scale=1.0 / 512, bias=EPS), [(stat, (t, 0))], [(stat, (t, 1))])
                k.op("dve", lambda e: e.reciprocal(out=rs, in_=sd), [(stat, (t, 1))], [(stat, (t, 2))])
                m_ = mn[t % 2]
                k.op("dve", lambda e: e.tensor_scalar(out=m_[:], in0=mt, scalar1=rs, scalar2=None, op0=ALU.mult), [mo, (stat, (t, 2))], [m_])
                pp = psA[t % 2].rearrange("p (c s) -> p c s", s=128)
                for c in range(4):
                    k.op("pe", lambda e: e.transpose(out=pp[:, c, :], in_=m_[:, c * 128:(c + 1) * 128], identity=ident[:]), [m_, ident], [psA[t % 2]], inc=(c == 3))
                sg = stg[(t // 4) % 2]
                for c in range(4):
                    k.op("act", lambda e: e.activation(out=sg[:, c, (t % 4) * 128:(t % 4 + 1) * 128], in_=pp[:, c, :], func=AF.Copy, scale=mv[:, 5 + c:6 + c]),
                         [psA[t % 2], mv], [(sg, (c, t % 4))])
                if t % 4 == 3:
                    b = t // 4
                    for c in range(4):
                        k.dma("sp", g.oT[c * 128:(c + 1) * 128, b * 512:(b + 1) * 512], sg[:, c, :], reads=[sg], writes=[(g.oT.tensor, ("m", c, b))])
            k.barrier()


def phase3(g):
    nc, k = g.nc, g.k
    gvd = g.din("gdn_vec", [128, 32])
    cwd = g.din("conv_wt", [64, 24, 4])
    ident, ones, projT = g.ident, g.ones, g.projT
    heads = g.cfg.get("gdn_heads", 8)
    NCH = S // 64
    BIG = 30000.0
    P = 64
    with contextlib.ExitStack() as p3:
        gv = k.sb("gv", [128, 32], F32, p3)
        k.dma("sp", gv[:], gvd, writes=[gv])
        cw = k.sb("cw", [P, 24, 4], F32, p3)
        k.dma("sp", cw[:], cwd, writes=[cw])
        trif = k.sb("trif", [128, 128], F32, p3)
        k.dma("sp", trif[:], g.consts[2], writes=[trif])
        bigm1 = k.sb("bigm1", [P, P], F32, p3)
        negb2 = k.sb("negb2", [P, P], F32, p3)
        k.op("dve", lambda e: e.tensor_scalar(out=bigm1[:], in0=trif[0:P, 0:P], scalar1=BIG, scalar2=None, op0=ALU.mult), [trif], [bigm1])
        k.op("dve", lambda e: e.tensor_scalar(out=negb2[:], in0=trif[0:P, 0:P], scalar1=-1.0, scalar2=BIG, op0=ALU.add, op1=ALU.mult), [trif], [negb2])
        beta = k.sb("beta", [P, NCH, 8], F32, p3)
        gc = k.sb("gc", [P, NCH, 8], F32, p3)
        ngc = k.sb("ngc", [P, NCH, 8], F32, p3)
        bgam = k.sb("bgam", [P, NCH, 8], F32, p3)
        ktl = k.sb("ktl", [P, NCH, 8], F32, p3)
        cdb = k.sb("cdb", [P, NCH, 8], F32, p3)
        ps = [k.ps("pgd%d" % i, [128, 512], F32, p3) for i in range(8)]
        psq = ps
        with contextlib.ExitStack() as pa:
            ab = k.sb("ab", [16, S], F32, pa)
            k.dma("sp", ab[:], projT[2720:2736, :], reads=[projT.tensor], writes=[ab])
            abt = k.sb("abt", [P, NCH, 16], F32, pa)
            for q4 in range(2):
                pp = ps[q4]
                for j in range(32):
                    n = q4 * 32 + j
                    k.op("pe", lambda e: e.transpose(out=pp[0:P, j * 16:(j + 1) * 16], in_=ab[0:16, n * 64:(n + 1) * 64], identity=ident[0:16, 0:16]),
                         [ab, ident], [pp], inc=(j == 31))
                k.op("act", lambda e: e.copy(out=abt[:, q4 * 32:(q4 + 1) * 32, :], in_=pp[0:P, :].rearrange("p (n f) -> p n f", f=16)), [pp], [(abt, q4)])
            xa = k.sb("xa_", [P, NCH, 8], F32, pa)
            ax = k.sb("ax_", [P, NCH, 8], F32, pa)
            gg = k.sb("gg_", [P, NCH, 8], F32, pa)
            ea = k.sb("ea_", [P, 8], F32, pa)
            gt = k.sb("gt_", [P, NCH, 8], F32, pa)
            k.op("act", lambda e: e.activation(out=beta[:], in_=abt[:, :, 8:16], func=AF.Sigmoid), [abt], [beta])
            for n in range(NCH):
                k.op("dve", lambda e: e.tensor_tensor(out=xa[:, n, :], in0=abt[:, n, 0:8], in1=gv[0:P, 8:16], op=ALU.add), [abt, gv], [(xa, n)])
            k.op("act", lambda e: e.activation(out=ax[:], in_=xa[:], func=AF.Abs), [xa], [ax])
            k.op("act", lambda e: e.activation(out=ax[:], in_=ax[:], func=AF.Exp, scale=-1.0), [ax], [ax])
            k.op("act", lambda e: e.activation(out=ax[:], in_=ax[:], func=AF.Ln, bias=1.0), [ax], [ax])
            k.op("dve", lambda e: e.scalar_tensor_tensor(out=xa[:], in0=xa[:], scalar=0.0, in1=ax[:], op0=ALU.max, op1=ALU.add), [xa, ax], [xa])
            k.op("act", lambda e: e.activation(out=ea[:], in_=gv[0:P, 0:8], func=AF.Exp), [gv], [ea])
            for n in range(NCH):
                k.op("dve", lambda e: e.tensor_tensor(out=gg[:, n, :], in0=xa[:, n, :], in1=ea[:], op=ALU.mult), [xa, ea], [(gg, n)])
            k.op("dve", lambda e: e.tensor_scalar(out=gg[:], in0=gg[:], scalar1=-1.0, scalar2=None, op0=ALU.mult), [gg], [gg])
            ggf = gg[:].rearrange("p n h -> p (n h)")
            pc_, pt_ = ps[2], ps[3]
            k.op("pe", lambda e: e.matmul(pc_[0:P, :], lhsT=trif[0:P, 0:P], rhs=ggf, start=True, stop=True), [trif, gg], [pc_])
            k.op("pe", lambda e: e.matmul(pt_[0:P, :], lhsT=ones[0:P, 0:P], rhs=ggf, start=True, stop=True), [ones, gg], [pt_])
            f2 = lambda t_: t_[:].rearrange("p n h -> p (n h)")
            k.op("act", lambda e: e.copy(out=f2(gc), in_=pc_[0:P, :]), [pc_], [gc])
            k.op("act", lambda e: e.copy(out=f2(ax), in_=pt_[0:P, :]), [pt_], [ax])
            k.op("act", lambda e: e.activation(out=f2(cdb), in_=f2(ax), func=AF.Exp), [ax], [cdb])
            k.op("dve", lambda e: e.tensor_tensor(out=f2(gt), in0=f2(ax), in1=f2(gc), op=ALU.subtract), [ax, gc], [gt])
            k.op("act", lambda e: e.activation(out=f2(ktl), in_=f2(gt), func=AF.Exp), [gt], [ktl])
            k.op("dve", lambda e: e.tensor_scalar(out=f2(ngc), in0=f2(gc), scalar1=-1.0, scalar2=None, op0=ALU.mult), [gc], [ngc])
            k.op("act", lambda e: e.activation(out=f2(gt), in_=f2(gc), func=AF.Exp), [gc], [gt])
            k.op("dve", lambda e: e.tensor_tensor(out=f2(bgam), in0=f2(gt), in1=f2(beta), op=ALU.mult), [gt, beta], [bgam])
            k.barrier()
        with contextlib.ExitStack() as pb:
            xp = k.sb("xp", [P, S + 4], F32, pb)
            cv = k.sb("cv", [P, S], F32, pb)
            cvo = k.sb("cvo", [P, S], F32, pb)
            u_all = k.sb("u_all", [P, NCH, 64], F32, pb)
            wT_all = k.sb("wT_all", [P, S], BF16, pb)
            qg_all = k.sb("qg_all", [P, S], BF16, pb)
            qk_all = k.sb("qk_all", [P, NCH, 64], BF16, pb)
            kt_all = k.sb("kt_all", [P, NCH, 64], BF16, pb)
            kTb2 = [k.sb("kTb%d" % i, [P, S], BF16, pb) for i in range(2)]
            qTb2 = [k.sb("qTb%d" % i, [P, S], BF16, pb) for i in range(2)]
            vTb2 = [k.sb("vTb%d" % i, [P, S], BF16, pb) for i in range(2)]
            identb = k.sb("identb", [P, P], BF16, pb)
            k.op("act", lambda e: e.copy(out=identb[:], in_=ident[0:P, 0:P]), [ident], [identb])
            o_all = u_all
            Xs = [k.sb("Xs%d" % i, [P, P], F32, pb) for i in range(2)]
            Gb = [k.sb("Gb%d" % i, [P, P], F32, pb) for i in range(2)]
            D1 = [k.sb("D1_%d" % i, [P, P], F32, pb) for i in range(2)]
            DT = [k.sb("DT_%d" % i, [P, P], F32, pb) for i in range(2)]
            Pm = [[k.sb("Pm%d_%d" % (i, j), [P, P], BF16, pb) for j in range(2)] for i in range(16)]
            Qm = [[k.sb("Qm%d_%d" % (i, j), [P, P], BF16, pb) for j in range(2)] for i in range(16)]
            Wm = [[k.sb("Wm%d_%d" % (i, j), [P, P], BF16, pb) for j in range(2)] for i in range(16)]
            kbg = [k.sb("kbg%d" % i, [P, P], BF16, pb) for i in range(2)]
            bv = [k.sb("bv%d" % i, [P, P], BF16, pb) for i in range(2)]
            Sst = [k.sb("Sst%d" % i, [P, P], F32, pb) for i in range(2)]
            Sbf = [k.sb("Sbf%d" % i, [P, P], BF16, pb) for i in range(2)]
            vn = [k.sb("vn%d" % i, [P, P], BF16, pb) for i in range(2)]
            rsd = k.sb("rsd", [P, 4, NCH], F32, pb)
            k.op("dve", lambda e: e.memset(xp[:, 0:4], 0.0), [], [(xp, "pad")])
            epsb = k.sb("epsb", [P, 1], F32, pb)
            k.op("dve", lambda e: e.memset(epsb[:], EPS), [], [epsb])
            psi = 0

            psf = 0

            psd = 0

            def nps(kind):
                nonlocal psi, psd
                if kind == "act":
                    psi += 1
                    return psq[psi % 4]
                psd += 1
                return psq[4 + psd % 4]

            def npsf(kind="act"):
                return nps(kind)
            def conv_gen(hh):
                for ti, (dstb, row0) in enumerate(((qTb2[hh % 2], 672), (kTb2[hh % 2], 1184), (vTb2[hh % 2], 1696))):
                    k.dma("sp", xp[:, 4:S + 4], projT[row0 + hh * 64:row0 + (hh + 1) * 64, :], reads=[projT.tensor], writes=[(xp, "x")])
                    ci = ti * 8 + hh
                    k.op("dve", lambda e: e.tensor_scalar(out=cv[:], in0=xp[:, 1:S + 1], scalar1=cw[:, ci, 0:1], scalar2=None, op0=ALU.mult), [xp, cw], [cv])
                    yield
                    for j in range(1, 4):
                        k.op("dve", lambda e: e.scalar_tensor_tensor(out=cv[:], in0=xp[:, 1 + j:S + 1 + j], scalar=cw[:, ci, j:j + 1], in1=cv[:], op0=ALU.mult, op1=ALU.add),
                             [xp, cw, cv], [cv])
                        yield
                    if ti == 2:
                        k.op("act", lambda e: e.activation(out=dstb[:], in_=cv[:], func=AF.Silu), [cv], [dstb])
                        yield
                        continue
                    k.op("act", lambda e: e.activation(out=cvo[:], in_=cv[:], func=AF.Silu), [cv], [cvo])
                    k.op("dve", lambda e: e.tensor_tensor(out=cv[:], in0=cvo[:], in1=cvo[:], op=ALU.mult), [cvo], [cv])
                    yield
                    for b in range(NB):
                        cs = slice(b * 512, (b + 1) * 512)
                        pp = npsf()
                        k.op("pe", lambda e: e.matmul(pp[0:P, :], lhsT=ones[0:P, 0:P], rhs=cv[:, cs], start=True, stop=True), [ones, cv], [pp])
                        k.op("act", lambda e: e.activation(out=cv[:, cs], in_=pp[0:P, :], func=AF.Ln, bias=epsb[0:P, 0:1]), [pp, epsb], [(cv, b)])
                        k.op("act", lambda e: e.activation(out=cv[:, cs], in_=cv[:, cs], func=AF.Exp, scale=-0.5), [(cv, b)], [(cv, b)])
                        sc_ = 0.125 if ti == 0 else 1.0
                        k.op("dve", lambda e: e.scalar_tensor_tensor(out=dstb[:, cs], in0=cvo[:, cs], scalar=sc_, in1=cv[:, cs], op0=ALU.mult, op1=ALU.mult),
                             [cvo, (cv, b)], [(dstb, b)])
                        yield

            ei = 0
            for h in range(heads):
                if h == 0:
                    for _ in conv_gen(0):
                        pass
                kTb, qTb, vTb = kTb2[h % 2], qTb2[h % 2], vTb2[h % 2]
                nxt_conv = conv_gen(h + 1) if h + 1 < heads else iter(())
                G = 8
                St = Sst[0]
                k.op("dve", lambda e: e.memset(St[:], 0.0), [], [St])
                k.op("dve", lambda e: e.memset(Sbf[0][:], 0.0), [], [Sbf[0]])

                def scan_step(n, St):
                    cs = slice(n * 64, (n + 1) * 64)
                    p1_, p2_, p3_ = nps("dve"), nps("act"), nps("dve")
                    v_ = vn[n % 2]
                    Sb = Sbf[n % 2]
                    k.op("pe", lambda e: e.matmul(p1_[0:P, 0:P], lhsT=wT_all[:, cs], rhs=Sb[:], start=True, stop=True), [(wT_all, n), Sb], [p1_])
                    k.op("dve", lambda e: e.tensor_tensor(out=v_[:], in0=u_all[:, n, :], in1=p1_[0:P, 0:P], op=ALU.subtract), [(u_all, n), p1_], [v_])
                    k.op("pe", lambda e: e.matmul(p2_[0:P, 0:P], lhsT=qg_all[:, cs], rhs=Sb[:], start=True, stop=False), [(qg_all, n), Sb], [p2_], inc=False)
                    k.op("pe", lambda e: e.matmul(p2_[0:P, 0:P], lhsT=qk_all[:, n, :], rhs=v_[:], start=False, stop=True), [(qk_all, n), v_], [p2_])
                    k.op("pe", lambda e: e.matmul(p3_[0:P, 0:P], lhsT=kt_all[:, n, :], rhs=v_[:], start=True, stop=True), [(kt_all, n), v_], [p3_])
                    Sn = Sst[(n + 1) % 2]
                    k.op("dve", lambda e: e.scalar_tensor_tensor(out=Sn[:], in0=St[:], scalar=cdb[:, n, h:h + 1], in1=p3_[0:P, 0:P], op0=ALU.mult, op1=ALU.add),
                         [St, cdb, p3_], [Sn])
                    k.op("act", lambda e: e.copy(out=Sbf[(n + 1) % 2][:], in_=Sn[:]), [Sn], [Sbf[(n + 1) % 2]])
                    k.op("act", lambda e: e.copy(out=o_all[:, n, :], in_=p2_[0:P, 0:P]), [p2_], [(o_all, n)])
                    return Sn

                def stage_a(n, sl):
                    cs = slice(n * 64, (n + 1) * 64)
                    kTn, qTn = kTb[:, cs], qTb[:, cs]
                    gcn, ngcn, btn = gc[:, n, h:h + 1], ngc[:, n, h:h + 1], beta[:, n, h:h + 1]
                    X = Xs[n % 2]
                    k.op("dve", lambda e: e.tensor_scalar(out=X[:], in0=ident[0:P, 0:P], scalar1=gcn, scalar2=None, op0=ALU.mult), [ident, gc], [X])
                    pR, pR2, pR3 = nps("act"), nps("act"), nps("act")
                    k.op("pe", lambda e: e.matmul(pR[0:P, 0:P], lhsT=ones[0:P, 0:P], rhs=X[:], start=True, stop=False), [ones, X], [pR], inc=False)
                    k.op("pe", lambda e: e.matmul(pR[0:P, 0:P], lhsT=ident[0:P, 0:P], rhs=bigm1[:], start=False, stop=True), [ident, bigm1], [pR])
                    k.op("pe", lambda e: e.matmul(pR2[0:P, 0:P], lhsT=ones[0:P, 0:P], rhs=X[:], start=True, stop=False), [ones, X], [pR2], inc=False)
                    k.op("pe", lambda e: e.matmul(pR2[0:P, 0:P], lhsT=ident[0:P, 0:P], rhs=negb2[:], start=False, stop=True), [ident, negb2], [pR2])
                    k.op("pe", lambda e: e.matmul(pR3[0:P, 0:P], lhsT=ones[0:P, 0:P], rhs=X[:], start=True, stop=True), [ones, X], [pR3])
                    d1, dt_, gb = D1[n % 2], DT[n % 2], Gb[n % 2]
                    k.op("act", lambda e: e.activation(out=d1[:], in_=pR[0:P, 0:P], func=AF.Exp, scale=-1.0, bias=gcn), [pR, gc], [d1])
                    k.op("act", lambda e: e.activation(out=dt_[:], in_=pR2[0:P, 0:P], func=AF.Exp, scale=1.0, bias=ngcn), [pR2, ngc], [dt_])
                    k.op("act", lambda e: e.activation(out=gb[:], in_=pR3[0:P, 0:P], func=AF.Exp), [pR3], [gb])
                    pK, pQ = nps("dve"), nps("dve")
                    k.op("pe", lambda e: e.matmul(pK[0:P, 0:P], lhsT=kTn, rhs=kTn, start=True, stop=True), [kTb], [pK])
                    k.op("pe", lambda e: e.matmul(pQ[0:P, 0:P], lhsT=kTn, rhs=qTn, start=True, stop=True), [kTb, qTb], [pQ])
                    A, Q0, W = Pm[sl][0], Qm[sl][0], Wm[sl][0]
                    k.op("dve", lambda e: e.scalar_tensor_tensor(out=A[:], in0=pK[0:P, 0:P], scalar=btn, in1=d1[:], op0=ALU.mult, op1=ALU.mult), [pK, beta, d1], [A])
                    k.op("dve", lambda e: e.tensor_tensor(out=qk_all[:, n, :], in0=pQ[0:P, 0:P], in1=dt_[:], op=ALU.mult), [pQ, dt_], [(qk_all, n)])
                    k.op("dve", lambda e: e.tensor_tensor(out=qg_all[:, cs], in0=qTn, in1=gb[:], op=ALU.mult), [qTb, gb], [(qg_all, n)])
                    pT_ = nps("act")
                    k.op("pe", lambda e: e.matmul(pT_[0:P, 0:P], lhsT=A[:], rhs=identb[:], start=True, stop=True), [A, identb], [pT_])
                    k.op("act", lambda e: e.copy(out=Q0[:], in_=pT_[0:P, 0:P]), [pT_], [Q0])
                    k.op("dve", lambda e: e.tensor_tensor(out=W[:], in0=ident[0:P, 0:P], in1=Q0[:], op=ALU.subtract), [ident, Q0], [W])

                def stage_b(sl, lv):
                    Pk, Qk, W = Pm[sl][lv % 2], Qm[sl][lv % 2], Wm[sl][lv % 2]
                    Pn, Qn, Wn = Pm[sl][(lv + 1) % 2], Qm[sl][(lv + 1) % 2], Wm[sl][(lv + 1) % 2]
                    pP = nps("act")
                    k.op("pe", lambda e: e.matmul(pP[0:P, 0:P], lhsT=Qk[:], rhs=Pk[:], start=True, stop=True), [Qk, Pk], [pP])
                    if lv < 4:
                        pQ2 = nps("dve")
                        k.op("pe", lambda e: e.matmul(pQ2[0:P, 0:P], lhsT=Pk[:], rhs=Qk[:], start=True, stop=True), [Pk, Qk], [pQ2])
                    k.op("act", lambda e: e.copy(out=Pn[:], in_=pP[0:P, 0:P]), [pP], [Pn])
                    if lv < 4:
                        k.op("dve", lambda e: e.tensor_copy(out=Qn[:], in_=pQ2[0:P, 0:P]), [pQ2], [Qn])
                    pW = nps("dve")
                    k.op("pe", lambda e: e.matmul(pW[0:P, 0:P], lhsT=Pn[:], rhs=W[:], start=True, stop=True), [Pn, W], [pW])
                    k.op("dve", lambda e: e.tensor_tensor(out=Wn[:], in0=pW[0:P, 0:P], in1=W[:], op=ALU.add), [pW, W], [Wn])

                def stage_c(n, sl):
                    cs = slice(n * 64, (n + 1) * 64)
                    kTn, vTn = kTb[:, cs], vTb[:, cs]
                    btn = beta[:, n, h:h + 1]
                    W = Wm[sl][1]
                    pk_, pv_ = nps("dve"), nps("act")
                    k.op("pe", lambda e: e.matmul(pk_[0:P, 0:P], lhsT=kTn, rhs=identb[:], start=True, stop=True), [kTb, identb], [pk_])
                    k.op("pe", lambda e: e.matmul(pv_[0:P, 0:P], lhsT=vTn, rhs=identb[:], start=True, stop=True), [vTb, identb], [pv_])
                    kb_, bv_ = kbg[n % 2], bv[n % 2]
                    k.op("dve", lambda e: e.tensor_scalar(out=kb_[:], in0=pk_[0:P, 0:P], scalar1=bgam[:, n, h:h + 1], scalar2=None, op0=ALU.mult), [pk_, bgam], [kb_])
                    k.op("dve", lambda e: e.tensor_scalar(out=kt_all[:, n, :], in0=pk_[0:P, 0:P], scalar1=ktl[:, n, h:h + 1], scalar2=None, op0=ALU.mult), [pk_, ktl], [(kt_all, n)])
                    k.op("act", lambda e: e.activation(out=bv_[:], in_=pv_[0:P, 0:P], func=AF.Copy, scale=btn), [pv_, beta], [bv_])
                    pu, pw = nps("act"), nps("dve")
                    k.op("pe", lambda e: e.matmul(pu[0:P, 0:P], lhsT=W[:], rhs=bv_[:], start=True, stop=True), [W, bv_], [pu])
                    k.op("pe", lambda e: e.matmul(pw[0:P, 0:P], lhsT=kb_[:], rhs=W[:], start=True, stop=True), [kb_, W], [pw])
                    k.op("act", lambda e: e.copy(out=u_all[:, n, :], in_=pu[0:P, 0:P]), [pu], [(u_all, n)])
                    k.op("dve", lambda e: e.tensor_copy(out=wT_all[:, cs], in_=pw[0:P, 0:P]), [pw], [(wT_all, n)])

                ngrp = NCH // G
                for gi in range(ngrp + 1):
                    for _ in range(7):
                        next(nxt_conv, None)
                    base = (gi % 2) * G
                    if gi < ngrp:
                        for c in range(G):
                            stage_a(gi * G + c, base + c)
                    for lv in range(5):
                        if gi < ngrp:
                            for c in range(G):
                                stage_b(base + c, lv)
                        if gi >= 1:
                            for sj in range(lv * G // 5, (lv + 1) * G // 5):
                                St = scan_step((gi - 1) * G + sj, St)
                    if gi < ngrp:
                        for c in range(G):
                            stage_c(gi * G + c, base + c)
                k.op("dve", lambda e: e.tensor_tensor(out=cv[:].rearrange("p (n d) -> p n d", d=64), in0=o_all[:], in1=o_all[:], op=ALU.mult), [o_all], [cv])
                k.op("dve", lambda e: e.tensor_reduce(out=rsd[:, 0, :], in_=cv[:].rearrange("p (n d) -> p n d", d=64), axis=AX.X, op=ALU.add), [cv], [(rsd, 0)])
                k.op("act", lambda e: e.activation(out=rsd[:, 1, :], in_=rsd[:, 0, :], func=AF.Sqrt, scale=1.0 / 64, bias=EPS), [(rsd, 0)], [(rsd, 1)])
                k.op("dve", lambda e: e.reciprocal(out=rsd[:, 2, :], in_=rsd[:, 1, :]), [(rsd, 1)], [(rsd, 2)])
                k.dma("sp", xp[:, 4:S + 4], projT[2208 + h * 64:2208 + (h + 1) * 64, :], reads=[projT.tensor], writes=[(xp, "x")])
                k.op("act", lambda e: e.activation(out=xp[:, 4:S + 4], in_=xp[:, 4:S + 4], func=AF.Silu), [(xp, "x")], [(xp, "x")])
                for b in range(NB):
                    pp = npsf("dve")
                    for j in range(8):
                        n = b * 8 + j
                        k.op("dve", lambda e: e.tensor_scalar(out=o_all[:, n, :], in0=o_all[:, n, :], scalar1=rsd[:, 2, n:n + 1], scalar2=None, op0=ALU.mult),
                             [(o_all, n), (rsd, 2)], [(o_all, n)])
                        k.op("pe", lambda e: e.transpose(out=pp[0:P, j * 64:(j + 1) * 64], in_=o_all[:, n, :], identity=ident[0:P, 0:P]), [(o_all, n), ident], [pp], inc=(j == 7))
                    cs = slice(b * 512, (b + 1) * 512)
                    k.op("dve", lambda e: e.scalar_tensor_tensor(out=cv[:, cs], in0=pp[0:P, :], scalar=gv[0:P, 16:17], in1=xp[:, 4 + b * 512:4 + (b + 1) * 512], op0=ALU.mult, op1=ALU.mult),
                         [pp, gv, (xp, "x")], [(cv, b)])
                k.dma("sp", g.oT[512 + h * 64:512 + (h + 1) * 64, :], cv[:], reads=[cv], writes=[(g.oT.tensor, ("g", h))])
            k.barrier()


def phase4(g):
    nc, k = g.nc, g.k
    w_out = g.din("w_out", [D, D])
    router_w = g.din("router_w", [D, NE])
    router_b = g.din("router_b", [1, NE])
    x, ident, ones = g.x, g.ident, g.ones
    with contextlib.ExitStack() as p4:
        woutb = k.sb("woutb", [128, 8, D], BF16, p4)
        wov = w_out.rearrange("(c p) n -> p c n", p=128)
        k.dma("pool", woutb[:], wov, writes=[woutb])
        rw = k.sb("rw", [128, 8, NE], F32, p4)
        k.dma("sp", rw[:], router_w.rearrange("(c p) n -> p c n", p=128), writes=[rw])
        rb = k.sb("rb", [1, NE], F32, p4)
        k.dma("sp", rb[:], router_b, writes=[rb])
        bcs = {}
        for nm, slot in (("gt1", 2), ("sh2", 3), ("s2", 4)):
            bcs[nm] = k.sb(nm + "_bc", [128, D], F32, p4)
            k.dma("sp", bcs[nm][:], g.modscr[slot], reads=[(g.modscr.tensor, slot)], writes=[bcs[nm]])
        oTb = [k.sb("oTb%d" % i, [128, 8, 512], BF16, p4) for i in range(2)]
        xts = [k.sb("xq%d" % i, [128, D], F32, p4) for i in range(2)]
        x1s = [k.sb("x1s%d" % i, [128, D], F32, p4) for i in range(2)]
        hts = [k.sb("h2t%d" % i, [128, D], F32, p4) for i in range(2)]
        junk = k.sb("junk4", [128, D], BF16, p4)
        stat = k.sb("stat4", [128, 4, NT], F32, p4)
        h2Tf = [k.sb("h2Tf%d" % i, [128, 8, 128], F32, p4) for i in range(2)]
        h2Tb = [k.sb("h2Tb%d" % i, [128, 8, 512], BF16, p4) for i in range(2)]
        lg = k.sb("lg", [128, NT, NE], F32, p4)
        m8 = k.sb("m8", [128, NT, 8], F32, p4)
        rt = k.sb("rt", [128, 4, NT], F32, p4)
        msk = [k.sb("msk%d" % i, [128, NE], F32, p4) for i in range(2)]
        ex = [k.sb("ex%d" % i, [128, NE], F32, p4) for i in range(2)]
        pmx = [k.ps("pmx%d" % i, [128, 512], F32, p4) for i in range(4)]
        ptr = [k.ps("ptr4_%d" % i, [128, 4, 128], F32, p4) for i in range(2)]
        plg = [k.ps("plg%d" % i, [128, NE], F32, p4) for i in range(2)]
        oTv = g.oT.rearrange("(c p) s -> p c s", p=128)
        h2Tv = g.h2T.rearrange("(c p) s -> p c s", p=128)
        for blk in range(NB):
            ob = oTb[blk % 2]
            k.dma("pool", ob[:], oTv[:, :, blk * 512:(blk + 1) * 512], reads=[g.oT.tensor], writes=[ob])
            for tt in range(4):
                t = blk * 4 + tt
                xt = xts[t % 2]
                x1t = x1s[t % 2]
                ht = hts[t % 2]
                k.dma("sp", xt[:], x[t * 128:(t + 1) * 128, :], writes=[xt])
                for half in range(2):
                    pp = pmx[(2 * t + half) % 4]
                    for kc in range(8):
                        k.op("pe", lambda e: e.matmul(pp[:], lhsT=ob[:, kc, tt * 128:(tt + 1) * 128], rhs=woutb[:, kc, half * 512:(half + 1) * 512],
                                                      start=(kc == 0), stop=(kc == 7)), [ob, woutb], [pp], inc=(kc == 7))
                    hs = slice(half * 512, (half + 1) * 512)
                    k.op("dve", lambda e: e.tensor_tensor(out=x1t[:, hs], in0=pp[:], in1=bcs["gt1"][:, hs], op=ALU.mult), [pp, bcs["gt1"]], [(x1t, half)])
                    k.op("dve", lambda e: e.tensor_tensor(out=x1t[:, hs], in0=x1t[:, hs], in1=xt[:, hs], op=ALU.add), [(x1t, half), xt], [(x1t, half)])
                k.dma("sp", g.x1[t * 128:(t + 1) * 128, :], x1t[:], reads=[x1t], writes=[(g.x1.tensor, t)])
                if g.cfg.get("p4_level", 9) < 0.2:
                    continue
                rs, rskey = rms_tile(g, x1t, stat, t, junk, 1.0 / D)
                if g.cfg.get("p4_level", 9) < 0.27:
                    continue
                k.op("dve", lambda e: e.scalar_tensor_tensor(out=ht[:], in0=x1t[:], scalar=rs, in1=bcs["s2"][:], op0=ALU.mult, op1=ALU.mult),
                     [x1t, rskey, bcs["s2"]], [ht])
                if g.cfg.get("p4_level", 9) < 0.29:
                    continue
                k.op("dve", lambda e: e.tensor_tensor(out=ht[:], in0=ht[:], in1=bcs["sh2"][:], op=ALU.add), [ht, bcs["sh2"]], [ht])
                if g.cfg.get("p4_level", 9) < 0.5:
                    continue
                hf = h2Tf[t % 2]
                hb = h2Tb[blk % 2]
                for half in range(2):
                    pt = ptr[half]
                    for j in range(4):
                        kc = half * 4 + j
                        k.op("pe", lambda e: e.transpose(out=pt[:, j, :], in_=ht[:, kc * 128:(kc + 1) * 128], identity=ident[:]),
                             [ht, ident], [pt], inc=(j == 3))
                    k.op("act", lambda e: e.copy(out=hf[:, half * 4:half * 4 + 4, :], in_=pt[:]), [pt], [(hf, half)])
                    k.op("dve", lambda e: e.tensor_copy(out=hb[:, half * 4:half * 4 + 4, tt * 128:(tt + 1) * 128], in_=hf[:, half * 4:half * 4 + 4, :]),
                         [(hf, half)], [(hb, (half, tt))])
                if tt == 3 and g.cfg.get("p4_level", 9) >= 0.7:
                    for kc in range(8):
                        k.dma("sp", g.h2T[kc * 128:(kc + 1) * 128, blk * 512:(blk + 1) * 512], hb[:, kc, :], reads=[hb], writes=[(g.h2T.tensor, (blk, kc))])
                if g.cfg.get("p4_level", 9) < 2:
                    continue
                pl = plg[t % 2]
                for kc in range(8):
                    k.op("pe", lambda e: e.matmul(pl[:], lhsT=hf[:, kc, :], rhs=rw[:, kc, :], start=(kc == 0), stop=False), [hf, rw], [pl], inc=False)
                k.op("pe", lambda e: e.matmul(pl[:], lhsT=ones[0:1, :], rhs=rb[0:1, :], start=False, stop=True), [ones, rb], [pl])
                lgt = lg[:, t, :]
                k.op("act", lambda e: e.copy(out=lgt, in_=pl[:]), [pl], [(lg, t)])
                k.op("dve", lambda e: e.max(out=m8[:, t, :], in_=lgt), [(lg, t)], [(m8, t)])
                mk = msk[t % 2]
                et = ex[t % 2]
                k.op("dve", lambda e: e.tensor_scalar(out=mk[:], in0=lgt, scalar1=m8[:, t, 3:4], scalar2=None, op0=ALU.is_ge), [(lg, t), (m8, t)], [mk])
                k.op("dve", lambda e: e.tensor_scalar(out=rt[:, 0, t:t + 1], in0=m8[:, t, 0:1], scalar1=-1.0, scalar2=None, op0=ALU.mult), [(m8, t)], [(rt, (t, 0))])
                k.op("act", lambda e: e.activation(out=et[:], in_=lgt, func=AF.Exp, bias=rt[:, 0, t:t + 1], scale=1.0), [(lg, t), (rt, (t, 0))], [et])
                k.op("dve", lambda e: e.tensor_tensor(out=et[:], in0=et[:], in1=mk[:], op=ALU.mult), [et, mk], [et])
                k.op("dve", lambda e: e.reduce_sum(out=rt[:, 1, t:t + 1], in_=et[:], axis=AX.X), [et], [(rt, (t, 1))])
                k.op("dve", lambda e: e.reciprocal(out=rt[:, 2, t:t + 1], in_=rt[:, 1, t:t + 1]), [(rt, (t, 1))], [(rt, (t, 2))])
                k.op("dve", lambda e: e.tensor_scalar(out=g.wr[:, t, :], in0=et[:], scalar1=rt[:, 2, t:t + 1], scalar2=None, op0=ALU.mult),
                     [et, (rt, (t, 2))], [(g.wr, t)])
        k.barrier()


def phase5(g):
    nc, k = g.nc, g.k
    cfg = g.cfg
    n_exp = cfg.get("n_exp", NE)
    g.cast_some(12)
    bgu_t = g.din("bgu_t", [128, NE, 16])
    b_dn = g.din("b_down", [NE, D])
    ident = g.ident
    PT = 1024
    npass = S // PT
    with contextlib.ExitStack() as p5:
        gt2 = k.sb("gt2_bc", [128, D], F32, p5)
        fg = k.sb("fg_bc", [128, D], F32, p5)
        k.dma("sp", gt2[:], g.modscr[5], reads=[(g.modscr.tensor, 5)], writes=[gt2])
        k.dma("sp", fg[:], g.gvec[2], writes=[fg])
        bgu = k.sb("bgu", [128, NE, 16], F32, p5)
        k.dma("sp", bgu[:], bgu_t, writes=[bgu])
        bgu1 = k.sb("bgu1", [128, NE, 16], F32, p5)
        k.op("dve", lambda e: e.tensor_scalar(out=bgu1[:], in0=bgu[:], scalar1=1.0, scalar2=None, op0=ALU.add), [bgu], [bgu1])
        bdn = k.sb("bdn", [NE, D], F32, p5)
        k.dma("sp", bdn[:], b_dn, writes=[bdn])
        h2s = k.sb("h2s", [128, 8, PT], BF16, p5)
        yacc = k.sb("yacc", [128, PT // 128, D], F32, p5)
        wgu = [k.sb("wgu%d" % i, [128, 8, 2 * D], BF16, p5) for i in range(2)]
        wdn = [k.sb("wdn%d" % i, [128, 8, D], BF16, p5) for i in range(2)]
        actT = [k.sb("actT%d" % i, [128, 8, 512], BF16, p5) for i in range(2)]
        gS = [k.sb("gS%d" % i, [128, 512], F32, p5) for i in range(2)]
        sS = [k.sb("sS%d" % i, [128, 512], F32, p5) for i in range(2)]
        uS = [k.sb("uS%d" % i, [128, 512], F32, p5) for i in range(2)]
        wrT = k.sb("wrT", [NE, PT], F32, p5)
        xa = [k.sb("xa%d" % i, [128, D], F32, p5) for i in range(2)]
        junk = k.sb("junk5", [128, D], BF16, p5)
        stat = k.sb("stat5", [128, 4, NT], F32, p5)
        pg = [k.ps("pg%d" % i, [128, 512], F32, p5) for i in range(2)]
        pu = [k.ps("pu%d" % i, [128, 512], F32, p5) for i in range(2)]
        pd = [k.ps("pd%d" % i, [128, 512], F32, p5) for i in range(4)]
        h2Tv = g.h2T.rearrange("(c p) s -> p c s", p=128)
        wguv = g.wgu_bf.rearrange("e (c p) n -> e p c n", p=128)
        wdnv = g.wdn_bf.rearrange("e (c p) n -> e p c n", p=128)

        n_tot = npass * n_exp

        def load_wg(ei):
            if ei >= n_tot:
                return
            wg = wgu[ei % 2]
            e_ = ei % n_exp
            for kc in range(0, 8, 4):
                k.dma("sp", wg[:, kc:kc + 4, :], wguv[e_, :, kc:kc + 4, :], reads=[(g.wgu_bf.tensor, e_)], writes=[(wg, kc + j) for j in range(4)])

        def load_wd(ei):
            if ei >= n_tot:
                return
            wd = wdn[ei % 2]
            e_ = ei % n_exp
            k.dma("sp", wd[:], wdnv[e_], reads=[(g.wdn_bf.tensor, e_)], writes=[(wd, kc) for kc in range(8)])

        fci = 0
        pdi = 0
        NBK = PT // 512

        def emit_gu(ei, bk):
            nonlocal fci
            e_ = ei % n_exp
            wg = wgu[ei % 2]
            aT = actT[(ei * NBK + bk) % 2]
            rhs_cols = slice(bk * 512, (bk + 1) * 512)
            for fc in range(8):
                pgt = pg[fci % 2]
                put = pu[fci % 2]
                g_ = gS[fci % 2]
                s_ = sS[fci % 2]
                u_ = uS[fci % 2]
                fci += 1
                for kc in range(8):
                    k.op("pe", lambda e: e.matmul(pgt[:], lhsT=wg[:, kc, fc * 128:(fc + 1) * 128], rhs=h2s[:, kc, rhs_cols],
                                                  start=(kc == 0), stop=(kc == 7)), [(wg, kc), h2s], [pgt], inc=(kc == 7))
                for kc in range(8):
                    k.op("pe", lambda e: e.matmul(put[:], lhsT=wg[:, kc, D + fc * 128:D + (fc + 1) * 128], rhs=h2s[:, kc, rhs_cols],
                                                  start=(kc == 0), stop=(kc == 7)), [(wg, kc), h2s], [put], inc=(kc == 7))
                k.op("dve", lambda e: e.tensor_scalar(out=g_[:], in0=pgt[:], scalar1=bgu[:, e_, fc:fc + 1], scalar2=7.0, op0=ALU.add, op1=ALU.min),
                     [pgt, bgu], [g_])
                k.op("act", lambda e: e.activation(out=s_[:], in_=g_[:], func=AF.Sigmoid, scale=1.702), [g_], [s_])
                k.op("dve", lambda e: e.tensor_scalar(out=u_[:], in0=put[:], scalar1=bgu1[:, e_, 8 + fc:9 + fc], scalar2=8.0, op0=ALU.add, op1=ALU.min),
                     [put, bgu1], [u_])
                k.op("dve", lambda e: e.tensor_tensor(out=g_[:], in0=g_[:], in1=s_[:], op=ALU.mult), [g_, s_], [g_])
                k.op("dve", lambda e: e.scalar_tensor_tensor(out=aT[:, fc, :], in0=u_[:], scalar=-6.0, in1=g_[:], op0=ALU.max, op1=ALU.mult), [g_, u_], [(aT, fc)])
            if bk == NBK - 1:
                load_wg(ei + 2)

        def emit_down(ei, bk, t0):
            nonlocal pdi
            e_ = ei % n_exp
            wd = wdn[ei % 2]
            aT = actT[(ei * NBK + bk) % 2]
            for tt in range(4):
                tl = bk * 4 + tt
                for half in range(2):
                    pp = pd[pdi % 4]
                    pdi += 1
                    for fc in range(8):
                        k.op("pe", lambda e: e.matmul(pp[:], lhsT=aT[:, fc, tt * 128:(tt + 1) * 128], rhs=wd[:, fc, half * 512:(half + 1) * 512],
                                                      start=(fc == 0), stop=(fc == 7)), [(aT, fc), (wd, fc)], [pp], inc=(fc == 7))
                    ya = yacc[:, tl, half * 512:(half + 1) * 512]
                    k.op("dve", lambda e: e.scalar_tensor_tensor(out=ya, in0=pp[:], scalar=g.wr[:, t0 + tl, e_:e_ + 1], in1=ya, op0=ALU.mult, op1=ALU.add),
                         [pp, (g.wr, t0 + tl), (yacc, (tl, half))], [(yacc, (tl, half))])
            if bk == NBK - 1:
                load_wd(ei + 2)

        for i in range(2):
            load_wg(i)
            load_wd(i)
        for ps_ in range(npass):
            t0 = ps_ * (PT // 128)
            k.dma("sp", h2s[:], h2Tv[:, :, ps_ * PT:(ps_ + 1) * PT], reads=[g.h2T.tensor], writes=[h2s])
            for tl in range(PT // 128):
                pt = pd[pdi % 4]
                pdi += 1
                k.op("pe", lambda e: e.transpose(out=pt[0:NE, 0:128], in_=g.wr[:, t0 + tl, :], identity=ident[:]), [(g.wr, t0 + tl), ident], [pt])
                k.op("act", lambda e: e.copy(out=wrT[:, tl * 128:(tl + 1) * 128], in_=pt[0:NE, 0:128]), [pt], [(wrT, tl)])
                for half in range(2):
                    pp = pd[pdi % 4]
                    pdi += 1
                    k.op("pe", lambda e: e.matmul(pp[:], lhsT=wrT[:, tl * 128:(tl + 1) * 128], rhs=bdn[:, half * 512:(half + 1) * 512], start=True, stop=True),
                         [(wrT, tl), bdn], [pp])
                    k.op("dve", lambda e: e.tensor_copy(out=yacc[:, tl, half * 512:(half + 1) * 512], in_=pp[:]), [pp], [(yacc, (tl, half))])
            units = [(ps_ * n_exp + e_, bk) for e_ in range(n_exp) for bk in range(NBK)]
            emit_gu(*units[0])
            for i, (ei_, bk_) in enumerate(units):
                if i + 1 < len(units):
                    emit_gu(*units[i + 1])
                emit_down(ei_, bk_, t0)
            for tl in range(PT // 128):
                t = t0 + tl
                xt = xa[t % 2]
                k.dma("sp", xt[:], g.x1[t * 128:(t + 1) * 128, :], reads=[(g.x1.tensor, t)], writes=[xt])
                ya = yacc[:, tl, :]
                k.op("dve", lambda e: e.tensor_tensor(out=ya, in0=ya, in1=gt2[:], op=ALU.mult), [(yacc, (tl, 0)), (yacc, (tl, 1)), gt2], [(yacc, (tl, 0)), (yacc, (tl, 1))])
                k.op("dve", lambda e: e.tensor_tensor(out=xt[:], in0=xt[:], in1=ya, op=ALU.add), [xt, (yacc, (tl, 0)), (yacc, (tl, 1))], [xt])
                rs, rskey = rms_tile(g, xt, stat, t, junk, 1.0 / D)
                k.op("dve", lambda e: e.scalar_tensor_tensor(out=xt[:], in0=xt[:], scalar=rs, in1=fg[:], op0=ALU.mult, op1=ALU.mult),
                     [xt, rskey, fg], [xt])
                k.dma("sp", g.out[t * 128:(t + 1) * 128, :], xt[:], reads=[xt], writes=[(g.out.tensor, t)])
        k.barrier()


def _consts():
    c = np.zeros((4, 128, 128), np.float32)
    c[0] = np.eye(128, dtype=np.float32)
    c[1] = 1.0
    c[2] = np.triu(np.ones((128, 128), np.float32))
    return c


def make_in_map(inp, b, names=None):
    f = lambda a: np.ascontiguousarray(np.asarray(a, dtype=np.float32))
    m = {}
    m["x"] = f(inp["x"][b])
    m["c_t"] = f(np.asarray(inp["c"][b]).reshape(8, 128).T)
    m["ada_w"] = f(inp["ada_w"][0])
    m["ada_b"] = f(inp["ada_b"][0]).reshape(1, -1)
    gv = np.stack([np.broadcast_to(np.asarray(inp[n]).reshape(-1), (128, D)) for n in ("norm1_g", "norm2_g", "final_g")])
    m["gvec"] = f(gv)
    m["w_in"] = f(inp["w_in"][0])
    m["consts"] = _consts()
    m["w_out"] = f(inp["w_out"][0])
    m["router_w"] = f(inp["router_w"][0])
    m["router_b"] = f(inp["router_b"][0]).reshape(1, -1)
    m["w_gate_up"] = f(inp["w_gate_up"][0])
    m["w_down"] = f(inp["w_down"][0])
    m["bgu_t"] = f(np.asarray(inp["b_gate_up"][0]).reshape(NE, 16, 128).transpose(2, 0, 1))
    m["b_down"] = f(inp["b_down"][0])
    m["w_q_b"] = f(inp["w_q_b"][0])
    m["w_kv_b"] = f(inp["w_kv_b"][0])
    mv = np.zeros((128, 16), np.float32)
    mv[:, 0:3] = np.asarray(inp["q_norm_g"][0]).reshape(3, 128).T
    mv[:, 3:5] = np.asarray(inp["kv_norm_g"][0]).reshape(2, 128).T
    mv[:, 5:9] = np.asarray(inp["mla_out_g"][0]).reshape(4, 128).T
    pidx = np.arange(128)
    mv[:, 9] = (np.float32(10000.0) ** (-(pidx % 16).astype(np.float32) / np.float32(16))).astype(np.float32)
    mv[:, 10] = np.where((pidx % 32) < 16, -1.0, 1.0)
    m["mla_vec"] = mv
    m["pos_bc"] = np.ascontiguousarray(np.broadcast_to(np.asarray(inp["positions"][b]).astype(np.int32).reshape(1, S), (128, S)))
    gvv = np.zeros((128, 32), np.float32)
    gvv[:, 0:8] = np.asarray(inp["A_log"][0]).reshape(1, 8)
    gvv[:, 8:16] = np.asarray(inp["dt_bias"][0]).reshape(1, 8)
    gvv[0:64, 16] = np.asarray(inp["gdn_norm_g"][0]).reshape(64)
    gvv[64:128, 16] = np.asarray(inp["gdn_norm_g"][0]).reshape(64)
    m["gdn_vec"] = gvv
    m["conv_wt"] = f(np.asarray(inp["conv_w"][0]).reshape(4, 24, 64).transpose(2, 1, 0))
    if names is not None:
        m = {n: v for n, v in m.items() if n in names}
    return m


def input_names(nc):
    return None


_CACHE = {}


def kernel(**inputs):
    if "nc" not in _CACHE:
        _CACHE["nc"] = build({})
    nc, kb = _CACHE["nc"]
    in_maps = [make_in_map(inputs, b) for b in range(8)]
    res = run_bass_kernel_spmd(nc, in_maps, core_ids=list(range(8)))
    out = np.stack([np.asarray(r["out"], dtype=np.float32) for r in res.results], axis=0)
    return out
```

```python
import contextlib
import numpy as np
import concourse.bass as bass
import concourse.mybir as mybir
from concourse.bass_utils import run_bass_kernel_spmd

F32 = mybir.dt.float32
BF16 = mybir.dt.bfloat16
AF = mybir.ActivationFunctionType
ALU = mybir.AluOpType
AX = mybir.AxisListType

S = 4096
D = 1024
NT = S // 128
NB = S // 512
IN_DIM = 2736
EPS = 1e-6
NE = 32


class KB:
    def __init__(self, nc):
        self.nc = nc
        self.es = contextlib.ExitStack()
        self.E = {}
        for name, eng in [("pe", nc.tensor), ("act", nc.scalar), ("dve", nc.vector),
                          ("pool", nc.gpsimd), ("sp", nc.sync)]:
            sem = self.es.enter_context(nc.semaphore("sem_" + name))
            self.E[name] = dict(eng=eng, sem=sem, cnt=0, seen={})
        self.sems = {n: e["sem"] for n, e in self.E.items()}
        self.ndma = 24
        self.dval = []
        for i in range(self.ndma):
            self.sems[("d", i)] = self.es.enter_context(nc.semaphore("sem_d%d" % i))
            self.dval.append(0)
        self.dptr = 0
        self.nw = 8
        for i in range(self.nw):
            self.sems[("d", self.ndma + i)] = self.es.enter_context(nc.semaphore("sem_w%d" % i))
            self.dval.append(0)
        self.wptr = 0
        self.nsw = 40
        self.swptr = 0
        for i in range(self.nsw):
            self.sems[("s", i)] = self.es.enter_context(nc.semaphore("sem_s%d" % i))
        self.state = {}
        self.n_ins = 0
        for sk, sem in self.sems.items():
            nc.gpsimd.sem_clear(sem)
        nc.all_engine_barrier()

    def sb(self, name, shape, dt, es=None):
        return (es or self.es).enter_context(self.nc.sbuf_tensor(name, list(shape), dt))

    def ps(self, name, shape, dt, es=None):
        return (es or self.es).enter_context(self.nc.psum_tensor(name, list(shape), dt))

    def _new(self):
        return {"w": None, "r": {}}

    def _sts(self, key):
        if not isinstance(key, tuple):
            key = (key, None)
        t, sub = key
        d = self.state.setdefault(id(t), {})
        if sub is None:
            if None not in d:
                d[None] = self._new()
            return list(d.values())
        if sub not in d:
            d[sub] = self._new()
        res = [d[sub]]
        if None in d:
            res.append(d[None])
        return res

    def _collect(self, ename, reads, writes):
        need = {}

        def add(sk, val):
            if sk == "pe" and ename == "pe":
                return
            if need.get(sk, 0) < val:
                need[sk] = val
        for k in reads:
            for st in self._sts(k):
                if st["w"] is not None:
                    add(*st["w"])
        for k in writes:
            for st in self._sts(k):
                if st["w"] is not None:
                    add(*st["w"])
                for sk, v in st["r"].items():
                    add(sk, v)
        return need

    def _wait(self, ename, need):
        e = self.E[ename]
        for sk, val in need.items():
            if e["seen"].get(sk, 0) < val:
                e["eng"].wait_ge(self.sems[sk], val)
                e["seen"][sk] = val

    def _update(self, reads, writes, sk, val):
        for k in reads:
            if not isinstance(k, tuple):
                k = (k, None)
            sts = self._sts(k)
            if k[1] is None:
                for st in sts:
                    st["r"][sk] = max(st["r"].get(sk, 0), val)
            else:
                sts[0]["r"][sk] = max(sts[0]["r"].get(sk, 0), val)
        for k in writes:
            if not isinstance(k, tuple):
                k = (k, None)
            sts = self._sts(k)
            if k[1] is None:
                for st in sts:
                    st["w"] = (sk, val)
                    st["r"] = {}
            else:
                sts[0]["w"] = (sk, val)
                sts[0]["r"] = {}

    def op(self, ename, fn, reads=(), writes=(), inc=True):
        e = self.E[ename]
        self._wait(ename, self._collect(ename, reads, writes))
        ins = fn(e["eng"])
        val = e["cnt"] + 1
        if inc:
            ins.then_inc(e["sem"], 1)
            e["cnt"] = val
        self._update(reads, writes, ename, val)
        self.n_ins += 1
        return ins

    def dma(self, ename, out, in_, reads=(), writes=(), wpool=False, **kw):
        e = self.E[ename]
        if ename == "pool":
            sk = ("s", self.swptr)
            self.swptr += 1
            assert self.swptr <= self.nsw, "out of single-use SW-DMA semaphores"
            self._wait(ename, self._collect(ename, reads, writes))
            ins = e["eng"].dma_start(out=out, in_=in_, **kw)
            ins.then_inc(self.sems[sk], 16)
            self._update(reads, writes, sk, 16)
            self.n_ins += 1
            return sk, 16
        if wpool:
            i = self.ndma + self.wptr
            self.wptr = (self.wptr + 1) % self.nw
        else:
            i = self.dptr
            self.dptr = (self.dptr + 1) % self.ndma
        sk = ("d", i)
        need = self._collect(ename, reads, writes)
        if self.dval[i] > 0:
            need[sk] = max(need.get(sk, 0), self.dval[i])
        self._wait(ename, need)
        ins = e["eng"].dma_start(out=out, in_=in_, **kw)
        self.dval[i] += 16
        ins.then_inc(self.sems[sk], 16)
        self._update(reads, writes, sk, self.dval[i])
        self.n_ins += 1
        return sk, self.dval[i]

    def barrier(self):
        for ename, e in self.E.items():
            for sk in self.sems:
                if isinstance(sk, tuple) and sk[0] == "s":
                    val = 16 if sk[1] < self.swptr else 0
                else:
                    val = self.E[sk]["cnt"] if sk in self.E else self.dval[sk[1]]
                if val > 0 and e["seen"].get(sk, 0) < val and sk != ename:
                    e["eng"].wait_ge(self.sems[sk], val)
                    e["seen"][sk] = val

    def finish(self, ename, keys):
        need = self._collect(ename, keys, ())
        self._wait(ename, need)


class Ctx:
    pass


class TV(tuple):
    def __new__(cls, tile, c):
        return super().__new__(cls, (tile, ("c", c)))

    def __getitem__(self, idx):
        if isinstance(idx, int):
            return tuple.__getitem__(self, idx)
        tile = tuple.__getitem__(self, 0)
        c = tuple.__getitem__(self, 1)[1]
        return tile[:, c * 64:(c + 1) * 64]


class PV(tuple):
    def __new__(cls, tile, q):
        return super().__new__(cls, (tile, ("q", q)))

    def __getitem__(self, idx):
        if isinstance(idx, int):
            return tuple.__getitem__(self, idx)
        tile = tuple.__getitem__(self, 0)
        q = tuple.__getitem__(self, 1)[1]
        p_, c_ = idx
        return tile[p_, q * 128 + c_.start:q * 128 + c_.stop]


def build(cfg):
    nc = bass.Bass("TRN2", target_bir_lowering=False)
    k = KB(nc)
    dbg = cfg.get("dbg", ())
    phases = cfg.get("phases", ("p0", "p1", "p2", "p3", "p4", "p5"))
    g = Ctx()
    g.nc, g.k, g.dbg, g.cfg = nc, k, dbg, cfg

    def din(name, shape, dt=F32):
        return nc.dram_tensor(name, list(shape), dt, kind="ExternalInput").ap()

    def dout(name, shape, dt=F32):
        return nc.dram_tensor(name, list(shape), dt, kind="ExternalOutput").ap()

    def dscr(name, shape, dt=F32):
        if name in dbg:
            return dout(name, shape, dt)
        if name in cfg.get("as_input", ()):
            return din(name, shape, dt)
        return nc.dram_tensor(name, list(shape), dt, kind="Internal").ap()
    g.din, g.dout, g.dscr = din, dout, dscr

    g.x = din("x", [S, D])
    g.consts = din("consts", [4, 128, 128])
    g.gvec = din("gvec", [3, 128, D])
    g.modscr = dscr("modscr", [6, 128, D])
    g.projT = dscr("projT", [IN_DIM, S])
    g.oT = dscr("oT", [D, S])
    g.x1 = dscr("x1", [S, D])
    g.h2T = dscr("h2T", [D, S], BF16)
    g.out = dout("out", [S, D])
    if "p5" in phases:
        g.w_gu = din("w_gate_up", [NE, D, 2 * D])
        g.w_dn = din("w_down", [NE, D, D])
        g.wgu_bf = dscr("wgu_bf", [NE, D, 2 * D], BF16)
        g.wdn_bf = dscr("wdn_bf", [NE, D, D], BF16)
    g.cast_next = 0 if "p5" in phases else 12

    def cast_some(n):
        for _ in range(n):
            i = g.cast_next
            if i >= 12:
                return
            g.cast_next += 1
            order = [("g", 0), ("g", 1), ("d", 0), ("g", 2), ("g", 3), ("d", 1), ("g", 4), ("g", 5), ("d", 2), ("g", 6), ("g", 7), ("d", 3)]
            kind, j = order[i]
            if kind == "g":
                k.dma("pool", g.wgu_bf[4 * j:4 * j + 4], g.w_gu[4 * j:4 * j + 4], writes=[(g.wgu_bf.tensor, e_) for e_ in range(4 * j, 4 * j + 4)])
            else:
                k.dma("pool", g.wdn_bf[8 * j:8 * j + 8], g.w_dn[8 * j:8 * j + 8], writes=[(g.wdn_bf.tensor, e_) for e_ in range(8 * j, 8 * j + 8)])
    g.cast_some = cast_some
    g.outputs = [g.out.tensor]
    for n in dbg:
        pass

    with k.es:
        g.ident = k.sb("ident", [128, 128], F32)
        g.ones = k.sb("ones", [128, 128], F32)
        k.dma("sp", g.ident[:], g.consts[0], writes=[g.ident])
        k.dma("sp", g.ones[:], g.consts[1], writes=[g.ones])
        g.wr = k.sb("wr", [128, NT, NE], F32)
        if "p0" in phases:
            phase0(g)
        if "p1" in phases:
            phase1(g)
        if "p2" in phases:
            phase2(g)
        if "p3" in phases:
            phase3(g)
        if "p4" in phases:
            phase4(g)
        if "p5" in phases:
            phase5(g)
        fin = list(g.outputs)
        for n in dbg:
            fin.append(getattr(g, n).tensor)
        k.finish("sp", fin)
        k.barrier()
        nc.all_engine_barrier()
    return nc, k


def phase0(g):
    nc, k = g.nc, g.k
    ada_w = g.din("ada_w", [D, 6 * D])
    ada_b = g.din("ada_b", [1, 6 * D])
    c_t = g.din("c_t", [128, 8])
    ones = g.ones
    with contextlib.ExitStack() as p0:
        mod_bc = k.sb("mod_bc", [128, 6 * D], F32, p0)
        ct = k.sb("ct", [128, 8], F32, p0)
        cact = k.sb("cact", [128, 8], F32, p0)
        cbc = k.sb("cbc", [128, 8, 128], F32, p0)
        adab = k.sb("adab", [1, 6 * D], F32, p0)
        g1 = k.sb("g1", [128, D], F32, p0)
        g2 = k.sb("g2", [128, D], F32, p0)
        awb = [k.sb("awb%d" % i, [128, 8, 512], F32, p0) for i in range(2)]
        pm = [k.ps("pm%d" % i, [128, 512], F32, p0) for i in range(2)]
        k.dma("sp", ct[:], c_t, writes=[ct])
        k.dma("sp", adab[:], ada_b, writes=[adab])
        k.dma("sp", g1[:], g.gvec[0], writes=[g1])
        k.dma("sp", g2[:], g.gvec[1], writes=[g2])
        k.op("act", lambda e: e.activation(out=cact[:], in_=ct[:], func=AF.Silu), [ct], [cact])
        for kc in range(8):
            k.op("dve", lambda e: e.tensor_scalar(out=cbc[:, kc, :], in0=ones[:], scalar1=cact[:, kc:kc + 1],
                                                  scalar2=None, op0=ALU.mult), [ones, cact], [(cbc, kc)])
        aw_v = ada_w.rearrange("(c p) n -> p c n", p=128)
        for blk in range(12):
            wb = awb[blk % 2]
            k.dma("sp", wb[:], aw_v[:, :, blk * 512:(blk + 1) * 512], writes=[wb])
            pp = pm[blk % 2]
            for kc in range(8):
                k.op("pe", lambda e: e.matmul(pp[:], lhsT=cbc[:, kc, :], rhs=wb[:, kc, :], start=(kc == 0), stop=False),
                     [(cbc, kc), wb], [pp], inc=False)
            k.op("pe", lambda e: e.matmul(pp[:], lhsT=ones[0:1, :], rhs=adab[0:1, blk * 512:(blk + 1) * 512], start=False, stop=True),
                 [ones, adab], [pp])
            k.op("act", lambda e: e.copy(out=mod_bc[:, blk * 512:(blk + 1) * 512], in_=pp[:]), [pp], [(mod_bc, blk)])
        k.op("dve", lambda e: e.scalar_tensor_tensor(out=mod_bc[:, D:2 * D], in0=mod_bc[:, D:2 * D], scalar=1.0, in1=g1[:],
                                                     op0=ALU.add, op1=ALU.mult), [mod_bc, g1], [mod_bc])
        k.op("dve", lambda e: e.scalar_tensor_tensor(out=mod_bc[:, 4 * D:5 * D], in0=mod_bc[:, 4 * D:5 * D], scalar=1.0, in1=g2[:],
                                                     op0=ALU.add, op1=ALU.mult), [mod_bc, g2], [mod_bc])
        for j in range(6):
            k.dma("sp", g.modscr[j], mod_bc[:, j * D:(j + 1) * D], reads=[mod_bc], writes=[(g.modscr.tensor, j)])
        k.barrier()


def rms_tile(g, xt, stat, col, junk, scale):
    k = g.k
    ss = stat[:, 0, col:col + 1]
    sd = stat[:, 1, col:col + 1]
    rs = stat[:, 2, col:col + 1]
    k.op("act", lambda e: e.activation(out=junk[:], in_=xt[:], func=AF.Square, accum_out=ss), [xt], [junk, (stat, (col, 0))])
    k.op("act", lambda e: e.activation(out=sd, in_=ss, func=AF.Sqrt, scale=scale, bias=EPS), [(stat, (col, 0))], [(stat, (col, 1))])
    k.op("dve", lambda e: e.reciprocal(out=rs, in_=sd), [(stat, (col, 1))], [(stat, (col, 2))])
    return rs, (stat, (col, 2))


def phase1(g):
    nc, k = g.nc, g.k
    w_in = g.din("w_in", [D, IN_DIM])
    x, ident, projT = g.x, g.ident, g.projT
    with contextlib.ExitStack() as p1:
        winb = k.sb("winb", [128, 8, IN_DIM], BF16, p1)
        wv = w_in.rearrange("(c p) n -> p c n", p=128)
        for hh in range(2):
            k.dma("pool", winb[:, :, hh * 1368:(hh + 1) * 1368], wv[:, :, hh * 1368:(hh + 1) * 1368], writes=[(winb, hh)])
        g.cast_some(12)
        s1_bc = k.sb("s1_bc", [128, D], F32, p1)
        sh1_bc = k.sb("sh1_bc", [128, D], F32, p1)
        k.dma("sp", sh1_bc[:], g.modscr[0], reads=[(g.modscr.tensor, 0)], writes=[sh1_bc])
        k.dma("sp", s1_bc[:], g.modscr[1], reads=[(g.modscr.tensor, 1)], writes=[s1_bc])
        xts = [k.sb("xt%d" % i, [128, D], F32, p1) for i in range(3)]
        junk = k.sb("junk", [128, D], BF16, p1)
        hts = [k.sb("ht%d" % i, [128, D], F32, p1) for i in range(2)]
        stat = k.sb("stat", [128, 4, NT], F32, p1)
        h1T = [k.sb("h1T%d" % i, [128, 8, 512], BF16, p1) for i in range(2)]
        stg = [k.sb("stg%d" % i, [128, 512], F32, p1) for i in range(4)]
        ptr = [k.ps("ptr%d" % i, [128, 4, 128], F32, p1) for i in range(4)]
        pmm = [k.ps("pmm%d" % i, [128, 512], F32, p1) for i in range(4)]
        nchunks = (IN_DIM + 127) // 128
        mmi = 0
        for t in range(NT):
            xt = xts[t % 3]
            ht = hts[t % 2]
            k.dma("sp", xt[:], x[t * 128:(t + 1) * 128, :], writes=[xt])
            rs, rskey = rms_tile(g, xt, stat, t, junk, 1.0 / D)
            k.op("dve", lambda e: e.scalar_tensor_tensor(out=ht[:], in0=xt[:], scalar=rs, in1=s1_bc[:], op0=ALU.mult, op1=ALU.mult),
                 [xt, rskey, s1_bc], [ht])
            k.op("dve", lambda e: e.tensor_tensor(out=ht[:], in0=ht[:], in1=sh1_bc[:], op=ALU.add), [ht, sh1_bc], [ht])
            hb = h1T[(t // 4) % 2]
            tt = t % 4
            for half in range(2):
                pt = ptr[(2 * t + half) % 4]
                for j in range(4):
                    kc = half * 4 + j
                    k.op("pe", lambda e: e.transpose(out=pt[:, j, :], in_=ht[:, kc * 128:(kc + 1) * 128], identity=ident[:]),
                         [ht, ident], [pt], inc=(j == 3))
                if half == 0:
                    k.op("act", lambda e: e.copy(out=hb[:, half * 4:half * 4 + 4, tt * 128:(tt + 1) * 128], in_=pt[:]), [pt], [(hb, (half, tt))])
                else:
                    k.op("dve", lambda e: e.tensor_copy(out=hb[:, half * 4:half * 4 + 4, tt * 128:(tt + 1) * 128], in_=pt[:]), [pt], [(hb, (half, tt))])
            if tt == 3:
                blk = t // 4
                for ci in range(nchunks):
                    c0 = ci * 128
                    cw = min(128, IN_DIM - c0)
                    pp = pmm[mmi % 4]
                    sg = stg[mmi % 4]
                    for kc in range(8):
                        k.op("pe", lambda e: e.matmul(pp[0:cw, :], lhsT=winb[:, kc, c0:c0 + cw], rhs=hb[:, kc, :],
                                                      start=(kc == 0), stop=(kc == 7)), [winb, hb], [pp], inc=(kc == 7))
                    if mmi % 2 == 0:
                        k.op("act", lambda e: e.copy(out=sg[0:cw, :], in_=pp[0:cw, :]), [pp], [sg])
                    else:
                        k.op("dve", lambda e: e.tensor_copy(out=sg[0:cw, :], in_=pp[0:cw, :]), [pp], [sg])
                    k.dma("sp", projT[c0:c0 + cw, blk * 512:(blk + 1) * 512], sg[0:cw, :], reads=[sg], writes=[(projT.tensor, (ci, blk))])
                    mmi += 1
        k.barrier()


def phase2(g):
    nc, k = g.nc, g.k
    HQ = 96
    wq_d = g.din("w_q_b", [384, 768])
    wkv_d = g.din("w_kv_b", [256, 1024])
    mvec = g.din("mla_vec", [128, 16])
    pos_d = g.din("pos_bc", [128, S], mybir.dt.int32)
    tri_d = g.consts[2]
    ident, ones, projT = g.ident, g.ones, g.projT
    heads = g.cfg.get("mla_heads", 8)
    TWO_PI = 2.0 * np.pi
    with contextlib.ExitStack() as p2:
        mv = k.sb("mv", [128, 16], F32, p2)
        k.dma("sp", mv[:], mvec, writes=[mv])
        tri = k.sb("tri", [128, 128], BF16, p2)
        trf = k.sb("trf", [128, 128], F32, p2)
        k.dma("sp", trf[:], tri_d, writes=[trf])
        k.op("dve", lambda e: e.tensor_copy(out=tri[:], in_=trf[:]), [trf], [tri])
        wqb = k.sb("wqb", [128, 3, 768], BF16, p2)
        wqr = k.sb("wqr", [128, 3, 768], BF16, p2)
        wkvb = k.sb("wkvb", [128, 2, 1024], BF16, p2)
        qlatn = k.sb("qlatn", [128, 3, S], BF16, p2)
        kvlatn = k.sb("kvlatn", [128, 2, S], BF16, p2)
        cosT = k.sb("cosT", [128, S], F32, p2)
        sinT = k.sb("sinT", [128, S], F32, p2)
        kper = k.sb("kper", [128, S], BF16, p2)
        mo = k.sb("mo", [128, NT, 512], BF16, p2)
        psA = [k.ps("psA%d" % i, [128, 512], F32, p2) for i in range(2)]
        psS = [k.ps("psS%d" % i, [128, 512], F32, p2) for i in range(2)]
        psO = [k.ps("psO%d" % i, [128, 512], F32, p2) for i in range(4)]
        with contextlib.ExitStack() as pa:
            wtmp = k.sb("wtmp", [128, 3, 1024], F32, pa)
            k.dma("sp", wtmp[:, :, 0:768], wq_d.rearrange("(c p) n -> p c n", p=128), writes=[wtmp])
            for c in range(3):
                k.op("dve", lambda e: e.tensor_scalar(out=wtmp[:, c, 0:768], in0=wtmp[:, c, 0:768], scalar1=mv[:, c:c + 1], scalar2=float(HQ ** -0.5),
                                                      op0=ALU.mult, op1=ALU.mult), [wtmp, mv], [wtmp])
            k.op("act", lambda e: e.copy(out=wqb[:], in_=wtmp[:, :, 0:768]), [wtmp], [wqb])
            k.op("dve", lambda e: e.memset(wqr[:], 0.0), [], [wqr])
            w4 = wtmp[:, :, 0:768].rearrange("p c (h d) -> p c h d", d=HQ)
            r4 = wqr[:].rearrange("p c (h d) -> p c h d", d=HQ)
            for c in range(3):
                k.op("dve", lambda e: e.tensor_scalar(out=r4[:, c, :, 64:80], in0=w4[:, c, :, 80:96], scalar1=-1.0, scalar2=None, op0=ALU.mult), [wtmp, wqr], [wqr])
                k.op("dve", lambda e: e.tensor_copy(out=r4[:, c, :, 80:96], in_=w4[:, c, :, 64:80]), [wtmp, wqr], [wqr])
            wtmp2 = k.sb("wtmp2", [128, 2, 1024], F32, pa)
            k.dma("sp", wtmp2[:], wkv_d.rearrange("(c p) n -> p c n", p=128), writes=[wtmp2])
            for c in range(2):
                k.op("dve", lambda e: e.tensor_scalar(out=wkvb[:, c, :], in0=wtmp2[:, c, :], scalar1=mv[:, 3 + c:4 + c], scalar2=None, op0=ALU.mult),
                     [wtmp2, mv], [wkvb])
            pi_ = k.sb("pi_", [128, 1024], mybir.dt.int32, pa)
            pf = k.sb("pf", [128, 1024], F32, pa)
            kf = k.sb("kf", [128, 1024], F32, pa)
            ki = k.sb("ki", [128, 1024], mybir.dt.int32, pa)
            m1 = k.sb("m1", [128, 1024], F32, pa)
            rc = k.sb("rc", [128, 1024], F32, pa)

            def wrap(r):
                k.op("dve", lambda e: e.tensor_scalar(out=m1[:], in0=r[:], scalar1=float(np.pi), scalar2=-TWO_PI, op0=ALU.is_gt, op1=ALU.mult), [r], [m1])
                k.op("dve", lambda e: e.tensor_tensor(out=r[:], in0=r[:], in1=m1[:], op=ALU.add), [r, m1], [r])
                k.op("dve", lambda e: e.tensor_scalar(out=m1[:], in0=r[:], scalar1=float(-np.pi), scalar2=TWO_PI, op0=ALU.is_lt, op1=ALU.mult), [r], [m1])
                k.op("dve", lambda e: e.tensor_tensor(out=r[:], in0=r[:], in1=m1[:], op=ALU.add), [r, m1], [r])
            for q4 in range(4):
                cs = slice(q4 * 1024, (q4 + 1) * 1024)
                k.dma("sp", pi_[:], pos_d[:, cs], writes=[pi_])
                k.op("dve", lambda e: e.tensor_copy(out=pf[:], in_=pi_[:]), [pi_], [pf])
                k.op("dve", lambda e: e.tensor_scalar(out=pf[:], in0=pf[:], scalar1=mv[:, 9:10], scalar2=None, op0=ALU.mult), [pf, mv], [pf])
                k.op("dve", lambda e: e.tensor_scalar(out=kf[:], in0=pf[:], scalar1=float(1.0 / TWO_PI), scalar2=None, op0=ALU.mult), [pf], [kf])
                k.op("dve", lambda e: e.tensor_copy(out=ki[:], in_=kf[:]), [kf], [ki])
                k.op("dve", lambda e: e.tensor_copy(out=kf[:], in_=ki[:]), [ki], [kf])
                k.op("dve", lambda e: e.scalar_tensor_tensor(out=pf[:], in0=kf[:], scalar=-TWO_PI, in1=pf[:], op0=ALU.mult, op1=ALU.add), [kf, pf], [pf])
                wrap(pf)
                k.op("act", lambda e: e.activation(out=sinT[:, cs], in_=pf[:], func=AF.Sin), [pf], [(sinT, q4)])
                k.op("dve", lambda e: e.tensor_scalar(out=rc[:], in0=pf[:], scalar1=float(np.pi / 2), scalar2=None, op0=ALU.add), [pf], [rc])
                wrap(rc)
                k.op("act", lambda e: e.activation(out=cosT[:, cs], in_=rc[:], func=AF.Sin), [rc], [(cosT, q4)])
            kp = k.sb("kp", [128, S], F32, pa)
            ksw = k.sb("ksw", [128, S], F32, pa)
            k.dma("sp", kp[64:96, :], projT[640:672, :], reads=[projT.tensor], writes=[kp])
            k.dma("sp", ksw[64:80, :], projT[656:672, :], reads=[projT.tensor], writes=[(ksw, 0)])
            k.dma("sp", ksw[80:96, :], projT[640:656, :], reads=[projT.tensor], writes=[(ksw, 1)])
            k.op("dve", lambda e: e.tensor_tensor(out=kp[64:96, :], in0=kp[64:96, :], in1=cosT[64:96, :], op=ALU.mult), [kp, cosT], [kp])
            k.op("dve", lambda e: e.scalar_tensor_tensor(out=ksw[64:96, :], in0=ksw[64:96, :], scalar=mv[64:96, 10:11], in1=sinT[64:96, :], op0=ALU.mult, op1=ALU.mult),
                 [ksw, mv, sinT], [ksw])
            k.op("dve", lambda e: e.tensor_tensor(out=kper[64:96, :], in0=kp[64:96, :], in1=ksw[64:96, :], op=ALU.add), [kp, ksw], [kper])
            k.barrier()
        with contextlib.ExitStack() as pb:
            lat = [k.sb("lat%d" % i, [128, 5, 512], F32, pb) for i in range(2)]
            sq = k.sb("sq", [128, 5, 512], F32, pb)
            rst = [k.sb("rst%d" % i, [128, 512], F32, pb) for i in range(2)]
            pv = projT[0:640, :].rearrange("(c p) s -> p c s", p=128)
            for b in range(NB):
                cs = slice(b * 512, (b + 1) * 512)
                lt = lat[b % 2]
                k.dma("sp", lt[:], pv[:, 0:5, cs], reads=[projT.tensor], writes=[lt])
                k.op("act", lambda e: e.activation(out=sq[:], in_=lt[:], func=AF.Square), [lt], [sq])
                for (c0, c1, n, dst, pp, rs_) in ((0, 3, 384.0, qlatn, psA[0], rst[0]), (3, 5, 256.0, kvlatn, psA[1], rst[1])):
                    for c in range(c0, c1):
                        k.op("pe", lambda e: e.matmul(pp[:], lhsT=ones[:], rhs=sq[:, c, :], start=(c == c0), stop=(c == c1 - 1)), [ones, sq], [pp], inc=(c == c1 - 1))
                    k.op("act", lambda e: e.activation(out=rs_[:], in_=pp[:], func=AF.Sqrt, scale=1.0 / n, bias=EPS), [pp], [rs_])
                    k.op("dve", lambda e: e.reciprocal(out=rs_[:], in_=rs_[:]), [rs_], [rs_])
                    for c in range(c0, c1):
                        k.op("dve", lambda e: e.tensor_tensor(out=dst[:, c - c0, cs], in0=lt[:, c, :], in1=rs_[:], op=ALU.mult), [lt, rs_], [(dst, (c - c0, b))])
            k.barrier()
        with contextlib.ExitStack() as pc:
            qh = [k.sb("qh%d" % i, [128, S], BF16, pc) for i in range(2)]
            kh = [k.sb("kh%d" % i, [128, S], BF16, pc) for i in range(2)]
            vh = [k.sb("vh%d" % i, [128, NT, 65], BF16, pc) for i in range(2)]
            pT = [k.sb("pT%d" % i, [128, 512], BF16, pc) for i in range(3)]
            t1 = [k.sb("t1_%d" % i, [128, 512], F32, pc) for i in range(2)]
            t2 = [k.sb("t2_%d" % i, [128, 512], F32, pc) for i in range(2)]
            rec = k.sb("rec", [128, 8, NT], F32, pc)
            for i in range(2):
                k.op("dve", lambda e: e.memset(vh[i][:, :, 64:65], 1.0), [], [vh[i]])
            pti = 0
            for h in range(heads):
                q_, k_, v_ = qh[h % 2], kh[h % 2], vh[h % 2]
                for b in range(NB):
                    cs = slice(b * 512, (b + 1) * 512)
                    p1, p2_ = psA[0], psA[1]
                    for c in range(3):
                        k.op("pe", lambda e: e.matmul(p1[0:HQ, :], lhsT=wqb[:, c, h * HQ:(h + 1) * HQ], rhs=qlatn[:, c, cs], start=(c == 0), stop=(c == 2)),
                             [wqb, (qlatn, (c, b))], [p1], inc=(c == 2))
                    k.op("act", lambda e: e.copy(out=q_[0:64, cs], in_=p1[0:64, :]), [p1], [(q_, (0, b))])
                    k.op("dve", lambda e: e.tensor_tensor(out=t1[b % 2][64:96, :], in0=p1[64:96, :], in1=cosT[64:96, cs], op=ALU.mult), [p1, cosT], [t1[b % 2]])
                    for c in range(3):
                        k.op("pe", lambda e: e.matmul(p2_[0:HQ, :], lhsT=wqr[:, c, h * HQ:(h + 1) * HQ], rhs=qlatn[:, c, cs], start=(c == 0), stop=(c == 2)),
                             [wqr, (qlatn, (c, b))], [p2_], inc=(c == 2))
                    k.op("dve", lambda e: e.tensor_tensor(out=t2[b % 2][64:96, :], in0=p2_[64:96, :], in1=sinT[64:96, cs], op=ALU.mult), [p2_, sinT], [t2[b % 2]])
                    k.op("dve", lambda e: e.tensor_tensor(out=q_[64:96, cs], in0=t1[b % 2][64:96, :], in1=t2[b % 2][64:96, :], op=ALU.add),
                         [t1[b % 2], t2[b % 2]], [(q_, (1, b))])
                    for c in range(2):
                        k.op("pe", lambda e: e.matmul(p1[0:64, :], lhsT=wkvb[:, c, h * 128:h * 128 + 64], rhs=kvlatn[:, c, cs], start=(c == 0), stop=(c == 1)),
                             [wkvb, (kvlatn, (c, b))], [p1], inc=(c == 1))
                    k.op("act", lambda e: e.copy(out=k_[0:64, cs], in_=p1[0:64, :]), [p1], [(k_, (0, b))])
                    k.op("act", lambda e: e.copy(out=k_[64:96, cs], in_=kper[64:96, cs]), [kper], [(k_, (1, b))])
                for g8 in range(NT // 8):
                    pp = psA[g8 % 2]
                    for tl in range(8):
                        t = g8 * 8 + tl
                        for c in range(2):
                            k.op("pe", lambda e: e.matmul(pp[:, tl * 64:(tl + 1) * 64], lhsT=kvlatn[:, c, t * 128:(t + 1) * 128], rhs=wkvb[:, c, h * 128 + 64:h * 128 + 128],
                                                          start=(c == 0), stop=(c == 1)), [kvlatn, wkvb], [pp], inc=(c == 1 and tl == 7))
                    k.op("act", lambda e: e.copy(out=v_[:, g8 * 8:(g8 + 1) * 8, 0:64], in_=pp[:].rearrange("p (t d) -> p t d", d=64)), [pp], [(v_, g8)])
                steps = [(qb, j) for qb in range(NB) for j in range(4 * qb + 4)]

                def emit_scores(i):
                    qb, j = steps[i]
                    c0 = max(0, j - 4 * qb) * 128
                    ps_ = psS[i % 2]
                    k.op("pe", lambda e: e.matmul(ps_[:, c0:512], lhsT=k_[0:HQ, j * 128:(j + 1) * 128], rhs=q_[0:HQ, qb * 512 + c0:(qb + 1) * 512], start=True, stop=True),
                         [k_, q_], [ps_])
                emit_scores(0)
                for i, (qb, j) in enumerate(steps):
                    if i + 1 < len(steps):
                        emit_scores(i + 1)
                    r = j - 4 * qb
                    c0 = max(0, r) * 128
                    ps_ = psS[i % 2]
                    pt_ = pT[i % 3]
                    k.op("act", lambda e: e.activation(out=pt_[:, c0:512], in_=ps_[:, c0:512], func=AF.Exp), [ps_], [pt_])
                    if r >= 0:
                        k.op("dve", lambda e: e.tensor_tensor(out=pt_[:, c0:c0 + 128], in0=pt_[:, c0:c0 + 128], in1=tri[:], op=ALU.mult), [pt_, tri], [pt_])
                    for s_ in range(c0 // 128, 4):
                        k.op("pe", lambda e: e.matmul(psO[s_][:, 0:65], lhsT=pt_[:, s_ * 128:(s_ + 1) * 128], rhs=v_[:, j, 0:65], start=(j == 0), stop=(j == 4 * qb + s_)),
                             [pt_, v_], [psO[s_]], inc=(s_ == 3))
                    if j == 4 * qb + 3:
                        for s_ in range(4):
                            t = qb * 4 + s_
                            k.op("dve", lambda e: e.reciprocal(out=rec[:, h, t:t + 1], in_=psO[s_][:, 64:65]), [psO[s_]], [(rec, (h, t))])
                            k.op("dve", lambda e: e.tensor_scalar(out=mo[:, t, h * 64:(h + 1) * 64], in0=psO[s_][:, 0:64], scalar1=rec[:, h, t:t + 1], scalar2=None, op0=ALU.mult),
                                 [psO[s_], (rec, (h, t))], [(mo, (t, h))])
            k.barrier()
        with contextlib.ExitStack() as pd_:
            stat = k.sb("stat2", [128, 4, NT], F32, pd_)
            junk = k.sb("junk2", [128, 512], BF16, pd_)
            mn = [k.sb("mn%d" % i, [128, 512], F32, pd_) for i in range(2)]
            stg = [k.sb("stg2_%d" % i, [128, 4, 512], F32, pd_) for i in range(2)]
            for t in range(NT):
                mt = mo[:, t, :]
                ss, sd, rs = stat[:, 0, t:t + 1], stat[:, 1, t:t + 1], stat[:, 2, t:t + 1]
                k.op("act", lambda e: e.activation(out=junk[:], in_=mt, func=AF.Square, accum_out=ss), [mo], [junk, (stat, (t, 0))])
                k.op("act", lambda e: e.activation(out=sd, in_=ss, func=AF.Sqrt, scale=1.0 / 512, bias=EPS), [(stat, (t, 0))], [(stat, (t, 1))])
                k.op("dve", lambda e: e.reciprocal(out=rs, in_=sd), [(stat, (t, 1))], [(stat, (t, 2))])
                m_ = mn[t % 2]
                k.op("dve", lambda e: e.tensor_scalar(out=m_[:], in0=mt, scalar1=rs, scalar2=None, op0=ALU.mult), [mo, (stat, (t, 2))], [m_])
                pp = psA[t % 2].rearrange("p (c s) -> p c s", s=128)
                for c in range(4):
                    k.op("pe", lambda e: e.transpose(out=pp[:, c, :], in_=m_[:, c * 128:(c + 1) * 128], identity=ident[:]), [m_, ident], [psA[t % 2]], inc=(c == 3))
                sg = stg[(t // 4) % 2]
                for c in range(4):
                    k.op("act", lambda e: e.activation(out=sg[:, c, (t % 4) * 128:(t % 4 + 1) * 128], in_=pp[:, c, :], func=AF.Copy, scale=mv[:, 5 + c:6 + c]),
                         [psA[t % 2], mv], [(sg, (c, t % 4))])
                if t % 4 == 3:
                    b = t // 4
                    for c in range(4):
                        k.dma("sp", g.oT[c * 128:(c + 1) * 128, b * 512:(b + 1) * 512], sg[:, c, :], reads=[sg], writes=[(g.oT.tensor, ("m", c, b))])
            k.barrier()


def phase3(g):
    nc, k = g.nc, g.k
    gvd = g.din("gdn_vec", [128, 32])
    cwd = g.din("conv_wt", [64, 24, 4])
    ident, ones, projT = g.ident, g.ones, g.projT
    heads = g.cfg.get("gdn_heads", 8)
    NCH = S // 64
    BIG = 30000.0
    P = 64
    with contextlib.ExitStack() as p3:
        gv = k.sb("gv", [128, 32], F32, p3)
        k.dma("sp", gv[:], gvd, writes=[gv])
        cw = k.sb("cw", [P, 24, 4], F32, p3)
        k.dma("sp", cw[:], cwd, writes=[cw])
        trif = k.sb("trif", [128, 128], F32, p3)
        k.dma("sp", trif[:], g.consts[2], writes=[trif])
        bigm1 = k.sb("bigm1", [P, P], F32, p3)
        negb2 = k.sb("negb2", [P, P], F32, p3)
        k.op("dve", lambda e: e.tensor_scalar(out=bigm1[:], in0=trif[0:P, 0:P], scalar1=BIG, scalar2=None, op0=ALU.mult), [trif], [bigm1])
        k.op("dve", lambda e: e.tensor_scalar(out=negb2[:], in0=trif[0:P, 0:P], scalar1=-1.0, scalar2=BIG, op0=ALU.add, op1=ALU.mult), [trif], [negb2])
        beta = k.sb("beta", [P, NCH, 8], F32, p3)
        gc = k.sb("gc", [P, NCH, 8], F32, p3)
        ngc = k.sb("ngc", [P, NCH, 8], F32, p3)
        bgam = k.sb("bgam", [P, NCH, 8], F32, p3)
        ktl = k.sb("ktl", [P, NCH, 8], F32, p3)
        cdb = k.sb("cdb", [P, NCH, 8], F32, p3)
        ps = [k.ps("pgd%d" % i, [128, 512], F32, p3) for i in range(8)]
        psq = ps
        with contextlib.ExitStack() as pa:
            ab = k.sb("ab", [16, S], F32, pa)
            k.dma("sp", ab[:], projT[2720:2736, :], reads=[projT.tensor], writes=[ab])
            abt = k.sb("abt", [P, NCH, 16], F32, pa)
            for q4 in range(2):
                pp = ps[q4]
                for j in range(32):
                    n = q4 * 32 + j
                    k.op("pe", lambda e: e.transpose(out=pp[0:P, j * 16:(j + 1) * 16], in_=ab[0:16, n * 64:(n + 1) * 64], identity=ident[0:16, 0:16]),
                         [ab, ident], [pp], inc=(j == 31))
                k.op("act", lambda e: e.copy(out=abt[:, q4 * 32:(q4 + 1) * 32, :], in_=pp[0:P, :].rearrange("p (n f) -> p n f", f=16)), [pp], [(abt, q4)])
            xa = k.sb("xa_", [P, NCH, 8], F32, pa)
            ax = k.sb("ax_", [P, NCH, 8], F32, pa)
            gg = k.sb("gg_", [P, NCH, 8], F32, pa)
            ea = k.sb("ea_", [P, 8], F32, pa)
            gt = k.sb("gt_", [P, NCH, 8], F32, pa)
            k.op("act", lambda e: e.activation(out=beta[:], in_=abt[:, :, 8:16], func=AF.Sigmoid), [abt], [beta])
            for n in range(NCH):
                k.op("dve", lambda e: e.tensor_tensor(out=xa[:, n, :], in0=abt[:, n, 0:8], in1=gv[0:P, 8:16], op=ALU.add), [abt, gv], [(xa, n)])
            k.op("act", lambda e: e.activation(out=ax[:], in_=xa[:], func=AF.Abs), [xa], [ax])
            k.op("act", lambda e: e.activation(out=ax[:], in_=ax[:], func=AF.Exp, scale=-1.0), [ax], [ax])
            k.op("act", lambda e: e.activation(out=ax[:], in_=ax[:], func=AF.Ln, bias=1.0), [ax], [ax])
            k.op("dve", lambda e: e.scalar_tensor_tensor(out=xa[:], in0=xa[:], scalar=0.0, in1=ax[:], op0=ALU.max, op1=ALU.add), [xa, ax], [xa])
            k.op("act", lambda e: e.activation(out=ea[:], in_=gv[0:P, 0:8], func=AF.Exp), [gv], [ea])
            for n in range(NCH):
                k.op("dve", lambda e: e.tensor_tensor(out=gg[:, n, :], in0=xa[:, n, :], in1=ea[:], op=ALU.mult), [xa, ea], [(gg, n)])
            k.op("dve", lambda e: e.tensor_scalar(out=gg[:], in0=gg[:], scalar1=-1.0, scalar2=None, op0=ALU.mult), [gg], [gg])
            ggf = gg[:].rearrange("p n h -> p (n h)")
            pc_, pt_ = ps[2], ps[3]
            k.op("pe", lambda e: e.matmul(pc_[0:P, :], lhsT=trif[0:P, 0:P], rhs=ggf, start=True, stop=True), [trif, gg], [pc_])
            k.op("pe", lambda e: e.matmul(pt_[0:P, :], lhsT=ones[0:P, 0:P], rhs=ggf, start=True, stop=True), [ones, gg], [pt_])
            f2 = lambda t_: t_[:].rearrange("p n h -> p (n h)")
            k.op("act", lambda e: e.copy(out=f2(gc), in_=pc_[0:P, :]), [pc_], [gc])
            k.op("act", lambda e: e.copy(out=f2(ax), in_=pt_[0:P, :]), [pt_], [ax])
            k.op("act", lambda e: e.activation(out=f2(cdb), in_=f2(ax), func=AF.Exp), [ax], [cdb])
            k.op("dve", lambda e: e.tensor_tensor(out=f2(gt), in0=f2(ax), in1=f2(gc), op=ALU.subtract), [ax, gc], [gt])
            k.op("act", lambda e: e.activation(out=f2(ktl), in_=f2(gt), func=AF.Exp), [gt], [ktl])
            k.op("dve", lambda e: e.tensor_scalar(out=f2(ngc), in0=f2(gc), scalar1=-1.0, scalar2=None, op0=ALU.mult), [gc], [ngc])
            k.op("act", lambda e: e.activation(out=f2(gt), in_=f2(gc), func=AF.Exp), [gc], [gt])
            k.op("dve", lambda e: e.tensor_tensor(out=f2(bgam), in0=f2(gt), in1=f2(beta), op=ALU.mult), [gt, beta], [bgam])
            k.barrier()
        with contextlib.ExitStack() as pb:
            xp = k.sb("xp", [P, S + 4], F32, pb)
            cv = k.sb("cv", [P, S], F32, pb)
            cvo = k.sb("cvo", [P, S], F32, pb)
            u_all = k.sb("u_all", [P, NCH, 64], F32, pb)
            wT_all = k.sb("wT_all", [P, S], BF16, pb)
            qg_all = k.sb("qg_all", [P, S], BF16, pb)
            qk_all = k.sb("qk_all", [P, NCH, 64], BF16, pb)
            kt_all = k.sb("kt_all", [P, NCH, 64], BF16, pb)
            kTb2 = [k.sb("kTb%d" % i, [P, S], BF16, pb) for i in range(2)]
            qTb2 = [k.sb("qTb%d" % i, [P, S], BF16, pb) for i in range(2)]
            vTb2 = [k.sb("vTb%d" % i, [P, S], BF16, pb) for i in range(2)]
            identb = k.sb("identb", [P, P], BF16, pb)
            k.op("act", lambda e: e.copy(out=identb[:], in_=ident[0:P, 0:P]), [ident], [identb])
            o_all = u_all
            Xs = [k.sb("Xs%d" % i, [P, P], F32, pb) for i in range(2)]
            Gb = [k.sb("Gb%d" % i, [P, P], F32, pb) for i in range(2)]
            D1 = [k.sb("D1_%d" % i, [P, P], F32, pb) for i in range(2)]
            DT = [k.sb("DT_%d" % i, [P, P], F32, pb) for i in range(2)]
            GW = 8
            P_w = [[k.sb("Pw%d_%d" % (i, j), [P, GW * 64], BF16, pb) for j in range(2)] for i in range(2)]
            Q_w = [[k.sb("Qw%d_%d" % (i, j), [P, GW * 64], BF16, pb) for j in range(2)] for i in range(2)]
            W_w = [[k.sb("Ww%d_%d" % (i, j), [P, GW * 64], BF16, pb) for j in range(2)] for i in range(2)]
            kbg = [k.sb("kbg%d" % i, [P, P], BF16, pb) for i in range(2)]
            bv = [k.sb("bv%d" % i, [P, P], BF16, pb) for i in range(2)]
            Sst = [k.sb("Sst%d" % i, [P, P], F32, pb) for i in range(2)]
            Sbf = [k.sb("Sbf%d" % i, [P, P], BF16, pb) for i in range(2)]
            vn = [k.sb("vn%d" % i, [P, P], BF16, pb) for i in range(2)]
            rsd = k.sb("rsd", [P, 4, NCH], F32, pb)
            k.op("dve", lambda e: e.memset(xp[:, 0:4], 0.0), [], [(xp, "pad")])
            epsb = k.sb("epsb", [P, 1], F32, pb)
            k.op("dve", lambda e: e.memset(epsb[:], EPS), [], [epsb])
            psi = 0

            psf = 0

            psd = 0

            def nps(kind):
                nonlocal psi, psd
                if kind == "act":
                    psi += 1
                    return psq[psi % 4]
                psd += 1
                return psq[4 + psd % 4]

            def npsf(kind="act"):
                return nps(kind)
            def conv_gen(hh):
                for ti, (dstb, row0) in enumerate(((qTb2[hh % 2], 672), (kTb2[hh % 2], 1184), (vTb2[hh % 2], 1696))):
                    k.dma("sp", xp[:, 4:S + 4], projT[row0 + hh * 64:row0 + (hh + 1) * 64, :], reads=[projT.tensor], writes=[(xp, "x")])
                    ci = ti * 8 + hh
                    k.op("dve", lambda e: e.tensor_scalar(out=cv[:], in0=xp[:, 1:S + 1], scalar1=cw[:, ci, 0:1], scalar2=None, op0=ALU.mult), [xp, cw], [cv])
                    yield
                    for j in range(1, 4):
                        k.op("dve", lambda e: e.scalar_tensor_tensor(out=cv[:], in0=xp[:, 1 + j:S + 1 + j], scalar=cw[:, ci, j:j + 1], in1=cv[:], op0=ALU.mult, op1=ALU.add),
                             [xp, cw, cv], [cv])
                        yield
                    if ti == 2:
                        k.op("act", lambda e: e.activation(out=dstb[:], in_=cv[:], func=AF.Silu), [cv], [dstb])
                        yield
                        continue
                    k.op("act", lambda e: e.activation(out=cvo[:], in_=cv[:], func=AF.Silu), [cv], [cvo])
                    k.op("dve", lambda e: e.tensor_tensor(out=cv[:], in0=cvo[:], in1=cvo[:], op=ALU.mult), [cvo], [cv])
                    yield
                    for b in range(NB):
                        cs = slice(b * 512, (b + 1) * 512)
                        pp = npsf()
                        k.op("pe", lambda e: e.matmul(pp[0:P, :], lhsT=ones[0:P, 0:P], rhs=cv[:, cs], start=True, stop=True), [ones, cv], [pp])
                        k.op("act", lambda e: e.activation(out=cv[:, cs], in_=pp[0:P, :], func=AF.Ln, bias=epsb[0:P, 0:1]), [pp, epsb], [(cv, b)])
                        k.op("act", lambda e: e.activation(out=cv[:, cs], in_=cv[:, cs], func=AF.Exp, scale=-0.5), [(cv, b)], [(cv, b)])
                        sc_ = 0.125 if ti == 0 else 1.0
                        k.op("dve", lambda e: e.scalar_tensor_tensor(out=dstb[:, cs], in0=cvo[:, cs], scalar=sc_, in1=cv[:, cs], op0=ALU.mult, op1=ALU.mult),
                             [cvo, (cv, b)], [(dstb, b)])
                        yield

            ei = 0
            for h in range(heads):
                if h == 0:
                    for _ in conv_gen(0):
                        pass
                kTb, qTb, vTb = kTb2[h % 2], qTb2[h % 2], vTb2[h % 2]
                nxt_conv = conv_gen(h + 1) if h + 1 < heads else iter(())
                G = 8
                St = Sst[0]
                k.op("dve", lambda e: e.memset(St[:], 0.0), [], [St])
                k.op("dve", lambda e: e.memset(Sbf[0][:], 0.0), [], [Sbf[0]])

                def scan_step(n, St):
                    cs = slice(n * 64, (n + 1) * 64)
                    p1_, p2_, p3_ = nps("dve"), nps("act"), nps("dve")
                    v_ = vn[n % 2]
                    Sb = Sbf[n % 2]
                    k.op("pe", lambda e: e.matmul(p1_[0:P, 0:P], lhsT=wT_all[:, cs], rhs=Sb[:], start=True, stop=True), [(wT_all, n), Sb], [p1_])
                    k.op("dve", lambda e: e.tensor_tensor(out=v_[:], in0=u_all[:, n, :], in1=p1_[0:P, 0:P], op=ALU.subtract), [(u_all, n), p1_], [v_])
                    k.op("pe", lambda e: e.matmul(p2_[0:P, 0:P], lhsT=qg_all[:, cs], rhs=Sb[:], start=True, stop=False), [(qg_all, n), Sb], [p2_], inc=False)
                    k.op("pe", lambda e: e.matmul(p2_[0:P, 0:P], lhsT=qk_all[:, n, :], rhs=v_[:], start=False, stop=True), [(qk_all, n), v_], [p2_])
                    k.op("pe", lambda e: e.matmul(p3_[0:P, 0:P], lhsT=kt_all[:, n, :], rhs=v_[:], start=True, stop=True), [(kt_all, n), v_], [p3_])
                    Sn = Sst[(n + 1) % 2]
                    k.op("dve", lambda e: e.scalar_tensor_tensor(out=Sn[:], in0=St[:], scalar=cdb[:, n, h:h + 1], in1=p3_[0:P, 0:P], op0=ALU.mult, op1=ALU.add),
                         [St, cdb, p3_], [Sn])
                    k.op("act", lambda e: e.copy(out=Sbf[(n + 1) % 2][:], in_=Sn[:]), [Sn], [Sbf[(n + 1) % 2]])
                    k.op("act", lambda e: e.copy(out=o_all[:, n, :], in_=p2_[0:P, 0:P]), [p2_], [(o_all, n)])
                    return Sn

                def stage_a(n, sl):
                    cs = slice(n * 64, (n + 1) * 64)
                    kTn, qTn = kTb[:, cs], qTb[:, cs]
                    gcn, ngcn, btn = gc[:, n, h:h + 1], ngc[:, n, h:h + 1], beta[:, n, h:h + 1]
                    X = Xs[n % 2]
                    k.op("dve", lambda e: e.tensor_scalar(out=X[:], in0=ident[0:P, 0:P], scalar1=gcn, scalar2=None, op0=ALU.mult), [ident, gc], [X])
                    pR, pR2, pR3 = nps("act"), nps("act"), nps("act")
                    k.op("pe", lambda e: e.matmul(pR[0:P, 0:P], lhsT=ones[0:P, 0:P], rhs=X[:], start=True, stop=False), [ones, X], [pR], inc=False)
                    k.op("pe", lambda e: e.matmul(pR[0:P, 0:P], lhsT=ident[0:P, 0:P], rhs=bigm1[:], start=False, stop=True), [ident, bigm1], [pR])
                    k.op("pe", lambda e: e.matmul(pR2[0:P, 0:P], lhsT=ones[0:P, 0:P], rhs=X[:], start=True, stop=False), [ones, X], [pR2], inc=False)
                    k.op("pe", lambda e: e.matmul(pR2[0:P, 0:P], lhsT=ident[0:P, 0:P], rhs=negb2[:], start=False, stop=True), [ident, negb2], [pR2])
                    k.op("pe", lambda e: e.matmul(pR3[0:P, 0:P], lhsT=ones[0:P, 0:P], rhs=X[:], start=True, stop=True), [ones, X], [pR3])
                    d1, dt_, gb = D1[n % 2], DT[n % 2], Gb[n % 2]
                    k.op("act", lambda e: e.activation(out=d1[:], in_=pR[0:P, 0:P], func=AF.Exp, scale=-1.0, bias=gcn), [pR, gc], [d1])
                    k.op("act", lambda e: e.activation(out=dt_[:], in_=pR2[0:P, 0:P], func=AF.Exp, scale=1.0, bias=ngcn), [pR2, ngc], [dt_])
                    k.op("act", lambda e: e.activation(out=gb[:], in_=pR3[0:P, 0:P], func=AF.Exp), [pR3], [gb])
                    pK, pQ = nps("dve"), nps("dve")
                    k.op("pe", lambda e: e.matmul(pK[0:P, 0:P], lhsT=kTn, rhs=kTn, start=True, stop=True), [kTb], [pK])
                    k.op("pe", lambda e: e.matmul(pQ[0:P, 0:P], lhsT=kTn, rhs=qTn, start=True, stop=True), [kTb, qTb], [pQ])
                    A, Q0, W = TV(P_w[sl // G][0], sl % G), TV(Q_w[sl // G][0], sl % G), TV(W_w[sl // G][0], sl % G)
                    k.op("dve", lambda e: e.scalar_tensor_tensor(out=A[:], in0=pK[0:P, 0:P], scalar=btn, in1=d1[:], op0=ALU.mult, op1=ALU.mult), [pK, beta, d1], [A])
                    k.op("dve", lambda e: e.tensor_tensor(out=qk_all[:, n, :], in0=pQ[0:P, 0:P], in1=dt_[:], op=ALU.mult), [pQ, dt_], [(qk_all, n)])
                    k.op("dve", lambda e: e.tensor_tensor(out=qg_all[:, cs], in0=qTn, in1=gb[:], op=ALU.mult), [qTb, gb], [(qg_all, n)])
                    pT_ = nps("act")
                    k.op("pe", lambda e: e.matmul(pT_[0:P, 0:P], lhsT=A[:], rhs=identb[:], start=True, stop=True), [A, identb], [pT_])
                    k.op("act", lambda e: e.copy(out=Q0[:], in_=pT_[0:P, 0:P]), [pT_], [Q0])
                    k.op("dve", lambda e: e.tensor_tensor(out=W[:], in0=ident[0:P, 0:P], in1=Q0[:], op=ALU.subtract), [ident, Q0], [W])

                def stage_b(par, lv):
                    a, b_ = lv % 2, (lv + 1) % 2
                    bankP, bankW = nps("act"), nps("dve")
                    for c in range(G):
                        Pk, Qk = TV(P_w[par][a], c), TV(Q_w[par][a], c)
                        k.op("pe", lambda e: e.matmul(bankP[0:P, c * 64:(c + 1) * 64], lhsT=Qk[:], rhs=Pk[:], start=True, stop=True), [Qk, Pk], [bankP], inc=(c == G - 1))
                    if lv < 4:
                        bankQ = nps("dve")
                        for c in range(G):
                            Pk, Qk = TV(P_w[par][a], c), TV(Q_w[par][a], c)
                            k.op("pe", lambda e: e.matmul(bankQ[0:P, c * 64:(c + 1) * 64], lhsT=Pk[:], rhs=Qk[:], start=True, stop=True), [Pk, Qk], [bankQ], inc=(c == G - 1))
                    k.op("act", lambda e: e.copy(out=P_w[par][b_][:], in_=bankP[0:P, 0:G * 64]), [bankP], [P_w[par][b_]])
                    if lv < 4:
                        k.op("dve", lambda e: e.tensor_copy(out=Q_w[par][b_][:], in_=bankQ[0:P, 0:G * 64]), [bankQ], [Q_w[par][b_]])
                    for c in range(G):
                        Pn, W = TV(P_w[par][b_], c), TV(W_w[par][a], c)
                        k.op("pe", lambda e: e.matmul(bankW[0:P, c * 64:(c + 1) * 64], lhsT=Pn[:], rhs=W[:], start=True, stop=True), [Pn, W], [bankW], inc=(c == G - 1))
                    k.op("dve", lambda e: e.tensor_tensor(out=W_w[par][b_][:], in0=bankW[0:P, 0:G * 64], in1=W_w[par][a][:], op=ALU.add), [bankW, W_w[par][a]], [W_w[par][b_]])

                def stage_c(n, sl):
                    cs = slice(n * 64, (n + 1) * 64)
                    kTn, vTn = kTb[:, cs], vTb[:, cs]
                    btn = beta[:, n, h:h + 1]
                    W = TV(W_w[sl // G][1], sl % G)
                    pk_, pv_ = nps("dve"), nps("act")
                    k.op("pe", lambda e: e.matmul(pk_[0:P, 0:P], lhsT=kTn, rhs=identb[:], start=True, stop=True), [kTb, identb], [pk_])
                    k.op("pe", lambda e: e.matmul(pv_[0:P, 0:P], lhsT=vTn, rhs=identb[:], start=True, stop=True), [vTb, identb], [pv_])
                    kb_, bv_ = kbg[n % 2], bv[n % 2]
                    k.op("dve", lambda e: e.tensor_scalar(out=kb_[:], in0=pk_[0:P, 0:P], scalar1=bgam[:, n, h:h + 1], scalar2=None, op0=ALU.mult), [pk_, bgam], [kb_])
                    k.op("dve", lambda e: e.tensor_scalar(out=kt_all[:, n, :], in0=pk_[0:P, 0:P], scalar1=ktl[:, n, h:h + 1], scalar2=None, op0=ALU.mult), [pk_, ktl], [(kt_all, n)])
                    k.op("act", lambda e: e.activation(out=bv_[:], in_=pv_[0:P, 0:P], func=AF.Copy, scale=btn), [pv_, beta], [bv_])
                    pu, pw = nps("act"), nps("dve")
                    k.op("pe", lambda e: e.matmul(pu[0:P, 0:P], lhsT=W[:], rhs=bv_[:], start=True, stop=True), [W, bv_], [pu])
                    k.op("pe", lambda e: e.matmul(pw[0:P, 0:P], lhsT=kb_[:], rhs=W[:], start=True, stop=True), [kb_, W], [pw])
                    k.op("act", lambda e: e.copy(out=u_all[:, n, :], in_=pu[0:P, 0:P]), [pu], [(u_all, n)])
                    k.op("dve", lambda e: e.tensor_copy(out=wT_all[:, cs], in_=pw[0:P, 0:P]), [pw], [(wT_all, n)])

                ngrp = NCH // G
                for gi in range(ngrp + 1):
                    for _ in range(7):
                        next(nxt_conv, None)
                    base = (gi % 2) * G
                    if gi < ngrp:
                        for c in range(G):
                            stage_a(gi * G + c, base + c)
                    for lv in range(5):
                        if gi < ngrp:
                            stage_b(gi % 2, lv)
                        if gi >= 1:
                            for sj in range(lv * G // 5, (lv + 1) * G // 5):
                                St = scan_step((gi - 1) * G + sj, St)
                    if gi < ngrp:
                        for c in range(G):
                            stage_c(gi * G + c, base + c)
                k.op("dve", lambda e: e.tensor_tensor(out=cv[:].rearrange("p (n d) -> p n d", d=64), in0=o_all[:], in1=o_all[:], op=ALU.mult), [o_all], [cv])
                k.op("dve", lambda e: e.tensor_reduce(out=rsd[:, 0, :], in_=cv[:].rearrange("p (n d) -> p n d", d=64), axis=AX.X, op=ALU.add), [cv], [(rsd, 0)])
                k.op("act", lambda e: e.activation(out=rsd[:, 1, :], in_=rsd[:, 0, :], func=AF.Sqrt, scale=1.0 / 64, bias=EPS), [(rsd, 0)], [(rsd, 1)])
                k.op("dve", lambda e: e.reciprocal(out=rsd[:, 2, :], in_=rsd[:, 1, :]), [(rsd, 1)], [(rsd, 2)])
                k.dma("sp", xp[:, 4:S + 4], projT[2208 + h * 64:2208 + (h + 1) * 64, :], reads=[projT.tensor], writes=[(xp, "x")])
                k.op("act", lambda e: e.activation(out=xp[:, 4:S + 4], in_=xp[:, 4:S + 4], func=AF.Silu), [(xp, "x")], [(xp, "x")])
                for b in range(NB):
                    pp = npsf("dve")
                    for j in range(8):
                        n = b * 8 + j
                        k.op("dve", lambda e: e.tensor_scalar(out=o_all[:, n, :], in0=o_all[:, n, :], scalar1=rsd[:, 2, n:n + 1], scalar2=None, op0=ALU.mult),
                             [(o_all, n), (rsd, 2)], [(o_all, n)])
                        k.op("pe", lambda e: e.transpose(out=pp[0:P, j * 64:(j + 1) * 64], in_=o_all[:, n, :], identity=ident[0:P, 0:P]), [(o_all, n), ident], [pp], inc=(j == 7))
                    cs = slice(b * 512, (b + 1) * 512)
                    k.op("dve", lambda e: e.scalar_tensor_tensor(out=cv[:, cs], in0=pp[0:P, :], scalar=gv[0:P, 16:17], in1=xp[:, 4 + b * 512:4 + (b + 1) * 512], op0=ALU.mult, op1=ALU.mult),
                         [pp, gv, (xp, "x")], [(cv, b)])
                k.dma("sp", g.oT[512 + h * 64:512 + (h + 1) * 64, :], cv[:], reads=[cv], writes=[(g.oT.tensor, ("g", h))])
            k.barrier()


def phase4(g):
    nc, k = g.nc, g.k
    w_out = g.din("w_out", [D, D])
    router_w = g.din("router_w", [D, NE])
    router_b = g.din("router_b", [1, NE])
    x, ident, ones = g.x, g.ident, g.ones
    with contextlib.ExitStack() as p4:
        woutb = k.sb("woutb", [128, 8, D], BF16, p4)
        wov = w_out.rearrange("(c p) n -> p c n", p=128)
        k.dma("pool", woutb[:], wov, writes=[woutb])
        rw = k.sb("rw", [128, 8, NE], F32, p4)
        k.dma("sp", rw[:], router_w.rearrange("(c p) n -> p c n", p=128), writes=[rw])
        rb = k.sb("rb", [1, NE], F32, p4)
        k.dma("sp", rb[:], router_b, writes=[rb])
        bcs = {}
        for nm, slot in (("gt1", 2), ("sh2", 3), ("s2", 4)):
            bcs[nm] = k.sb(nm + "_bc", [128, D], F32, p4)
            k.dma("sp", bcs[nm][:], g.modscr[slot], reads=[(g.modscr.tensor, slot)], writes=[bcs[nm]])
        oTb = [k.sb("oTb%d" % i, [128, 8, 512], BF16, p4) for i in range(2)]
        xts = [k.sb("xq%d" % i, [128, D], F32, p4) for i in range(2)]
        x1s = [k.sb("x1s%d" % i, [128, D], F32, p4) for i in range(2)]
        hts = [k.sb("h2t%d" % i, [128, D], F32, p4) for i in range(2)]
        junk = k.sb("junk4", [128, D], BF16, p4)
        stat = k.sb("stat4", [128, 4, NT], F32, p4)
        h2Tf = [k.sb("h2Tf%d" % i, [128, 8, 128], F32, p4) for i in range(2)]
        h2Tb = [k.sb("h2Tb%d" % i, [128, 8, 512], BF16, p4) for i in range(2)]
        lg = k.sb("lg", [128, NT, NE], F32, p4)
        m8 = k.sb("m8", [128, NT, 8], F32, p4)
        rt = k.sb("rt", [128, 4, NT], F32, p4)
        msk = [k.sb("msk%d" % i, [128, NE], F32, p4) for i in range(2)]
        ex = [k.sb("ex%d" % i, [128, NE], F32, p4) for i in range(2)]
        pmx = [k.ps("pmx%d" % i, [128, 512], F32, p4) for i in range(4)]
        ptr = [k.ps("ptr4_%d" % i, [128, 4, 128], F32, p4) for i in range(2)]
        plg = [k.ps("plg%d" % i, [128, NE], F32, p4) for i in range(2)]
        oTv = g.oT.rearrange("(c p) s -> p c s", p=128)
        h2Tv = g.h2T.rearrange("(c p) s -> p c s", p=128)
        for blk in range(NB):
            ob = oTb[blk % 2]
            k.dma("pool", ob[:], oTv[:, :, blk * 512:(blk + 1) * 512], reads=[g.oT.tensor], writes=[ob])
            for tt in range(4):
                t = blk * 4 + tt
                xt = xts[t % 2]
                x1t = x1s[t % 2]
                ht = hts[t % 2]
                k.dma("sp", xt[:], x[t * 128:(t + 1) * 128, :], writes=[xt])
                for half in range(2):
                    pp = pmx[(2 * t + half) % 4]
                    for kc in range(8):
                        k.op("pe", lambda e: e.matmul(pp[:], lhsT=ob[:, kc, tt * 128:(tt + 1) * 128], rhs=woutb[:, kc, half * 512:(half + 1) * 512],
                                                      start=(kc == 0), stop=(kc == 7)), [ob, woutb], [pp], inc=(kc == 7))
                    hs = slice(half * 512, (half + 1) * 512)
                    k.op("dve", lambda e: e.tensor_tensor(out=x1t[:, hs], in0=pp[:], in1=bcs["gt1"][:, hs], op=ALU.mult), [pp, bcs["gt1"]], [(x1t, half)])
                    k.op("dve", lambda e: e.tensor_tensor(out=x1t[:, hs], in0=x1t[:, hs], in1=xt[:, hs], op=ALU.add), [(x1t, half), xt], [(x1t, half)])
                k.dma("sp", g.x1[t * 128:(t + 1) * 128, :], x1t[:], reads=[x1t], writes=[(g.x1.tensor, t)])
                if g.cfg.get("p4_level", 9) < 0.2:
                    continue
                rs, rskey = rms_tile(g, x1t, stat, t, junk, 1.0 / D)
                if g.cfg.get("p4_level", 9) < 0.27:
                    continue
                k.op("dve", lambda e: e.scalar_tensor_tensor(out=ht[:], in0=x1t[:], scalar=rs, in1=bcs["s2"][:], op0=ALU.mult, op1=ALU.mult),
                     [x1t, rskey, bcs["s2"]], [ht])
                if g.cfg.get("p4_level", 9) < 0.29:
                    continue
                k.op("dve", lambda e: e.tensor_tensor(out=ht[:], in0=ht[:], in1=bcs["sh2"][:], op=ALU.add), [ht, bcs["sh2"]], [ht])
                if g.cfg.get("p4_level", 9) < 0.5:
                    continue
                hf = h2Tf[t % 2]
                hb = h2Tb[blk % 2]
                for half in range(2):
                    pt = ptr[half]
                    for j in range(4):
                        kc = half * 4 + j
                        k.op("pe", lambda e: e.transpose(out=pt[:, j, :], in_=ht[:, kc * 128:(kc + 1) * 128], identity=ident[:]),
                             [ht, ident], [pt], inc=(j == 3))
                    k.op("act", lambda e: e.copy(out=hf[:, half * 4:half * 4 + 4, :], in_=pt[:]), [pt], [(hf, half)])
                    k.op("dve", lambda e: e.tensor_copy(out=hb[:, half * 4:half * 4 + 4, tt * 128:(tt + 1) * 128], in_=hf[:, half * 4:half * 4 + 4, :]),
                         [(hf, half)], [(hb, (half, tt))])
                if tt == 3 and g.cfg.get("p4_level", 9) >= 0.7:
                    for kc in range(8):
                        k.dma("sp", g.h2T[kc * 128:(kc + 1) * 128, blk * 512:(blk + 1) * 512], hb[:, kc, :], reads=[hb], writes=[(g.h2T.tensor, (blk, kc))])
                if g.cfg.get("p4_level", 9) < 2:
                    continue
                pl = plg[t % 2]
                for kc in range(8):
                    k.op("pe", lambda e: e.matmul(pl[:], lhsT=hf[:, kc, :], rhs=rw[:, kc, :], start=(kc == 0), stop=False), [hf, rw], [pl], inc=False)
                k.op("pe", lambda e: e.matmul(pl[:], lhsT=ones[0:1, :], rhs=rb[0:1, :], start=False, stop=True), [ones, rb], [pl])
                lgt = lg[:, t, :]
                k.op("act", lambda e: e.copy(out=lgt, in_=pl[:]), [pl], [(lg, t)])
                k.op("dve", lambda e: e.max(out=m8[:, t, :], in_=lgt), [(lg, t)], [(m8, t)])
                mk = msk[t % 2]
                et = ex[t % 2]
                k.op("dve", lambda e: e.tensor_scalar(out=mk[:], in0=lgt, scalar1=m8[:, t, 3:4], scalar2=None, op0=ALU.is_ge), [(lg, t), (m8, t)], [mk])
                k.op("dve", lambda e: e.tensor_scalar(out=rt[:, 0, t:t + 1], in0=m8[:, t, 0:1], scalar1=-1.0, scalar2=None, op0=ALU.mult), [(m8, t)], [(rt, (t, 0))])
                k.op("act", lambda e: e.activation(out=et[:], in_=lgt, func=AF.Exp, bias=rt[:, 0, t:t + 1], scale=1.0), [(lg, t), (rt, (t, 0))], [et])
                k.op("dve", lambda e: e.tensor_tensor(out=et[:], in0=et[:], in1=mk[:], op=ALU.mult), [et, mk], [et])
                k.op("dve", lambda e: e.reduce_sum(out=rt[:, 1, t:t + 1], in_=et[:], axis=AX.X), [et], [(rt, (t, 1))])
                k.op("dve", lambda e: e.reciprocal(out=rt[:, 2, t:t + 1], in_=rt[:, 1, t:t + 1]), [(rt, (t, 1))], [(rt, (t, 2))])
                k.op("dve", lambda e: e.tensor_scalar(out=g.wr[:, t, :], in0=et[:], scalar1=rt[:, 2, t:t + 1], scalar2=None, op0=ALU.mult),
                     [et, (rt, (t, 2))], [(g.wr, t)])
        k.barrier()


def phase5(g):
    nc, k = g.nc, g.k
    cfg = g.cfg
    n_exp = cfg.get("n_exp", NE)
    g.cast_some(12)
    bgu_t = g.din("bgu_t", [128, NE, 16])
    b_dn = g.din("b_down", [NE, D])
    ident = g.ident
    PT = 1024
    npass = S // PT
    with contextlib.ExitStack() as p5:
        gt2 = k.sb("gt2_bc", [128, D], F32, p5)
        fg = k.sb("fg_bc", [128, D], F32, p5)
        k.dma("sp", gt2[:], g.modscr[5], reads=[(g.modscr.tensor, 5)], writes=[gt2])
        k.dma("sp", fg[:], g.gvec[2], writes=[fg])
        bgu = k.sb("bgu", [128, NE, 16], F32, p5)
        k.dma("sp", bgu[:], bgu_t, writes=[bgu])
        bgu1 = k.sb("bgu1", [128, NE, 16], F32, p5)
        k.op("dve", lambda e: e.tensor_scalar(out=bgu1[:], in0=bgu[:], scalar1=1.0, scalar2=None, op0=ALU.add), [bgu], [bgu1])
        bdn = k.sb("bdn", [NE, D], F32, p5)
        k.dma("sp", bdn[:], b_dn, writes=[bdn])
        h2s = k.sb("h2s", [128, 8, PT], BF16, p5)
        yacc = k.sb("yacc", [128, PT // 128, D], F32, p5)
        wgu = [k.sb("wgu%d" % i, [128, 8, 2 * D], BF16, p5) for i in range(2)]
        wdn = [k.sb("wdn%d" % i, [128, 8, D], BF16, p5) for i in range(2)]
        actT = [k.sb("actT%d" % i, [128, 8, 512], BF16, p5) for i in range(2)]
        gS = [k.sb("gS%d" % i, [128, 512], F32, p5) for i in range(2)]
        sS = [k.sb("sS%d" % i, [128, 512], F32, p5) for i in range(2)]
        uS = [k.sb("uS%d" % i, [128, 512], F32, p5) for i in range(2)]
        wrT = k.sb("wrT", [NE, PT], F32, p5)
        xa = [k.sb("xa%d" % i, [128, D], F32, p5) for i in range(2)]
        junk = k.sb("junk5", [128, D], BF16, p5)
        stat = k.sb("stat5", [128, 4, NT], F32, p5)
        pg = [k.ps("pg%d" % i, [128, 512], F32, p5) for i in range(2)]
        pu = [k.ps("pu%d" % i, [128, 512], F32, p5) for i in range(2)]
        pd = [k.ps("pd%d" % i, [128, 512], F32, p5) for i in range(4)]
        h2Tv = g.h2T.rearrange("(c p) s -> p c s", p=128)
        wguv = g.wgu_bf.rearrange("e (c p) n -> e p c n", p=128)
        wdnv = g.wdn_bf.rearrange("e (c p) n -> e p c n", p=128)

        n_tot = npass * n_exp

        def load_wg(ei):
            if ei >= n_tot:
                return
            wg = wgu[ei % 2]
            e_ = ei % n_exp
            for kc in range(0, 8, 4):
                k.dma("sp", wg[:, kc:kc + 4, :], wguv[e_, :, kc:kc + 4, :], reads=[(g.wgu_bf.tensor, e_)], writes=[(wg, kc + j) for j in range(4)])

        def load_wd(ei):
            if ei >= n_tot:
                return
            wd = wdn[ei % 2]
            e_ = ei % n_exp
            k.dma("sp", wd[:], wdnv[e_], reads=[(g.wdn_bf.tensor, e_)], writes=[(wd, kc) for kc in range(8)])

        fci = 0
        pdi = 0
        NBK = PT // 512

        def emit_gu(ei, bk):
            nonlocal fci
            e_ = ei % n_exp
            wg = wgu[ei % 2]
            aT = actT[(ei * NBK + bk) % 2]
            rhs_cols = slice(bk * 512, (bk + 1) * 512)
            for fc in range(8):
                pgt = pg[fci % 2]
                put = pu[fci % 2]
                g_ = gS[fci % 2]
                s_ = sS[fci % 2]
                u_ = uS[fci % 2]
                fci += 1
                for kc in range(8):
                    k.op("pe", lambda e: e.matmul(pgt[:], lhsT=wg[:, kc, fc * 128:(fc + 1) * 128], rhs=h2s[:, kc, rhs_cols],
                                                  start=(kc == 0), stop=(kc == 7)), [(wg, kc), h2s], [pgt], inc=(kc == 7))
                for kc in range(8):
                    k.op("pe", lambda e: e.matmul(put[:], lhsT=wg[:, kc, D + fc * 128:D + (fc + 1) * 128], rhs=h2s[:, kc, rhs_cols],
                                                  start=(kc == 0), stop=(kc == 7)), [(wg, kc), h2s], [put], inc=(kc == 7))
                k.op("dve", lambda e: e.tensor_scalar(out=g_[:], in0=pgt[:], scalar1=bgu[:, e_, fc:fc + 1], scalar2=7.0, op0=ALU.add, op1=ALU.min),
                     [pgt, bgu], [g_])
                k.op("act", lambda e: e.activation(out=s_[:], in_=g_[:], func=AF.Sigmoid, scale=1.702), [g_], [s_])
                k.op("dve", lambda e: e.tensor_scalar(out=u_[:], in0=put[:], scalar1=bgu1[:, e_, 8 + fc:9 + fc], scalar2=8.0, op0=ALU.add, op1=ALU.min),
                     [put, bgu1], [u_])
                k.op("dve", lambda e: e.tensor_tensor(out=g_[:], in0=g_[:], in1=s_[:], op=ALU.mult), [g_, s_], [g_])
                k.op("dve", lambda e: e.scalar_tensor_tensor(out=aT[:, fc, :], in0=u_[:], scalar=-6.0, in1=g_[:], op0=ALU.max, op1=ALU.mult), [g_, u_], [(aT, fc)])
            if bk == NBK - 1:
                load_wg(ei + 2)

        def emit_down(ei, bk, t0):
            nonlocal pdi
            e_ = ei % n_exp
            wd = wdn[ei % 2]
            aT = actT[(ei * NBK + bk) % 2]
            for tt in range(4):
                tl = bk * 4 + tt
                for half in range(2):
                    pp = pd[pdi % 4]
                    pdi += 1
                    for fc in range(8):
                        k.op("pe", lambda e: e.matmul(pp[:], lhsT=aT[:, fc, tt * 128:(tt + 1) * 128], rhs=wd[:, fc, half * 512:(half + 1) * 512],
                                                      start=(fc == 0), stop=(fc == 7)), [(aT, fc), (wd, fc)], [pp], inc=(fc == 7))
                    ya = yacc[:, tl, half * 512:(half + 1) * 512]
                    k.op("dve", lambda e: e.scalar_tensor_tensor(out=ya, in0=pp[:], scalar=g.wr[:, t0 + tl, e_:e_ + 1], in1=ya, op0=ALU.mult, op1=ALU.add),
                         [pp, (g.wr, t0 + tl), (yacc, (tl, half))], [(yacc, (tl, half))])
            if bk == NBK - 1:
                load_wd(ei + 2)

        for i in range(2):
            load_wg(i)
            load_wd(i)
        for ps_ in range(npass):
            t0 = ps_ * (PT // 128)
            k.dma("sp", h2s[:], h2Tv[:, :, ps_ * PT:(ps_ + 1) * PT], reads=[g.h2T.tensor], writes=[h2s])
            for tl in range(PT // 128):
                pt = pd[pdi % 4]
                pdi += 1
                k.op("pe", lambda e: e.transpose(out=pt[0:NE, 0:128], in_=g.wr[:, t0 + tl, :], identity=ident[:]), [(g.wr, t0 + tl), ident], [pt])
                k.op("act", lambda e: e.copy(out=wrT[:, tl * 128:(tl + 1) * 128], in_=pt[0:NE, 0:128]), [pt], [(wrT, tl)])
                for half in range(2):
                    pp = pd[pdi % 4]
                    pdi += 1
                    k.op("pe", lambda e: e.matmul(pp[:], lhsT=wrT[:, tl * 128:(tl + 1) * 128], rhs=bdn[:, half * 512:(half + 1) * 512], start=True, stop=True),
                         [(wrT, tl), bdn], [pp])
                    k.op("dve", lambda e: e.tensor_copy(out=yacc[:, tl, half * 512:(half + 1) * 512], in_=pp[:]), [pp], [(yacc, (tl, half))])
            units = [(ps_ * n_exp + e_, bk) for e_ in range(n_exp) for bk in range(NBK)]
            emit_gu(*units[0])
            for i, (ei_, bk_) in enumerate(units):
                if i + 1 < len(units):
                    emit_gu(*units[i + 1])
                emit_down(ei_, bk_, t0)
            for tl in range(PT // 128):
                t = t0 + tl
                xt = xa[t % 2]
                k.dma("sp", xt[:], g.x1[t * 128:(t + 1) * 128, :], reads=[(g.x1.tensor, t)], writes=[xt])
                ya = yacc[:, tl, :]
                k.op("dve", lambda e: e.tensor_tensor(out=ya, in0=ya, in1=gt2[:], op=ALU.mult), [(yacc, (tl, 0)), (yacc, (tl, 1)), gt2], [(yacc, (tl, 0)), (yacc, (tl, 1))])
                k.op("dve", lambda e: e.tensor_tensor(out=xt[:], in0=xt[:], in1=ya, op=ALU.add), [xt, (yacc, (tl, 0)), (yacc, (tl, 1))], [xt])
                rs, rskey = rms_tile(g, xt, stat, t, junk, 1.0 / D)
                k.op("dve", lambda e: e.scalar_tensor_tensor(out=xt[:], in0=xt[:], scalar=rs, in1=fg[:], op0=ALU.mult, op1=ALU.mult),
                     [xt, rskey, fg], [xt])
                k.dma("sp", g.out[t * 128:(t + 1) * 128, :], xt[:], reads=[xt], writes=[(g.out.tensor, t)])
        k.barrier()


def _consts():
    c = np.zeros((4, 128, 128), np.float32)
    c[0] = np.eye(128, dtype=np.float32)
    c[1] = 1.0
    c[2] = np.triu(np.ones((128, 128), np.float32))
    return c


def make_in_map(inp, b, names=None):
    f = lambda a: np.ascontiguousarray(np.asarray(a, dtype=np.float32))
    m = {}
    m["x"] = f(inp["x"][b])
    m["c_t"] = f(np.asarray(inp["c"][b]).reshape(8, 128).T)
    m["ada_w"] = f(inp["ada_w"][0])
    m["ada_b"] = f(inp["ada_b"][0]).reshape(1, -1)
    gv = np.stack([np.broadcast_to(np.asarray(inp[n]).reshape(-1), (128, D)) for n in ("norm1_g", "norm2_g", "final_g")])
    m["gvec"] = f(gv)
    m["w_in"] = f(inp["w_in"][0])
    m["consts"] = _consts()
    m["w_out"] = f(inp["w_out"][0])
    m["router_w"] = f(inp["router_w"][0])
    m["router_b"] = f(inp["router_b"][0]).reshape(1, -1)
    m["w_gate_up"] = f(inp["w_gate_up"][0])
    m["w_down"] = f(inp["w_down"][0])
    m["bgu_t"] = f(np.asarray(inp["b_gate_up"][0]).reshape(NE, 16, 128).transpose(2, 0, 1))
    m["b_down"] = f(inp["b_down"][0])
    m["w_q_b"] = f(inp["w_q_b"][0])
    m["w_kv_b"] = f(inp["w_kv_b"][0])
    mv = np.zeros((128, 16), np.float32)
    mv[:, 0:3] = np.asarray(inp["q_norm_g"][0]).reshape(3, 128).T
    mv[:, 3:5] = np.asarray(inp["kv_norm_g"][0]).reshape(2, 128).T
    mv[:, 5:9] = np.asarray(inp["mla_out_g"][0]).reshape(4, 128).T
    pidx = np.arange(128)
    mv[:, 9] = (np.float32(10000.0) ** (-(pidx % 16).astype(np.float32) / np.float32(16))).astype(np.float32)
    mv[:, 10] = np.where((pidx % 32) < 16, -1.0, 1.0)
    m["mla_vec"] = mv
    m["pos_bc"] = np.ascontiguousarray(np.broadcast_to(np.asarray(inp["positions"][b]).astype(np.int32).reshape(1, S), (128, S)))
    gvv = np.zeros((128, 32), np.float32)
    gvv[:, 0:8] = np.asarray(inp["A_log"][0]).reshape(1, 8)
    gvv[:, 8:16] = np.asarray(inp["dt_bias"][0]).reshape(1, 8)
    gvv[0:64, 16] = np.asarray(inp["gdn_norm_g"][0]).reshape(64)
    gvv[64:128, 16] = np.asarray(inp["gdn_norm_g"][0]).reshape(64)
    m["gdn_vec"] = gvv
    m["conv_wt"] = f(np.asarray(inp["conv_w"][0]).reshape(4, 24, 64).transpose(2, 1, 0))
    if names is not None:
        m = {n: v for n, v in m.items() if n in names}
    return m


def input_names(nc):
    return None


_CACHE = {}


def kernel(**inputs):
    if "nc" not in _CACHE:
        _CACHE["nc"] = build({})
    nc, kb = _CACHE["nc"]
    in_maps = [make_in_map(inputs, b) for b in range(8)]
    res = run_bass_kernel_spmd(nc, in_maps, core_ids=list(range(8)))
    out = np.stack([np.asarray(r["out"], dtype=np.float32) for r in res.results], axis=0)
    return out
```

```python
import contextlib
import numpy as np
import concourse.bass as bass
import concourse.mybir as mybir
from concourse.bass_utils import run_bass_kernel_spmd

F32 = mybir.dt.float32
BF16 = mybir.dt.bfloat16
AF = mybir.ActivationFunctionType
ALU = mybir.AluOpType
AX = mybir.AxisListType

S = 4096
D = 1024
NT = S // 128
NB = S // 512
IN_DIM = 2736
EPS = 1e-6
NE = 32


class KB:
    def __init__(self, nc):
        self.nc = nc
        self.es = contextlib.ExitStack()
        self.E = {}
        for name, eng in [("pe", nc.tensor), ("act", nc.scalar), ("dve", nc.vector),
                          ("pool", nc.gpsimd), ("sp", nc.sync)]:
            sem = self.es.enter_context(nc.semaphore("sem_" + name))
            self.E[name] = dict(eng=eng, sem=sem, cnt=0, seen={})
        self.sems = {n: e["sem"] for n, e in self.E.items()}
        self.ndma = 24
        self.dval = []
        for i in range(self.ndma):
            self.sems[("d", i)] = self.es.enter_context(nc.semaphore("sem_d%d" % i))
            self.dval.append(0)
        self.dptr = 0
        self.nw = 8
        for i in range(self.nw):
            self.sems[("d", self.ndma + i)] = self.es.enter_context(nc.semaphore("sem_w%d" % i))
            self.dval.append(0)
        self.wptr = 0
        self.nsw = 40
        self.swptr = 0
        for i in range(self.nsw):
            self.sems[("s", i)] = self.es.enter_context(nc.semaphore("sem_s%d" % i))
        self.state = {}
        self.n_ins = 0
        for sk, sem in self.sems.items():
            nc.gpsimd.sem_clear(sem)
        nc.all_engine_barrier()

    def sb(self, name, shape, dt, es=None):
        return (es or self.es).enter_context(self.nc.sbuf_tensor(name, list(shape), dt))

    def ps(self, name, shape, dt, es=None):
        return (es or self.es).enter_context(self.nc.psum_tensor(name, list(shape), dt))

    def _new(self):
        return {"w": None, "r": {}}

    def _sts(self, key):
        if not isinstance(key, tuple):
            key = (key, None)
        t, sub = key
        d = self.state.setdefault(id(t), {})
        if sub is None:
            if None not in d:
                d[None] = self._new()
            return list(d.values())
        if sub not in d:
            d[sub] = self._new()
        res = [d[sub]]
        if None in d:
            res.append(d[None])
        return res

    def _collect(self, ename, reads, writes):
        need = {}

        def add(sk, val):
            if sk == "pe" and ename == "pe":
                return
            if need.get(sk, 0) < val:
                need[sk] = val
        for k in reads:
            for st in self._sts(k):
                if st["w"] is not None:
                    add(*st["w"])
        for k in writes:
            for st in self._sts(k):
                if st["w"] is not None:
                    add(*st["w"])
                for sk, v in st["r"].items():
                    add(sk, v)
        return need

    def _wait(self, ename, need):
        e = self.E[ename]
        for sk, val in need.items():
            if e["seen"].get(sk, 0) < val:
                e["eng"].wait_ge(self.sems[sk], val)
                e["seen"][sk] = val

    def _update(self, reads, writes, sk, val):
        for k in reads:
            if not isinstance(k, tuple):
                k = (k, None)
            sts = self._sts(k)
            if k[1] is None:
                for st in sts:
                    st["r"][sk] = max(st["r"].get(sk, 0), val)
            else:
                sts[0]["r"][sk] = max(sts[0]["r"].get(sk, 0), val)
        for k in writes:
            if not isinstance(k, tuple):
                k = (k, None)
            sts = self._sts(k)
            if k[1] is None:
                for st in sts:
                    st["w"] = (sk, val)
                    st["r"] = {}
            else:
                sts[0]["w"] = (sk, val)
                sts[0]["r"] = {}

    def op(self, ename, fn, reads=(), writes=(), inc=True):
        e = self.E[ename]
        self._wait(ename, self._collect(ename, reads, writes))
        ins = fn(e["eng"])
        val = e["cnt"] + 1
        if inc:
            ins.then_inc(e["sem"], 1)
            e["cnt"] = val
        self._update(reads, writes, ename, val)
        self.n_ins += 1
        return ins

    def dma(self, ename, out, in_, reads=(), writes=(), wpool=False, **kw):
        e = self.E[ename]
        if ename == "pool":
            sk = ("s", self.swptr)
            self.swptr += 1
            assert self.swptr <= self.nsw, "out of single-use SW-DMA semaphores"
            self._wait(ename, self._collect(ename, reads, writes))
            ins = e["eng"].dma_start(out=out, in_=in_, **kw)
            ins.then_inc(self.sems[sk], 16)
            self._update(reads, writes, sk, 16)
            self.n_ins += 1
            return sk, 16
        if wpool:
            i = self.ndma + self.wptr
            self.wptr = (self.wptr + 1) % self.nw
        else:
            i = self.dptr
            self.dptr = (self.dptr + 1) % self.ndma
        sk = ("d", i)
        need = self._collect(ename, reads, writes)
        if self.dval[i] > 0:
            need[sk] = max(need.get(sk, 0), self.dval[i])
        self._wait(ename, need)
        ins = e["eng"].dma_start(out=out, in_=in_, **kw)
        self.dval[i] += 16
        ins.then_inc(self.sems[sk], 16)
        self._update(reads, writes, sk, self.dval[i])
        self.n_ins += 1
        return sk, self.dval[i]

    def barrier(self):
        for ename, e in self.E.items():
            for sk in self.sems:
                if isinstance(sk, tuple) and sk[0] == "s":
                    val = 16 if sk[1] < self.swptr else 0
                else:
                    val = self.E[sk]["cnt"] if sk in self.E else self.dval[sk[1]]
                if val > 0 and e["seen"].get(sk, 0) < val and sk != ename:
                    e["eng"].wait_ge(self.sems[sk], val)
                    e["seen"][sk] = val

    def finish(self, ename, keys):
        need = self._collect(ename, keys, ())
        self._wait(ename, need)


class Ctx:
    pass


class TV(tuple):
    def __new__(cls, tile, c):
        return super().__new__(cls, (tile, ("c", c)))

    def __getitem__(self, idx):
        if isinstance(idx, int):
            return tuple.__getitem__(self, idx)
        tile = tuple.__getitem__(self, 0)
        c = tuple.__getitem__(self, 1)[1]
        return tile[:, c * 64:(c + 1) * 64]


class PV(tuple):
    def __new__(cls, tile, q):
        return super().__new__(cls, (tile, ("q", q)))

    def __getitem__(self, idx):
        if isinstance(idx, int):
            return tuple.__getitem__(self, idx)
        tile = tuple.__getitem__(self, 0)
        q = tuple.__getitem__(self, 1)[1]
        p_, c_ = idx
        return tile[p_, q * 128 + c_.start:q * 128 + c_.stop]


def build(cfg):
    nc = bass.Bass("TRN2", target_bir_lowering=False)
    k = KB(nc)
    dbg = cfg.get("dbg", ())
    phases = cfg.get("phases", ("p0", "p1", "p2", "p3", "p4", "p5"))
    g = Ctx()
    g.nc, g.k, g.dbg, g.cfg = nc, k, dbg, cfg

    def din(name, shape, dt=F32):
        return nc.dram_tensor(name, list(shape), dt, kind="ExternalInput").ap()

    def dout(name, shape, dt=F32):
        return nc.dram_tensor(name, list(shape), dt, kind="ExternalOutput").ap()

    def dscr(name, shape, dt=F32):
        if name in dbg:
            return dout(name, shape, dt)
        if name in cfg.get("as_input", ()):
            return din(name, shape, dt)
        return nc.dram_tensor(name, list(shape), dt, kind="Internal").ap()
    g.din, g.dout, g.dscr = din, dout, dscr

    g.x = din("x", [S, D])
    g.consts = din("consts", [4, 128, 128])
    g.gvec = din("gvec", [3, 128, D])
    g.modscr = dscr("modscr", [6, 128, D])
    g.projT = dscr("projT", [IN_DIM, S])
    g.oT = dscr("oT", [D, S])
    g.x1 = dscr("x1", [S, D])
    g.h2T = dscr("h2T", [D, S], BF16)
    g.out = dout("out", [S, D])
    if "p5" in phases:
        g.w_gu = din("w_gate_up", [NE, D, 2 * D])
        g.w_dn = din("w_down", [NE, D, D])
        g.wgu_bf = dscr("wgu_bf", [NE, D, 2 * D], BF16)
        g.wdn_bf = dscr("wdn_bf", [NE, D, D], BF16)
    g.cast_next = 0 if "p5" in phases else 12

    def cast_some(n):
        for _ in range(n):
            i = g.cast_next
            if i >= 12:
                return
            g.cast_next += 1
            order = [("g", 0), ("g", 1), ("d", 0), ("g", 2), ("g", 3), ("d", 1), ("g", 4), ("g", 5), ("d", 2), ("g", 6), ("g", 7), ("d", 3)]
            kind, j = order[i]
            if kind == "g":
                k.dma("pool", g.wgu_bf[4 * j:4 * j + 4], g.w_gu[4 * j:4 * j + 4], writes=[(g.wgu_bf.tensor, e_) for e_ in range(4 * j, 4 * j + 4)])
            else:
                k.dma("pool", g.wdn_bf[8 * j:8 * j + 8], g.w_dn[8 * j:8 * j + 8], writes=[(g.wdn_bf.tensor, e_) for e_ in range(8 * j, 8 * j + 8)])
    g.cast_some = cast_some
    g.outputs = [g.out.tensor]
    for n in dbg:
        pass

    with k.es:
        g.ident = k.sb("ident", [128, 128], F32)
        g.ones = k.sb("ones", [128, 128], F32)
        k.dma("sp", g.ident[:], g.consts[0], writes=[g.ident])
        k.dma("sp", g.ones[:], g.consts[1], writes=[g.ones])
        g.wr = k.sb("wr", [128, NT, NE], F32)
        if "p0" in phases:
            phase0(g)
        if "p1" in phases:
            phase1(g)
        if "p2" in phases:
            phase2(g)
        if "p3" in phases:
            phase3(g)
        if "p4" in phases:
            phase4(g)
        if "p5" in phases:
            phase5(g)
        fin = list(g.outputs)
        for n in dbg:
            fin.append(getattr(g, n).tensor)
        k.finish("sp", fin)
        k.barrier()
        nc.all_engine_barrier()
    return nc, k


def phase0(g):
    nc, k = g.nc, g.k
    ada_w = g.din("ada_w", [D, 6 * D])
    ada_b = g.din("ada_b", [1, 6 * D])
    c_t = g.din("c_t", [128, 8])
    ones = g.ones
    with contextlib.ExitStack() as p0:
        mod_bc = k.sb("mod_bc", [128, 6 * D], F32, p0)
        ct = k.sb("ct", [128, 8], F32, p0)
        cact = k.sb("cact", [128, 8], F32, p0)
        cbc = k.sb("cbc", [128, 8, 128], F32, p0)
        adab = k.sb("adab", [1, 6 * D], F32, p0)
        g1 = k.sb("g1", [128, D], F32, p0)
        g2 = k.sb("g2", [128, D], F32, p0)
        awb = [k.sb("awb%d" % i, [128, 8, 512], F32, p0) for i in range(2)]
        pm = [k.ps("pm%d" % i, [128, 512], F32, p0) for i in range(2)]
        k.dma("sp", ct[:], c_t, writes=[ct])
        k.dma("sp", adab[:], ada_b, writes=[adab])
        k.dma("sp", g1[:], g.gvec[0], writes=[g1])
        k.dma("sp", g2[:], g.gvec[1], writes=[g2])
        k.op("act", lambda e: e.activation(out=cact[:], in_=ct[:], func=AF.Silu), [ct], [cact])
        for kc in range(8):
            k.op("dve", lambda e: e.tensor_scalar(out=cbc[:, kc, :], in0=ones[:], scalar1=cact[:, kc:kc + 1],
                                                  scalar2=None, op0=ALU.mult), [ones, cact], [(cbc, kc)])
        aw_v = ada_w.rearrange("(c p) n -> p c n", p=128)
        for blk in range(12):
            wb = awb[blk % 2]
            k.dma("sp", wb[:], aw_v[:, :, blk * 512:(blk + 1) * 512], writes=[wb])
            pp = pm[blk % 2]
            for kc in range(8):
                k.op("pe", lambda e: e.matmul(pp[:], lhsT=cbc[:, kc, :], rhs=wb[:, kc, :], start=(kc == 0), stop=False),
                     [(cbc, kc), wb], [pp], inc=False)
            k.op("pe", lambda e: e.matmul(pp[:], lhsT=ones[0:1, :], rhs=adab[0:1, blk * 512:(blk + 1) * 512], start=False, stop=True),
                 [ones, adab], [pp])
            k.op("act", lambda e: e.copy(out=mod_bc[:, blk * 512:(blk + 1) * 512], in_=pp[:]), [pp], [(mod_bc, blk)])
        k.op("dve", lambda e: e.scalar_tensor_tensor(out=mod_bc[:, D:2 * D], in0=mod_bc[:, D:2 * D], scalar=1.0, in1=g1[:],
                                                     op0=ALU.add, op1=ALU.mult), [mod_bc, g1], [mod_bc])
        k.op("dve", lambda e: e.scalar_tensor_tensor(out=mod_bc[:, 4 * D:5 * D], in0=mod_bc[:, 4 * D:5 * D], scalar=1.0, in1=g2[:],
                                                     op0=ALU.add, op1=ALU.mult), [mod_bc, g2], [mod_bc])
        for j in range(6):
            k.dma("sp", g.modscr[j], mod_bc[:, j * D:(j + 1) * D], reads=[mod_bc], writes=[(g.modscr.tensor, j)])
        k.barrier()


def rms_tile(g, xt, stat, col, junk, scale):
    k = g.k
    ss = stat[:, 0, col:col + 1]
    sd = stat[:, 1, col:col + 1]
    rs = stat[:, 2, col:col + 1]
    k.op("act", lambda e: e.activation(out=junk[:], in_=xt[:], func=AF.Square, accum_out=ss), [xt], [junk, (stat, (col, 0))])
    k.op("act", lambda e: e.activation(out=sd, in_=ss, func=AF.Sqrt, scale=scale, bias=EPS), [(stat, (col, 0))], [(stat, (col, 1))])
    k.op("dve", lambda e: e.reciprocal(out=rs, in_=sd), [(stat, (col, 1))], [(stat, (col, 2))])
    return rs, (stat, (col, 2))


def phase1(g):
    nc, k = g.nc, g.k
    w_in = g.din("w_in", [D, IN_DIM])
    x, ident, projT = g.x, g.ident, g.projT
    with contextlib.ExitStack() as p1:
        winb = k.sb("winb", [128, 8, IN_DIM], BF16, p1)
        wv = w_in.rearrange("(c p) n -> p c n", p=128)
        for hh in range(2):
            k.dma("pool", winb[:, :, hh * 1368:(hh + 1) * 1368], wv[:, :, hh * 1368:(hh + 1) * 1368], writes=[(winb, hh)])
        g.cast_some(12)
        s1_bc = k.sb("s1_bc", [128, D], F32, p1)
        sh1_bc = k.sb("sh1_bc", [128, D], F32, p1)
        k.dma("sp", sh1_bc[:], g.modscr[0], reads=[(g.modscr.tensor, 0)], writes=[sh1_bc])
        k.dma("sp", s1_bc[:], g.modscr[1], reads=[(g.modscr.tensor, 1)], writes=[s1_bc])
        xts = [k.sb("xt%d" % i, [128, D], F32, p1) for i in range(3)]
        junk = k.sb("junk", [128, D], BF16, p1)
        hts = [k.sb("ht%d" % i, [128, D], F32, p1) for i in range(2)]
        stat = k.sb("stat", [128, 4, NT], F32, p1)
        h1T = [k.sb("h1T%d" % i, [128, 8, 512], BF16, p1) for i in range(2)]
        stg = [k.sb("stg%d" % i, [128, 512], F32, p1) for i in range(4)]
        ptr = [k.ps("ptr%d" % i, [128, 4, 128], F32, p1) for i in range(4)]
        pmm = [k.ps("pmm%d" % i, [128, 512], F32, p1) for i in range(4)]
        nchunks = (IN_DIM + 127) // 128
        mmi = 0
        for t in range(NT):
            xt = xts[t % 3]
            ht = hts[t % 2]
            k.dma("sp", xt[:], x[t * 128:(t + 1) * 128, :], writes=[xt])
            rs, rskey = rms_tile(g, xt, stat, t, junk, 1.0 / D)
            k.op("dve", lambda e: e.scalar_tensor_tensor(out=ht[:], in0=xt[:], scalar=rs, in1=s1_bc[:], op0=ALU.mult, op1=ALU.mult),
                 [xt, rskey, s1_bc], [ht])
            k.op("dve", lambda e: e.tensor_tensor(out=ht[:], in0=ht[:], in1=sh1_bc[:], op=ALU.add), [ht, sh1_bc], [ht])
            hb = h1T[(t // 4) % 2]
            tt = t % 4
            for half in range(2):
                pt = ptr[(2 * t + half) % 4]
                for j in range(4):
                    kc = half * 4 + j
                    k.op("pe", lambda e: e.transpose(out=pt[:, j, :], in_=ht[:, kc * 128:(kc + 1) * 128], identity=ident[:]),
                         [ht, ident], [pt], inc=(j == 3))
                if half == 0:
                    k.op("act", lambda e: e.copy(out=hb[:, half * 4:half * 4 + 4, tt * 128:(tt + 1) * 128], in_=pt[:]), [pt], [(hb, (half, tt))])
                else:
                    k.op("dve", lambda e: e.tensor_copy(out=hb[:, half * 4:half * 4 + 4, tt * 128:(tt + 1) * 128], in_=pt[:]), [pt], [(hb, (half, tt))])
            if tt == 3:
                blk = t // 4
                for ci in range(nchunks):
                    c0 = ci * 128
                    cw = min(128, IN_DIM - c0)
                    pp = pmm[mmi % 4]
                    sg = stg[mmi % 4]
                    for kc in range(8):
                        k.op("pe", lambda e: e.matmul(pp[0:cw, :], lhsT=winb[:, kc, c0:c0 + cw], rhs=hb[:, kc, :],
                                                      start=(kc == 0), stop=(kc == 7)), [winb, hb], [pp], inc=(kc == 7))
                    if mmi % 2 == 0:
                        k.op("act", lambda e: e.copy(out=sg[0:cw, :], in_=pp[0:cw, :]), [pp], [sg])
                    else:
                        k.op("dve", lambda e: e.tensor_copy(out=sg[0:cw, :], in_=pp[0:cw, :]), [pp], [sg])
                    k.dma("sp", projT[c0:c0 + cw, blk * 512:(blk + 1) * 512], sg[0:cw, :], reads=[sg], writes=[(projT.tensor, (ci, blk))])
                    mmi += 1
        k.barrier()


def phase2(g):
    nc, k = g.nc, g.k
    HQ = 96
    wq_d = g.din("w_q_b", [384, 768])
    wkv_d = g.din("w_kv_b", [256, 1024])
    mvec = g.din("mla_vec", [128, 16])
    pos_d = g.din("pos_bc", [128, S], mybir.dt.int32)
    tri_d = g.consts[2]
    ident, ones, projT = g.ident, g.ones, g.projT
    heads = g.cfg.get("mla_heads", 8)
    TWO_PI = 2.0 * np.pi
    with contextlib.ExitStack() as p2:
        mv = k.sb("mv", [128, 16], F32, p2)
        k.dma("sp", mv[:], mvec, writes=[mv])
        tri = k.sb("tri", [128, 128], BF16, p2)
        trf = k.sb("trf", [128, 128], F32, p2)
        k.dma("sp", trf[:], tri_d, writes=[trf])
        k.op("dve", lambda e: e.tensor_copy(out=tri[:], in_=trf[:]), [trf], [tri])
        wqb = k.sb("wqb", [128, 3, 768], BF16, p2)
        wqr = k.sb("wqr", [128, 3, 768], BF16, p2)
        wkvb = k.sb("wkvb", [128, 2, 1024], BF16, p2)
        qlatn = k.sb("qlatn", [128, 3, S], BF16, p2)
        kvlatn = k.sb("kvlatn", [128, 2, S], BF16, p2)
        cosT = k.sb("cosT", [128, S], F32, p2)
        sinT = k.sb("sinT", [128, S], F32, p2)
        kper = k.sb("kper", [128, S], BF16, p2)
        mo = k.sb("mo", [128, NT, 512], BF16, p2)
        psA = [k.ps("psA%d" % i, [128, 512], F32, p2) for i in range(2)]
        psS = [k.ps("psS%d" % i, [128, 512], F32, p2) for i in range(2)]
        psO = [k.ps("psO%d" % i, [128, 512], F32, p2) for i in range(4)]
        with contextlib.ExitStack() as pa:
            wtmp = k.sb("wtmp", [128, 3, 1024], F32, pa)
            k.dma("sp", wtmp[:, :, 0:768], wq_d.rearrange("(c p) n -> p c n", p=128), writes=[wtmp])
            for c in range(3):
                k.op("dve", lambda e: e.tensor_scalar(out=wtmp[:, c, 0:768], in0=wtmp[:, c, 0:768], scalar1=mv[:, c:c + 1], scalar2=float(HQ ** -0.5),
                                                      op0=ALU.mult, op1=ALU.mult), [wtmp, mv], [wtmp])
            k.op("act", lambda e: e.copy(out=wqb[:], in_=wtmp[:, :, 0:768]), [wtmp], [wqb])
            k.op("dve", lambda e: e.memset(wqr[:], 0.0), [], [wqr])
            w4 = wtmp[:, :, 0:768].rearrange("p c (h d) -> p c h d", d=HQ)
            r4 = wqr[:].rearrange("p c (h d) -> p c h d", d=HQ)
            for c in range(3):
                k.op("dve", lambda e: e.tensor_scalar(out=r4[:, c, :, 64:80], in0=w4[:, c, :, 80:96], scalar1=-1.0, scalar2=None, op0=ALU.mult), [wtmp, wqr], [wqr])
                k.op("dve", lambda e: e.tensor_copy(out=r4[:, c, :, 80:96], in_=w4[:, c, :, 64:80]), [wtmp, wqr], [wqr])
            wtmp2 = k.sb("wtmp2", [128, 2, 1024], F32, pa)
            k.dma("sp", wtmp2[:], wkv_d.rearrange("(c p) n -> p c n", p=128), writes=[wtmp2])
            for c in range(2):
                k.op("dve", lambda e: e.tensor_scalar(out=wkvb[:, c, :], in0=wtmp2[:, c, :], scalar1=mv[:, 3 + c:4 + c], scalar2=None, op0=ALU.mult),
                     [wtmp2, mv], [wkvb])
            pi_ = k.sb("pi_", [128, 1024], mybir.dt.int32, pa)
            pf = k.sb("pf", [128, 1024], F32, pa)
            kf = k.sb("kf", [128, 1024], F32, pa)
            ki = k.sb("ki", [128, 1024], mybir.dt.int32, pa)
            m1 = k.sb("m1", [128, 1024], F32, pa)
            rc = k.sb("rc", [128, 1024], F32, pa)

            def wrap(r):
                k.op("dve", lambda e: e.tensor_scalar(out=m1[:], in0=r[:], scalar1=float(np.pi), scalar2=-TWO_PI, op0=ALU.is_gt, op1=ALU.mult), [r], [m1])
                k.op("dve", lambda e: e.tensor_tensor(out=r[:], in0=r[:], in1=m1[:], op=ALU.add), [r, m1], [r])
                k.op("dve", lambda e: e.tensor_scalar(out=m1[:], in0=r[:], scalar1=float(-np.pi), scalar2=TWO_PI, op0=ALU.is_lt, op1=ALU.mult), [r], [m1])
                k.op("dve", lambda e: e.tensor_tensor(out=r[:], in0=r[:], in1=m1[:], op=ALU.add), [r, m1], [r])
            for q4 in range(4):
                cs = slice(q4 * 1024, (q4 + 1) * 1024)
                k.dma("sp", pi_[:], pos_d[:, cs], writes=[pi_])
                k.op("dve", lambda e: e.tensor_copy(out=pf[:], in_=pi_[:]), [pi_], [pf])
                k.op("dve", lambda e: e.tensor_scalar(out=pf[:], in0=pf[:], scalar1=mv[:, 9:10], scalar2=None, op0=ALU.mult), [pf, mv], [pf])
                k.op("dve", lambda e: e.tensor_scalar(out=kf[:], in0=pf[:], scalar1=float(1.0 / TWO_PI), scalar2=None, op0=ALU.mult), [pf], [kf])
                k.op("dve", lambda e: e.tensor_copy(out=ki[:], in_=kf[:]), [kf], [ki])
                k.op("dve", lambda e: e.tensor_copy(out=kf[:], in_=ki[:]), [ki], [kf])
                k.op("dve", lambda e: e.scalar_tensor_tensor(out=pf[:], in0=kf[:], scalar=-TWO_PI, in1=pf[:], op0=ALU.mult, op1=ALU.add), [kf, pf], [pf])
                wrap(pf)
                k.op("act", lambda e: e.activation(out=sinT[:, cs], in_=pf[:], func=AF.Sin), [pf], [(sinT, q4)])
                k.op("dve", lambda e: e.tensor_scalar(out=rc[:], in0=pf[:], scalar1=float(np.pi / 2), scalar2=None, op0=ALU.add), [pf], [rc])
                wrap(rc)
                k.op("act", lambda e: e.activation(out=cosT[:, cs], in_=rc[:], func=AF.Sin), [rc], [(cosT, q4)])
            kp = k.sb("kp", [128, S], F32, pa)
            ksw = k.sb("ksw", [128, S], F32, pa)
            k.dma("sp", kp[64:96, :], projT[640:672, :], reads=[projT.tensor], writes=[kp])
            k.dma("sp", ksw[64:80, :], projT[656:672, :], reads=[projT.tensor], writes=[(ksw, 0)])
            k.dma("sp", ksw[80:96, :], projT[640:656, :], reads=[projT.tensor], writes=[(ksw, 1)])
            k.op("dve", lambda e: e.tensor_tensor(out=kp[64:96, :], in0=kp[64:96, :], in1=cosT[64:96, :], op=ALU.mult), [kp, cosT], [kp])
            k.op("dve", lambda e: e.scalar_tensor_tensor(out=ksw[64:96, :], in0=ksw[64:96, :], scalar=mv[64:96, 10:11], in1=sinT[64:96, :], op0=ALU.mult, op1=ALU.mult),
                 [ksw, mv, sinT], [ksw])
            k.op("dve", lambda e: e.tensor_tensor(out=kper[64:96, :], in0=kp[64:96, :], in1=ksw[64:96, :], op=ALU.add), [kp, ksw], [kper])
            k.barrier()
        with contextlib.ExitStack() as pb:
            lat = [k.sb("lat%d" % i, [128, 5, 512], F32, pb) for i in range(2)]
            sq = k.sb("sq", [128, 5, 512], F32, pb)
            rst = [k.sb("rst%d" % i, [128, 512], F32, pb) for i in range(2)]
            pv = projT[0:640, :].rearrange("(c p) s -> p c s", p=128)
            for b in range(NB):
                cs = slice(b * 512, (b + 1) * 512)
                lt = lat[b % 2]
                k.dma("sp", lt[:], pv[:, 0:5, cs], reads=[projT.tensor], writes=[lt])
                k.op("act", lambda e: e.activation(out=sq[:], in_=lt[:], func=AF.Square), [lt], [sq])
                for (c0, c1, n, dst, pp, rs_) in ((0, 3, 384.0, qlatn, psA[0], rst[0]), (3, 5, 256.0, kvlatn, psA[1], rst[1])):
                    for c in range(c0, c1):
                        k.op("pe", lambda e: e.matmul(pp[:], lhsT=ones[:], rhs=sq[:, c, :], start=(c == c0), stop=(c == c1 - 1)), [ones, sq], [pp], inc=(c == c1 - 1))
                    k.op("act", lambda e: e.activation(out=rs_[:], in_=pp[:], func=AF.Sqrt, scale=1.0 / n, bias=EPS), [pp], [rs_])
                    k.op("dve", lambda e: e.reciprocal(out=rs_[:], in_=rs_[:]), [rs_], [rs_])
                    for c in range(c0, c1):
                        k.op("dve", lambda e: e.tensor_tensor(out=dst[:, c - c0, cs], in0=lt[:, c, :], in1=rs_[:], op=ALU.mult), [lt, rs_], [(dst, (c - c0, b))])
            k.barrier()
        with contextlib.ExitStack() as pc:
            qh = [k.sb("qh%d" % i, [128, S], BF16, pc) for i in range(2)]
            kh = [k.sb("kh%d" % i, [128, S], BF16, pc) for i in range(2)]
            vh = [k.sb("vh%d" % i, [128, NT, 65], BF16, pc) for i in range(2)]
            pT = [k.sb("pT%d" % i, [128, 512], BF16, pc) for i in range(3)]
            t1 = [k.sb("t1_%d" % i, [128, 512], F32, pc) for i in range(2)]
            t2 = [k.sb("t2_%d" % i, [128, 512], F32, pc) for i in range(2)]
            rec = k.sb("rec", [128, 8, NT], F32, pc)
            for i in range(2):
                k.op("dve", lambda e: e.memset(vh[i][:, :, 64:65], 1.0), [], [vh[i]])
            pti = 0
            for h in range(heads):
                q_, k_, v_ = qh[h % 2], kh[h % 2], vh[h % 2]
                for b in range(NB):
                    cs = slice(b * 512, (b + 1) * 512)
                    p1, p2_ = psA[0], psA[1]
                    for c in range(3):
                        k.op("pe", lambda e: e.matmul(p1[0:HQ, :], lhsT=wqb[:, c, h * HQ:(h + 1) * HQ], rhs=qlatn[:, c, cs], start=(c == 0), stop=(c == 2)),
                             [wqb, (qlatn, (c, b))], [p1], inc=(c == 2))
                    k.op("act", lambda e: e.copy(out=q_[0:64, cs], in_=p1[0:64, :]), [p1], [(q_, (0, b))])
                    k.op("dve", lambda e: e.tensor_tensor(out=t1[b % 2][64:96, :], in0=p1[64:96, :], in1=cosT[64:96, cs], op=ALU.mult), [p1, cosT], [t1[b % 2]])
                    for c in range(3):
                        k.op("pe", lambda e: e.matmul(p2_[0:HQ, :], lhsT=wqr[:, c, h * HQ:(h + 1) * HQ], rhs=qlatn[:, c, cs], start=(c == 0), stop=(c == 2)),
                             [wqr, (qlatn, (c, b))], [p2_], inc=(c == 2))
                    k.op("dve", lambda e: e.tensor_tensor(out=t2[b % 2][64:96, :], in0=p2_[64:96, :], in1=sinT[64:96, cs], op=ALU.mult), [p2_, sinT], [t2[b % 2]])
                    k.op("dve", lambda e: e.tensor_tensor(out=q_[64:96, cs], in0=t1[b % 2][64:96, :], in1=t2[b % 2][64:96, :], op=ALU.add),
                         [t1[b % 2], t2[b % 2]], [(q_, (1, b))])
                    for c in range(2):
                        k.op("pe", lambda e: e.matmul(p1[0:64, :], lhsT=wkvb[:, c, h * 128:h * 128 + 64], rhs=kvlatn[:, c, cs], start=(c == 0), stop=(c == 1)),
                             [wkvb, (kvlatn, (c, b))], [p1], inc=(c == 1))
                    k.op("act", lambda e: e.copy(out=k_[0:64, cs], in_=p1[0:64, :]), [p1], [(k_, (0, b))])
                    k.op("act", lambda e: e.copy(out=k_[64:96, cs], in_=kper[64:96, cs]), [kper], [(k_, (1, b))])
                for g8 in range(NT // 8):
                    pp = psA[g8 % 2]
                    for tl in range(8):
                        t = g8 * 8 + tl
                        for c in range(2):
                            k.op("pe", lambda e: e.matmul(pp[:, tl * 64:(tl + 1) * 64], lhsT=kvlatn[:, c, t * 128:(t + 1) * 128], rhs=wkvb[:, c, h * 128 + 64:h * 128 + 128],
                                                          start=(c == 0), stop=(c == 1)), [kvlatn, wkvb], [pp], inc=(c == 1 and tl == 7))
                    k.op("act", lambda e: e.copy(out=v_[:, g8 * 8:(g8 + 1) * 8, 0:64], in_=pp[:].rearrange("p (t d) -> p t d", d=64)), [pp], [(v_, g8)])
                steps = [(qb, j) for qb in range(NB) for j in range(4 * qb + 4)]

                def emit_scores(i):
                    qb, j = steps[i]
                    c0 = max(0, j - 4 * qb) * 128
                    ps_ = psS[i % 2]
                    k.op("pe", lambda e: e.matmul(ps_[:, c0:512], lhsT=k_[0:HQ, j * 128:(j + 1) * 128], rhs=q_[0:HQ, qb * 512 + c0:(qb + 1) * 512], start=True, stop=True),
                         [k_, q_], [ps_])
                emit_scores(0)
                for i, (qb, j) in enumerate(steps):
                    if i + 1 < len(steps):
                        emit_scores(i + 1)
                    r = j - 4 * qb
                    c0 = max(0, r) * 128
                    ps_ = psS[i % 2]
                    pt_ = pT[i % 3]
                    k.op("act", lambda e: e.activation(out=pt_[:, c0:512], in_=ps_[:, c0:512], func=AF.Exp), [ps_], [pt_])
                    if r >= 0:
                        k.op("dve", lambda e: e.tensor_tensor(out=pt_[:, c0:c0 + 128], in0=pt_[:, c0:c0 + 128], in1=tri[:], op=ALU.mult), [pt_, tri], [pt_])
                    for s_ in range(c0 // 128, 4):
                        k.op("pe", lambda e: e.matmul(psO[s_][:, 0:65], lhsT=pt_[:, s_ * 128:(s_ + 1) * 128], rhs=v_[:, j, 0:65], start=(j == 0), stop=(j == 4 * qb + s_)),
                             [pt_, v_], [psO[s_]], inc=(s_ == 3))
                    if j == 4 * qb + 3:
                        for s_ in range(4):
                            t = qb * 4 + s_
                            k.op("dve", lambda e: e.reciprocal(out=rec[:, h, t:t + 1], in_=psO[s_][:, 64:65]), [psO[s_]], [(rec, (h, t))])
                            k.op("dve", lambda e: e.tensor_scalar(out=mo[:, t, h * 64:(h + 1) * 64], in0=psO[s_][:, 0:64], scalar1=rec[:, h, t:t + 1], scalar2=None, op0=ALU.mult),
                                 [psO[s_], (rec, (h, t))], [(mo, (t, h))])
            k.barrier()
        with contextlib.ExitStack() as pd_:
            stat = k.sb("stat2", [128, 4, NT], F32, pd_)
            junk = k.sb("junk2", [128, 512], BF16, pd_)
            mn = [k.sb("mn%d" % i, [128, 512], F32, pd_) for i in range(2)]
            stg = [k.sb("stg2_%d" % i, [128, 4, 512], F32, pd_) for i in range(2)]
            for t in range(NT):
                mt = mo[:, t, :]
                ss, sd, rs = stat[:, 0, t:t + 1], stat[:, 1, t:t + 1], stat[:, 2, t:t + 1]
                k.op("act", lambda e: e.activation(out=junk[:], in_=mt, func=AF.Square, accum_out=ss), [mo], [junk, (stat, (t, 0))])
                k.op("act", lambda e: e.activation(out=sd, in_=ss, func=AF.Sqrt, scale=1.0 / 512, bias=EPS), [(stat, (t, 0))], [(stat, (t, 1))])
                k.op("dve", lambda e: e.reciprocal(out=rs, in_=sd), [(stat, (t, 1))], [(stat, (t, 2))])
                m_ = mn[t % 2]
                k.op("dve", lambda e: e.tensor_scalar(out=m_[:], in0=mt, scalar1=rs, scalar2=None, op0=ALU.mult), [mo, (stat, (t, 2))], [m_])
                pp = psA[t % 2].rearrange("p (c s) -> p c s", s=128)
                for c in range(4):
                    k.op("pe", lambda e: e.transpose(out=pp[:, c, :], in_=m_[:, c * 128:(c + 1) * 128], identity=ident[:]), [m_, ident], [psA[t % 2]], inc=(c == 3))
                sg = stg[(t // 4) % 2]
                for c in range(4):
                    k.op("act", lambda e: e.activation(out=sg[:, c, (t % 4) * 128:(t % 4 + 1) * 128], in_=pp[:, c, :], func=AF.Copy, scale=mv[:, 5 + c:6 + c]),
                         [psA[t % 2], mv], [(sg, (c, t % 4))])
                if t % 4 == 3:
                    b = t // 4
                    for c in range(4):
                        k.dma("sp", g.oT[c * 128:(c + 1) * 128, b * 512:(b + 1) * 512], sg[:, c, :], reads=[sg], writes=[(g.oT.tensor, ("m", c, b))])
            k.barrier()


def phase3(g):
    nc, k = g.nc, g.k
    gvd = g.din("gdn_vec", [128, 32])
    cwd = g.din("conv_wt", [64, 24, 4])
    ident, ones, projT = g.ident, g.ones, g.projT
    heads = g.cfg.get("gdn_heads", 8)
    NCH = S // 64
    BIG = 30000.0
    P = 64
    with contextlib.ExitStack() as p3:
        gv = k.sb("gv", [128, 32], F32, p3)
        k.dma("sp", gv[:], gvd, writes=[gv])
        cw = k.sb("cw", [P, 24, 4], F32, p3)
        k.dma("sp", cw[:], cwd, writes=[cw])
        trif = k.sb("trif", [128, 128], F32, p3)
        k.dma("sp", trif[:], g.consts[2], writes=[trif])
        bigm1 = k.sb("bigm1", [P, P], F32, p3)
        negb2 = k.sb("negb2", [P, P], F32, p3)
        k.op("dve", lambda e: e.tensor_scalar(out=bigm1[:], in0=trif[0:P, 0:P], scalar1=BIG, scalar2=None, op0=ALU.mult), [trif], [bigm1])
        k.op("dve", lambda e: e.tensor_scalar(out=negb2[:], in0=trif[0:P, 0:P], scalar1=-1.0, scalar2=BIG, op0=ALU.add, op1=ALU.mult), [trif], [negb2])
        beta = k.sb("beta", [P, NCH, 8], F32, p3)
        gc = k.sb("gc", [P, NCH, 8], F32, p3)
        ngc = k.sb("ngc", [P, NCH, 8], F32, p3)
        bgam = k.sb("bgam", [P, NCH, 8], F32, p3)
        ktl = k.sb("ktl", [P, NCH, 8], F32, p3)
        cdb = k.sb("cdb", [P, NCH, 8], F32, p3)
        ps = [k.ps("pgd%d" % i, [128, 512], F32, p3) for i in range(8)]
        psq = ps
        with contextlib.ExitStack() as pa:
            ab = k.sb("ab", [16, S], F32, pa)
            k.dma("sp", ab[:], projT[2720:2736, :], reads=[projT.tensor], writes=[ab])
            abt = k.sb("abt", [P, NCH, 16], F32, pa)
            for q4 in range(2):
                pp = ps[q4]
                for j in range(32):
                    n = q4 * 32 + j
                    k.op("pe", lambda e: e.transpose(out=pp[0:P, j * 16:(j + 1) * 16], in_=ab[0:16, n * 64:(n + 1) * 64], identity=ident[0:16, 0:16]),
                         [ab, ident], [pp], inc=(j == 31))
                k.op("act", lambda e: e.copy(out=abt[:, q4 * 32:(q4 + 1) * 32, :], in_=pp[0:P, :].rearrange("p (n f) -> p n f", f=16)), [pp], [(abt, q4)])
            xa = k.sb("xa_", [P, NCH, 8], F32, pa)
            ax = k.sb("ax_", [P, NCH, 8], F32, pa)
            gg = k.sb("gg_", [P, NCH, 8], F32, pa)
            ea = k.sb("ea_", [P, 8], F32, pa)
            gt = k.sb("gt_", [P, NCH, 8], F32, pa)
            k.op("act", lambda e: e.activation(out=beta[:], in_=abt[:, :, 8:16], func=AF.Sigmoid), [abt], [beta])
            for n in range(NCH):
                k.op("dve", lambda e: e.tensor_tensor(out=xa[:, n, :], in0=abt[:, n, 0:8], in1=gv[0:P, 8:16], op=ALU.add), [abt, gv], [(xa, n)])
            k.op("act", lambda e: e.activation(out=ax[:], in_=xa[:], func=AF.Abs), [xa], [ax])
            k.op("act", lambda e: e.activation(out=ax[:], in_=ax[:], func=AF.Exp, scale=-1.0), [ax], [ax])
            k.op("act", lambda e: e.activation(out=ax[:], in_=ax[:], func=AF.Ln, bias=1.0), [ax], [ax])
            k.op("dve", lambda e: e.scalar_tensor_tensor(out=xa[:], in0=xa[:], scalar=0.0, in1=ax[:], op0=ALU.max, op1=ALU.add), [xa, ax], [xa])
            k.op("act", lambda e: e.activation(out=ea[:], in_=gv[0:P, 0:8], func=AF.Exp), [gv], [ea])
            for n in range(NCH):
                k.op("dve", lambda e: e.tensor_tensor(out=gg[:, n, :], in0=xa[:, n, :], in1=ea[:], op=ALU.mult), [xa, ea], [(gg, n)])
            k.op("dve", lambda e: e.tensor_scalar(out=gg[:], in0=gg[:], scalar1=-1.0, scalar2=None, op0=ALU.mult), [gg], [gg])
            ggf = gg[:].rearrange("p n h -> p (n h)")
            pc_, pt_ = ps[2], ps[3]
            k.op("pe", lambda e: e.matmul(pc_[0:P, :], lhsT=trif[0:P, 0:P], rhs=ggf, start=True, stop=True), [trif, gg], [pc_])
            k.op("pe", lambda e: e.matmul(pt_[0:P, :], lhsT=ones[0:P, 0:P], rhs=ggf, start=True, stop=True), [ones, gg], [pt_])
            f2 = lambda t_: t_[:].rearrange("p n h -> p (n h)")
            k.op("act", lambda e: e.copy(out=f2(gc), in_=pc_[0:P, :]), [pc_], [gc])
            k.op("act", lambda e: e.copy(out=f2(ax), in_=pt_[0:P, :]), [pt_], [ax])
            k.op("act", lambda e: e.activation(out=f2(cdb), in_=f2(ax), func=AF.Exp), [ax], [cdb])
            k.op("dve", lambda e: e.tensor_tensor(out=f2(gt), in0=f2(ax), in1=f2(gc), op=ALU.subtract), [ax, gc], [gt])
            k.op("act", lambda e: e.activation(out=f2(ktl), in_=f2(gt), func=AF.Exp), [gt], [ktl])
            k.op("dve", lambda e: e.tensor_scalar(out=f2(ngc), in0=f2(gc), scalar1=-1.0, scalar2=None, op0=ALU.mult), [gc], [ngc])
            k.op("act", lambda e: e.activation(out=f2(gt), in_=f2(gc), func=AF.Exp), [gc], [gt])
            k.op("dve", lambda e: e.tensor_tensor(out=f2(bgam), in0=f2(gt), in1=f2(beta), op=ALU.mult), [gt, beta], [bgam])
            k.barrier()
        with contextlib.ExitStack() as pb:
            xp = k.sb("xp", [P, S + 4], F32, pb)
            cv = k.sb("cv", [P, S], F32, pb)
            cvo = k.sb("cvo", [P, S], F32, pb)
            u_all = k.sb("u_all", [P, NCH, 64], F32, pb)
            wT_all = k.sb("wT_all", [P, S], BF16, pb)
            qg_all = k.sb("qg_all", [P, S], BF16, pb)
            qk_all = k.sb("qk_all", [P, NCH, 64], BF16, pb)
            kt_all = k.sb("kt_all", [P, NCH, 64], BF16, pb)
            kTb2 = [k.sb("kTb%d" % i, [P, S], BF16, pb) for i in range(2)]
            qTb2 = [k.sb("qTb%d" % i, [P, S], BF16, pb) for i in range(2)]
            vTb2 = [k.sb("vTb%d" % i, [P, S], BF16, pb) for i in range(2)]
            identb = k.sb("identb", [P, P], BF16, pb)
            k.op("act", lambda e: e.copy(out=identb[:], in_=ident[0:P, 0:P]), [ident], [identb])
            o_all = u_all
            X_w = k.sb("X_w", [P, 512], F32, pb)
            gb_w = k.sb("gb_w", [P, 512], F32, pb)
            d1_w = k.sb("d1_w", [P, 512], F32, pb)
            dt_w = k.sb("dt_w", [P, 512], F32, pb)
            identw = k.sb("identw", [P, 512], BF16, pb)
            for c in range(8):
                k.op("act", lambda e: e.copy(out=identw[:, c * 64:(c + 1) * 64], in_=ident[0:P, 0:P]), [ident], [(identw, c)])
            GW = 8
            P_w = [[k.sb("Pw%d_%d" % (i, j), [P, GW * 64], BF16, pb) for j in range(2)] for i in range(2)]
            Q_w = [[k.sb("Qw%d_%d" % (i, j), [P, GW * 64], BF16, pb) for j in range(2)] for i in range(2)]
            W_w = [[k.sb("Ww%d_%d" % (i, j), [P, GW * 64], BF16, pb) for j in range(2)] for i in range(2)]
            kbg_w = [k.sb("kbgw%d" % i, [P, 512], BF16, pb) for i in range(2)]
            bv_w = [k.sb("bvw%d" % i, [P, 512], BF16, pb) for i in range(2)]
            Sst = [k.sb("Sst%d" % i, [P, P], F32, pb) for i in range(2)]
            Sbf = [k.sb("Sbf%d" % i, [P, P], BF16, pb) for i in range(2)]
            vn = [k.sb("vn%d" % i, [P, P], BF16, pb) for i in range(2)]
            rsd = k.sb("rsd", [P, 4, NCH], F32, pb)
            k.op("dve", lambda e: e.memset(xp[:, 0:4], 0.0), [], [(xp, "pad")])
            epsb = k.sb("epsb", [P, 1], F32, pb)
            k.op("dve", lambda e: e.memset(epsb[:], EPS), [], [epsb])
            psi = 0

            psf = 0

            psd = 0

            def nps(kind):
                nonlocal psi, psd
                if kind == "act":
                    psi += 1
                    return psq[psi % 4]
                psd += 1
                return psq[4 + psd % 4]

            def npsf(kind="act"):
                return nps(kind)
            def conv_gen(hh):
                for ti, (dstb, row0) in enumerate(((qTb2[hh % 2], 672), (kTb2[hh % 2], 1184), (vTb2[hh % 2], 1696))):
                    k.dma("sp", xp[:, 4:S + 4], projT[row0 + hh * 64:row0 + (hh + 1) * 64, :], reads=[projT.tensor], writes=[(xp, "x")])
                    ci = ti * 8 + hh
                    k.op("dve", lambda e: e.tensor_scalar(out=cv[:], in0=xp[:, 1:S + 1], scalar1=cw[:, ci, 0:1], scalar2=None, op0=ALU.mult), [xp, cw], [cv])
                    yield
                    for j in range(1, 4):
                        k.op("dve", lambda e: e.scalar_tensor_tensor(out=cv[:], in0=xp[:, 1 + j:S + 1 + j], scalar=cw[:, ci, j:j + 1], in1=cv[:], op0=ALU.mult, op1=ALU.add),
                             [xp, cw, cv], [cv])
                        yield
                    if ti == 2:
                        k.op("act", lambda e: e.activation(out=dstb[:], in_=cv[:], func=AF.Silu), [cv], [dstb])
                        yield
                        continue
                    k.op("act", lambda e: e.activation(out=cvo[:], in_=cv[:], func=AF.Silu), [cv], [cvo])
                    k.op("dve", lambda e: e.tensor_tensor(out=cv[:], in0=cvo[:], in1=cvo[:], op=ALU.mult), [cvo], [cv])
                    yield
                    for b in range(NB):
                        cs = slice(b * 512, (b + 1) * 512)
                        pp = npsf()
                        k.op("pe", lambda e: e.matmul(pp[0:P, :], lhsT=ones[0:P, 0:P], rhs=cv[:, cs], start=True, stop=True), [ones, cv], [pp])
                        k.op("act", lambda e: e.activation(out=cv[:, cs], in_=pp[0:P, :], func=AF.Ln, bias=epsb[0:P, 0:1]), [pp, epsb], [(cv, b)])
                        k.op("act", lambda e: e.activation(out=cv[:, cs], in_=cv[:, cs], func=AF.Exp, scale=-0.5), [(cv, b)], [(cv, b)])
                        sc_ = 0.125 if ti == 0 else 1.0
                        k.op("dve", lambda e: e.scalar_tensor_tensor(out=dstb[:, cs], in0=cvo[:, cs], scalar=sc_, in1=cv[:, cs], op0=ALU.mult, op1=ALU.mult),
                             [cvo, (cv, b)], [(dstb, b)])
                        yield

            ei = 0
            for h in range(heads):
                if h == 0:
                    for _ in conv_gen(0):
                        pass
                kTb, qTb, vTb = kTb2[h % 2], qTb2[h % 2], vTb2[h % 2]
                nxt_conv = conv_gen(h + 1) if h + 1 < heads else iter(())
                G = 8
                St = Sst[0]
                k.op("dve", lambda e: e.memset(St[:], 0.0), [], [St])
                k.op("dve", lambda e: e.memset(Sbf[0][:], 0.0), [], [Sbf[0]])

                def scan_step(n, St):
                    cs = slice(n * 64, (n + 1) * 64)
                    p1_, p2_, p3_ = nps("dve"), nps("act"), nps("dve")
                    v_ = vn[n % 2]
                    Sb = Sbf[n % 2]
                    k.op("pe", lambda e: e.matmul(p1_[0:P, 0:P], lhsT=wT_all[:, cs], rhs=Sb[:], start=True, stop=True), [(wT_all, n), Sb], [p1_])
                    k.op("dve", lambda e: e.tensor_tensor(out=v_[:], in0=u_all[:, n, :], in1=p1_[0:P, 0:P], op=ALU.subtract), [(u_all, n), p1_], [v_])
                    k.op("pe", lambda e: e.matmul(p2_[0:P, 0:P], lhsT=qg_all[:, cs], rhs=Sb[:], start=True, stop=False), [(qg_all, n), Sb], [p2_], inc=False)
                    k.op("pe", lambda e: e.matmul(p2_[0:P, 0:P], lhsT=qk_all[:, n, :], rhs=v_[:], start=False, stop=True), [(qk_all, n), v_], [p2_])
                    k.op("pe", lambda e: e.matmul(p3_[0:P, 0:P], lhsT=kt_all[:, n, :], rhs=v_[:], start=True, stop=True), [(kt_all, n), v_], [p3_])
                    Sn = Sst[(n + 1) % 2]
                    k.op("dve", lambda e: e.scalar_tensor_tensor(out=Sn[:], in0=St[:], scalar=cdb[:, n, h:h + 1], in1=p3_[0:P, 0:P], op0=ALU.mult, op1=ALU.add),
                         [St, cdb, p3_], [Sn])
                    k.op("act", lambda e: e.copy(out=Sbf[(n + 1) % 2][:], in_=Sn[:]), [Sn], [Sbf[(n + 1) % 2]])
                    k.op("act", lambda e: e.copy(out=o_all[:, n, :], in_=p2_[0:P, 0:P]), [p2_], [(o_all, n)])
                    return Sn

                def stage_a(gi, par):
                    n0 = gi * G
                    for c in range(G):
                        n = n0 + c
                        k.op("dve", lambda e: e.tensor_scalar(out=X_w[:, c * 64:(c + 1) * 64], in0=ident[0:P, 0:P], scalar1=gc[:, n, h:h + 1], scalar2=None, op0=ALU.mult),
                             [ident, gc], [(X_w, ("c", c))])
                    bR, bR2, bR3 = nps("act"), nps("act"), nps("act")
                    for c in range(G):
                        sl_ = slice(c * 64, (c + 1) * 64)
                        k.op("pe", lambda e: e.matmul(bR[0:P, sl_], lhsT=ones[0:P, 0:P], rhs=X_w[:, sl_], start=True, stop=False), [ones, (X_w, ("c", c))], [bR], inc=False)
                        k.op("pe", lambda e: e.matmul(bR[0:P, sl_], lhsT=ident[0:P, 0:P], rhs=bigm1[:], start=False, stop=True), [ident, bigm1], [bR], inc=(c == G - 1))
                    for c in range(G):
                        sl_ = slice(c * 64, (c + 1) * 64)
                        k.op("pe", lambda e: e.matmul(bR2[0:P, sl_], lhsT=ones[0:P, 0:P], rhs=X_w[:, sl_], start=True, stop=False), [ones, (X_w, ("c", c))], [bR2], inc=False)
                        k.op("pe", lambda e: e.matmul(bR2[0:P, sl_], lhsT=ident[0:P, 0:P], rhs=negb2[:], start=False, stop=True), [ident, negb2], [bR2], inc=(c == G - 1))
                    for c in range(G):
                        sl_ = slice(c * 64, (c + 1) * 64)
                        k.op("pe", lambda e: e.matmul(bR3[0:P, sl_], lhsT=ones[0:P, 0:P], rhs=X_w[:, sl_], start=True, stop=True), [ones, (X_w, ("c", c))], [bR3], inc=(c == G - 1))
                    for c in range(G):
                        n = n0 + c
                        sl_ = slice(c * 64, (c + 1) * 64)
                        k.op("act", lambda e: e.activation(out=d1_w[:, sl_], in_=bR[0:P, sl_], func=AF.Exp, scale=-1.0, bias=gc[:, n, h:h + 1]), [bR, gc], [(d1_w, ("c", c))])
                        k.op("act", lambda e: e.activation(out=dt_w[:, sl_], in_=bR2[0:P, sl_], func=AF.Exp, scale=1.0, bias=ngc[:, n, h:h + 1]), [bR2, ngc], [(dt_w, ("c", c))])
                    k.op("act", lambda e: e.activation(out=gb_w[:], in_=bR3[0:P, 0:G * 64], func=AF.Exp), [bR3], [gb_w])
                    bK, bQ = nps("dve"), nps("dve")
                    for c in range(G):
                        cs = slice((n0 + c) * 64, (n0 + c + 1) * 64)
                        k.op("pe", lambda e: e.matmul(bK[0:P, c * 64:(c + 1) * 64], lhsT=kTb[:, cs], rhs=kTb[:, cs], start=True, stop=True), [kTb], [bK], inc=(c == G - 1))
                    for c in range(G):
                        cs = slice((n0 + c) * 64, (n0 + c + 1) * 64)
                        k.op("pe", lambda e: e.matmul(bQ[0:P, c * 64:(c + 1) * 64], lhsT=kTb[:, cs], rhs=qTb[:, cs], start=True, stop=True), [kTb, qTb], [bQ], inc=(c == G - 1))
                    for c in range(G):
                        n = n0 + c
                        sl_ = slice(c * 64, (c + 1) * 64)
                        A = TV(P_w[par][0], c)
                        k.op("dve", lambda e: e.scalar_tensor_tensor(out=A[:], in0=bK[0:P, sl_], scalar=beta[:, n, h:h + 1], in1=d1_w[:, sl_], op0=ALU.mult, op1=ALU.mult),
                             [bK, beta, (d1_w, ("c", c))], [A])
                    k.op("dve", lambda e: e.tensor_tensor(out=qk_all[:, n0:n0 + G, :], in0=bQ[0:P, 0:G * 64].rearrange("p (n d) -> p n d", d=64),
                                                          in1=dt_w[:].rearrange("p (n d) -> p n d", d=64), op=ALU.mult), [bQ, dt_w], [(qk_all, n0 + c) for c in range(G)])
                    k.op("dve", lambda e: e.tensor_tensor(out=qg_all[:, n0 * 64:(n0 + G) * 64], in0=qTb[:, n0 * 64:(n0 + G) * 64], in1=gb_w[:], op=ALU.mult),
                         [qTb, gb_w], [(qg_all, n0 + c) for c in range(G)])
                    bT = nps("act")
                    for c in range(G):
                        A = TV(P_w[par][0], c)
                        k.op("pe", lambda e: e.matmul(bT[0:P, c * 64:(c + 1) * 64], lhsT=A[:], rhs=identb[:], start=True, stop=True), [A, identb], [bT], inc=(c == G - 1))
                    k.op("act", lambda e: e.copy(out=Q_w[par][0][:], in_=bT[0:P, 0:G * 64]), [bT], [Q_w[par][0]])
                    k.op("dve", lambda e: e.tensor_tensor(out=W_w[par][0][:], in0=identw[:], in1=Q_w[par][0][:], op=ALU.subtract), [identw, Q_w[par][0]], [W_w[par][0]])

                def stage_b(par, lv):
                    a, b_ = lv % 2, (lv + 1) % 2
                    bankP, bankW = nps("act"), nps("dve")
                    for c in range(G):
                        Pk, Qk = TV(P_w[par][a], c), TV(Q_w[par][a], c)
                        k.op("pe", lambda e: e.matmul(bankP[0:P, c * 64:(c + 1) * 64], lhsT=Qk[:], rhs=Pk[:], start=True, stop=True), [Qk, Pk], [bankP], inc=(c == G - 1))
                    if lv < 4:
                        bankQ = nps("dve")
                        for c in range(G):
                            Pk, Qk = TV(P_w[par][a], c), TV(Q_w[par][a], c)
                            k.op("pe", lambda e: e.matmul(bankQ[0:P, c * 64:(c + 1) * 64], lhsT=Pk[:], rhs=Qk[:], start=True, stop=True), [Pk, Qk], [bankQ], inc=(c == G - 1))
                    k.op("act", lambda e: e.copy(out=P_w[par][b_][:], in_=bankP[0:P, 0:G * 64]), [bankP], [P_w[par][b_]])
                    if lv < 4:
                        k.op("dve", lambda e: e.tensor_copy(out=Q_w[par][b_][:], in_=bankQ[0:P, 0:G * 64]), [bankQ], [Q_w[par][b_]])
                    for c in range(G):
                        Pn, W = TV(P_w[par][b_], c), TV(W_w[par][a], c)
                        k.op("pe", lambda e: e.matmul(bankW[0:P, c * 64:(c + 1) * 64], lhsT=Pn[:], rhs=W[:], start=True, stop=True), [Pn, W], [bankW], inc=(c == G - 1))
                    k.op("dve", lambda e: e.tensor_tensor(out=W_w[par][b_][:], in0=bankW[0:P, 0:G * 64], in1=W_w[par][a][:], op=ALU.add), [bankW, W_w[par][a]], [W_w[par][b_]])

                def stage_c(gi, par):
                    bk_, bv2, bu, bw = nps("dve"), nps("act"), nps("act"), nps("dve")
                    for c in range(G):
                        n = gi * G + c
                        cs = slice(n * 64, (n + 1) * 64)
                        k.op("pe", lambda e: e.matmul(bk_[0:P, c * 64:(c + 1) * 64], lhsT=kTb[:, cs], rhs=identb[:], start=True, stop=True), [kTb, identb], [bk_], inc=(c == G - 1))
                    for c in range(G):
                        n = gi * G + c
                        cs = slice(n * 64, (n + 1) * 64)
                        k.op("pe", lambda e: e.matmul(bv2[0:P, c * 64:(c + 1) * 64], lhsT=vTb[:, cs], rhs=identb[:], start=True, stop=True), [vTb, identb], [bv2], inc=(c == G - 1))
                    for c in range(G):
                        n = gi * G + c
                        sl_ = slice(c * 64, (c + 1) * 64)
                        k.op("dve", lambda e: e.tensor_scalar(out=kbg_w[par][:, sl_], in0=bk_[0:P, sl_], scalar1=bgam[:, n, h:h + 1], scalar2=None, op0=ALU.mult), [bk_, bgam], [(kbg_w[par], ("c", c))])
                        k.op("dve", lambda e: e.tensor_scalar(out=kt_all[:, n, :], in0=bk_[0:P, sl_], scalar1=ktl[:, n, h:h + 1], scalar2=None, op0=ALU.mult), [bk_, ktl], [(kt_all, n)])
                        k.op("act", lambda e: e.activation(out=bv_w[par][:, sl_], in_=bv2[0:P, sl_], func=AF.Copy, scale=beta[:, n, h:h + 1]), [bv2, beta], [(bv_w[par], ("c", c))])
                    for c in range(G):
                        W = TV(W_w[par][1], c)
                        k.op("pe", lambda e: e.matmul(bu[0:P, c * 64:(c + 1) * 64], lhsT=W[:], rhs=bv_w[par][:, c * 64:(c + 1) * 64], start=True, stop=True),
                             [W, (bv_w[par], ("c", c))], [bu], inc=(c == G - 1))
                    for c in range(G):
                        W = TV(W_w[par][1], c)
                        k.op("pe", lambda e: e.matmul(bw[0:P, c * 64:(c + 1) * 64], lhsT=kbg_w[par][:, c * 64:(c + 1) * 64], rhs=W[:], start=True, stop=True),
                             [(kbg_w[par], ("c", c)), W], [bw], inc=(c == G - 1))
                    n0 = gi * G
                    k.op("act", lambda e: e.copy(out=u_all[:, n0:n0 + G, :], in_=bu[0:P, 0:G * 64].rearrange("p (n d) -> p n d", d=64)), [bu], [(u_all, n0 + c) for c in range(G)])
                    k.op("dve", lambda e: e.tensor_copy(out=wT_all[:, n0 * 64:(n0 + G) * 64], in_=bw[0:P, 0:G * 64]), [bw], [(wT_all, n0 + c) for c in range(G)])

                ngrp = NCH // G
                for gi in range(ngrp + 1):
                    for _ in range(7):
                        next(nxt_conv, None)
                    base = (gi % 2) * G
                    if gi < ngrp:
                        stage_a(gi, gi % 2)
                    for lv in range(5):
                        if gi < ngrp:
                            stage_b(gi % 2, lv)
                        if gi >= 1:
                            for sj in range(lv * G // 5, (lv + 1) * G // 5):
                                St = scan_step((gi - 1) * G + sj, St)
                    if gi < ngrp:
                        stage_c(gi, gi % 2)
                k.op("dve", lambda e: e.tensor_tensor(out=cv[:].rearrange("p (n d) -> p n d", d=64), in0=o_all[:], in1=o_all[:], op=ALU.mult), [o_all], [cv])
                k.op("dve", lambda e: e.tensor_reduce(out=rsd[:, 0, :], in_=cv[:].rearrange("p (n d) -> p n d", d=64), axis=AX.X, op=ALU.add), [cv], [(rsd, 0)])
                k.op("act", lambda e: e.activation(out=rsd[:, 1, :], in_=rsd[:, 0, :], func=AF.Sqrt, scale=1.0 / 64, bias=EPS), [(rsd, 0)], [(rsd, 1)])
                k.op("dve", lambda e: e.reciprocal(out=rsd[:, 2, :], in_=rsd[:, 1, :]), [(rsd, 1)], [(rsd, 2)])
                k.dma("sp", xp[:, 4:S + 4], projT[2208 + h * 64:2208 + (h + 1) * 64, :], reads=[projT.tensor], writes=[(xp, "x")])
                k.op("act", lambda e: e.activation(out=xp[:, 4:S + 4], in_=xp[:, 4:S + 4], func=AF.Silu), [(xp, "x")], [(xp, "x")])
                for b in range(NB):
                    pp = npsf("dve")
                    for j in range(8):
                        n = b * 8 + j
                        k.op("dve", lambda e: e.tensor_scalar(out=o_all[:, n, :], in0=o_all[:, n, :], scalar1=rsd[:, 2, n:n + 1], scalar2=None, op0=ALU.mult),
                             [(o_all, n), (rsd, 2)], [(o_all, n)])
                        k.op("pe", lambda e: e.transpose(out=pp[0:P, j * 64:(j + 1) * 64], in_=o_all[:, n, :], identity=ident[0:P, 0:P]), [(o_all, n), ident], [pp], inc=(j == 7))
                    cs = slice(b * 512, (b + 1) * 512)
                    k.op("dve", lambda e: e.scalar_tensor_tensor(out=cv[:, cs], in0=pp[0:P, :], scalar=gv[0:P, 16:17], in1=xp[:, 4 + b * 512:4 + (b + 1) * 512], op0=ALU.mult, op1=ALU.mult),
                         [pp, gv, (xp, "x")], [(cv, b)])
                k.dma("sp", g.oT[512 + h * 64:512 + (h + 1) * 64, :], cv[:], reads=[cv], writes=[(g.oT.tensor, ("g", h))])
            k.barrier()


def phase4(g):
    nc, k = g.nc, g.k
    w_out = g.din("w_out", [D, D])
    router_w = g.din("router_w", [D, NE])
    router_b = g.din("router_b", [1, NE])
    x, ident, ones = g.x, g.ident, g.ones
    with contextlib.ExitStack() as p4:
        woutb = k.sb("woutb", [128, 8, D], BF16, p4)
        wov = w_out.rearrange("(c p) n -> p c n", p=128)
        k.dma("pool", woutb[:], wov, writes=[woutb])
        rw = k.sb("rw", [128, 8, NE], F32, p4)
        k.dma("sp", rw[:], router_w.rearrange("(c p) n -> p c n", p=128), writes=[rw])
        rb = k.sb("rb", [1, NE], F32, p4)
        k.dma("sp", rb[:], router_b, writes=[rb])
        bcs = {}
        for nm, slot in (("gt1", 2), ("sh2", 3), ("s2", 4)):
            bcs[nm] = k.sb(nm + "_bc", [128, D], F32, p4)
            k.dma("sp", bcs[nm][:], g.modscr[slot], reads=[(g.modscr.tensor, slot)], writes=[bcs[nm]])
        oTb = [k.sb("oTb%d" % i, [128, 8, 512], BF16, p4) for i in range(2)]
        xts = [k.sb("xq%d" % i, [128, D], F32, p4) for i in range(2)]
        x1s = [k.sb("x1s%d" % i, [128, D], F32, p4) for i in range(2)]
        hts = [k.sb("h2t%d" % i, [128, D], F32, p4) for i in range(2)]
        junk = k.sb("junk4", [128, D], BF16, p4)
        stat = k.sb("stat4", [128, 4, NT], F32, p4)
        h2Tf = [k.sb("h2Tf%d" % i, [128, 8, 128], F32, p4) for i in range(2)]
        h2Tb = [k.sb("h2Tb%d" % i, [128, 8, 512], BF16, p4) for i in range(2)]
        lg = k.sb("lg", [128, NT, NE], F32, p4)
        m8 = k.sb("m8", [128, NT, 8], F32, p4)
        rt = k.sb("rt", [128, 4, NT], F32, p4)
        msk = [k.sb("msk%d" % i, [128, NE], F32, p4) for i in range(2)]
        ex = [k.sb("ex%d" % i, [128, NE], F32, p4) for i in range(2)]
        pmx = [k.ps("pmx%d" % i, [128, 512], F32, p4) for i in range(4)]
        ptr = [k.ps("ptr4_%d" % i, [128, 4, 128], F32, p4) for i in range(2)]
        plg = [k.ps("plg%d" % i, [128, NE], F32, p4) for i in range(2)]
        oTv = g.oT.rearrange("(c p) s -> p c s", p=128)
        h2Tv = g.h2T.rearrange("(c p) s -> p c s", p=128)
        for blk in range(NB):
            ob = oTb[blk % 2]
            k.dma("pool", ob[:], oTv[:, :, blk * 512:(blk + 1) * 512], reads=[g.oT.tensor], writes=[ob])
            for tt in range(4):
                t = blk * 4 + tt
                xt = xts[t % 2]
                x1t = x1s[t % 2]
                ht = hts[t % 2]
                k.dma("sp", xt[:], x[t * 128:(t + 1) * 128, :], writes=[xt])
                for half in range(2):
                    pp = pmx[(2 * t + half) % 4]
                    for kc in range(8):
                        k.op("pe", lambda e: e.matmul(pp[:], lhsT=ob[:, kc, tt * 128:(tt + 1) * 128], rhs=woutb[:, kc, half * 512:(half + 1) * 512],
                                                      start=(kc == 0), stop=(kc == 7)), [ob, woutb], [pp], inc=(kc == 7))
                    hs = slice(half * 512, (half + 1) * 512)
                    k.op("dve", lambda e: e.tensor_tensor(out=x1t[:, hs], in0=pp[:], in1=bcs["gt1"][:, hs], op=ALU.mult), [pp, bcs["gt1"]], [(x1t, half)])
                    k.op("dve", lambda e: e.tensor_tensor(out=x1t[:, hs], in0=x1t[:, hs], in1=xt[:, hs], op=ALU.add), [(x1t, half), xt], [(x1t, half)])
                k.dma("sp", g.x1[t * 128:(t + 1) * 128, :], x1t[:], reads=[x1t], writes=[(g.x1.tensor, t)])
                if g.cfg.get("p4_level", 9) < 0.2:
                    continue
                rs, rskey = rms_tile(g, x1t, stat, t, junk, 1.0 / D)
                if g.cfg.get("p4_level", 9) < 0.27:
                    continue
                k.op("dve", lambda e: e.scalar_tensor_tensor(out=ht[:], in0=x1t[:], scalar=rs, in1=bcs["s2"][:], op0=ALU.mult, op1=ALU.mult),
                     [x1t, rskey, bcs["s2"]], [ht])
                if g.cfg.get("p4_level", 9) < 0.29:
                    continue
                k.op("dve", lambda e: e.tensor_tensor(out=ht[:], in0=ht[:], in1=bcs["sh2"][:], op=ALU.add), [ht, bcs["sh2"]], [ht])
                if g.cfg.get("p4_level", 9) < 0.5:
                    continue
                hf = h2Tf[t % 2]
                hb = h2Tb[blk % 2]
                for half in range(2):
                    pt = ptr[half]
                    for j in range(4):
                        kc = half * 4 + j
                        k.op("pe", lambda e: e.transpose(out=pt[:, j, :], in_=ht[:, kc * 128:(kc + 1) * 128], identity=ident[:]),
                             [ht, ident], [pt], inc=(j == 3))
                    k.op("act", lambda e: e.copy(out=hf[:, half * 4:half * 4 + 4, :], in_=pt[:]), [pt], [(hf, half)])
                    k.op("dve", lambda e: e.tensor_copy(out=hb[:, half * 4:half * 4 + 4, tt * 128:(tt + 1) * 128], in_=hf[:, half * 4:half * 4 + 4, :]),
                         [(hf, half)], [(hb, (half, tt))])
                if tt == 3 and g.cfg.get("p4_level", 9) >= 0.7:
                    for kc in range(8):
                        k.dma("sp", g.h2T[kc * 128:(kc + 1) * 128, blk * 512:(blk + 1) * 512], hb[:, kc, :], reads=[hb], writes=[(g.h2T.tensor, (blk, kc))])
                if g.cfg.get("p4_level", 9) < 2:
                    continue
                pl = plg[t % 2]
                for kc in range(8):
                    k.op("pe", lambda e: e.matmul(pl[:], lhsT=hf[:, kc, :], rhs=rw[:, kc, :], start=(kc == 0), stop=False), [hf, rw], [pl], inc=False)
                k.op("pe", lambda e: e.matmul(pl[:], lhsT=ones[0:1, :], rhs=rb[0:1, :], start=False, stop=True), [ones, rb], [pl])
                lgt = lg[:, t, :]
                k.op("act", lambda e: e.copy(out=lgt, in_=pl[:]), [pl], [(lg, t)])
                k.op("dve", lambda e: e.max(out=m8[:, t, :], in_=lgt), [(lg, t)], [(m8, t)])
                mk = msk[t % 2]
                et = ex[t % 2]
                k.op("dve", lambda e: e.tensor_scalar(out=mk[:], in0=lgt, scalar1=m8[:, t, 3:4], scalar2=None, op0=ALU.is_ge), [(lg, t), (m8, t)], [mk])
                k.op("dve", lambda e: e.tensor_scalar(out=rt[:, 0, t:t + 1], in0=m8[:, t, 0:1], scalar1=-1.0, scalar2=None, op0=ALU.mult), [(m8, t)], [(rt, (t, 0))])
                k.op("act", lambda e: e.activation(out=et[:], in_=lgt, func=AF.Exp, bias=rt[:, 0, t:t + 1], scale=1.0), [(lg, t), (rt, (t, 0))], [et])
                k.op("dve", lambda e: e.tensor_tensor(out=et[:], in0=et[:], in1=mk[:], op=ALU.mult), [et, mk], [et])
                k.op("dve", lambda e: e.reduce_sum(out=rt[:, 1, t:t + 1], in_=et[:], axis=AX.X), [et], [(rt, (t, 1))])
                k.op("dve", lambda e: e.reciprocal(out=rt[:, 2, t:t + 1], in_=rt[:, 1, t:t + 1]), [(rt, (t, 1))], [(rt, (t, 2))])
                k.op("dve", lambda e: e.tensor_scalar(out=g.wr[:, t, :], in0=et[:], scalar1=rt[:, 2, t:t + 1], scalar2=None, op0=ALU.mult),
                     [et, (rt, (t, 2))], [(g.wr, t)])
        k.barrier()


def phase5(g):
    nc, k = g.nc, g.k
    cfg = g.cfg
    n_exp = cfg.get("n_exp", NE)
    g.cast_some(12)
    bgu_t = g.din("bgu_t", [128, NE, 16])
    b_dn = g.din("b_down", [NE, D])
    ident = g.ident
    PT = 1024
    npass = S // PT
    with contextlib.ExitStack() as p5:
        gt2 = k.sb("gt2_bc", [128, D], F32, p5)
        fg = k.sb("fg_bc", [128, D], F32, p5)
        k.dma("sp", gt2[:], g.modscr[5], reads=[(g.modscr.tensor, 5)], writes=[gt2])
        k.dma("sp", fg[:], g.gvec[2], writes=[fg])
        bgu = k.sb("bgu", [128, NE, 16], F32, p5)
        k.dma("sp", bgu[:], bgu_t, writes=[bgu])
        bgu1 = k.sb("bgu1", [128, NE, 16], F32, p5)
        k.op("dve", lambda e: e.tensor_scalar(out=bgu1[:], in0=bgu[:], scalar1=1.0, scalar2=None, op0=ALU.add), [bgu], [bgu1])
        bdn = k.sb("bdn", [NE, D], F32, p5)
        k.dma("sp", bdn[:], b_dn, writes=[bdn])
        h2s = k.sb("h2s", [128, 8, PT], BF16, p5)
        yacc = k.sb("yacc", [128, PT // 128, D], F32, p5)
        wgu = [k.sb("wgu%d" % i, [128, 8, 2 * D], BF16, p5) for i in range(2)]
        wdn = [k.sb("wdn%d" % i, [128, 8, D], BF16, p5) for i in range(2)]
        actT = [k.sb("actT%d" % i, [128, 8, 512], BF16, p5) for i in range(2)]
        gS = [k.sb("gS%d" % i, [128, 512], F32, p5) for i in range(2)]
        sS = [k.sb("sS%d" % i, [128, 512], F32, p5) for i in range(2)]
        uS = [k.sb("uS%d" % i, [128, 512], F32, p5) for i in range(2)]
        wrT = k.sb("wrT", [NE, PT], F32, p5)
        xa = [k.sb("xa%d" % i, [128, D], F32, p5) for i in range(2)]
        junk = k.sb("junk5", [128, D], BF16, p5)
        stat = k.sb("stat5", [128, 4, NT], F32, p5)
        pg = [k.ps("pg%d" % i, [128, 512], F32, p5) for i in range(2)]
        pu = [k.ps("pu%d" % i, [128, 512], F32, p5) for i in range(2)]
        pd = [k.ps("pd%d" % i, [128, 512], F32, p5) for i in range(4)]
        h2Tv = g.h2T.rearrange("(c p) s -> p c s", p=128)
        wguv = g.wgu_bf.rearrange("e (c p) n -> e p c n", p=128)
        wdnv = g.wdn_bf.rearrange("e (c p) n -> e p c n", p=128)

        n_tot = npass * n_exp

        def load_wg(ei):
            if ei >= n_tot:
                return
            wg = wgu[ei % 2]
            e_ = ei % n_exp
            for kc in range(0, 8, 4):
                k.dma("sp", wg[:, kc:kc + 4, :], wguv[e_, :, kc:kc + 4, :], reads=[(g.wgu_bf.tensor, e_)], writes=[(wg, kc + j) for j in range(4)])

        def load_wd(ei):
            if ei >= n_tot:
                return
            wd = wdn[ei % 2]
            e_ = ei % n_exp
            k.dma("sp", wd[:], wdnv[e_], reads=[(g.wdn_bf.tensor, e_)], writes=[(wd, kc) for kc in range(8)])

        fci = 0
        pdi = 0
        NBK = PT // 512

        def emit_gu(ei, bk):
            nonlocal fci
            e_ = ei % n_exp
            wg = wgu[ei % 2]
            aT = actT[(ei * NBK + bk) % 2]
            rhs_cols = slice(bk * 512, (bk + 1) * 512)
            for fc in range(8):
                pgt = pg[fci % 2]
                put = pu[fci % 2]
                g_ = gS[fci % 2]
                s_ = sS[fci % 2]
                u_ = uS[fci % 2]
                fci += 1
                for kc in range(8):
                    k.op("pe", lambda e: e.matmul(pgt[:], lhsT=wg[:, kc, fc * 128:(fc + 1) * 128], rhs=h2s[:, kc, rhs_cols],
                                                  start=(kc == 0), stop=(kc == 7)), [(wg, kc), h2s], [pgt], inc=(kc == 7))
                for kc in range(8):
                    k.op("pe", lambda e: e.matmul(put[:], lhsT=wg[:, kc, D + fc * 128:D + (fc + 1) * 128], rhs=h2s[:, kc, rhs_cols],
                                                  start=(kc == 0), stop=(kc == 7)), [(wg, kc), h2s], [put], inc=(kc == 7))
                k.op("dve", lambda e: e.tensor_scalar(out=g_[:], in0=pgt[:], scalar1=bgu[:, e_, fc:fc + 1], scalar2=7.0, op0=ALU.add, op1=ALU.min),
                     [pgt, bgu], [g_])
                k.op("act", lambda e: e.activation(out=s_[:], in_=g_[:], func=AF.Sigmoid, scale=1.702), [g_], [s_])
                k.op("dve", lambda e: e.tensor_scalar(out=u_[:], in0=put[:], scalar1=bgu1[:, e_, 8 + fc:9 + fc], scalar2=8.0, op0=ALU.add, op1=ALU.min),
                     [put, bgu1], [u_])
                k.op("dve", lambda e: e.tensor_tensor(out=g_[:], in0=g_[:], in1=s_[:], op=ALU.mult), [g_, s_], [g_])
                k.op("dve", lambda e: e.scalar_tensor_tensor(out=aT[:, fc, :], in0=u_[:], scalar=-6.0, in1=g_[:], op0=ALU.max, op1=ALU.mult), [g_, u_], [(aT, fc)])
            if bk == NBK - 1:
                load_wg(ei + 2)

        def emit_down(ei, bk, t0):
            nonlocal pdi
            e_ = ei % n_exp
            wd = wdn[ei % 2]
            aT = actT[(ei * NBK + bk) % 2]
            for tt in range(4):
                tl = bk * 4 + tt
                for half in range(2):
                    pp = pd[pdi % 4]
                    pdi += 1
                    for fc in range(8):
                        k.op("pe", lambda e: e.matmul(pp[:], lhsT=aT[:, fc, tt * 128:(tt + 1) * 128], rhs=wd[:, fc, half * 512:(half + 1) * 512],
                                                      start=(fc == 0), stop=(fc == 7)), [(aT, fc), (wd, fc)], [pp], inc=(fc == 7))
                    ya = yacc[:, tl, half * 512:(half + 1) * 512]
                    k.op("dve", lambda e: e.scalar_tensor_tensor(out=ya, in0=pp[:], scalar=g.wr[:, t0 + tl, e_:e_ + 1], in1=ya, op0=ALU.mult, op1=ALU.add),
                         [pp, (g.wr, t0 + tl), (yacc, (tl, half))], [(yacc, (tl, half))])
            if bk == NBK - 1:
                load_wd(ei + 2)

        for i in range(2):
            load_wg(i)
            load_wd(i)
        for ps_ in range(npass):
            t0 = ps_ * (PT // 128)
            k.dma("sp", h2s[:], h2Tv[:, :, ps_ * PT:(ps_ + 1) * PT], reads=[g.h2T.tensor], writes=[h2s])
            for tl in range(PT // 128):
                pt = pd[pdi % 4]
                pdi += 1
                k.op("pe", lambda e: e.transpose(out=pt[0:NE, 0:128], in_=g.wr[:, t0 + tl, :], identity=ident[:]), [(g.wr, t0 + tl), ident], [pt])
                k.op("act", lambda e: e.copy(out=wrT[:, tl * 128:(tl + 1) * 128], in_=pt[0:NE, 0:128]), [pt], [(wrT, tl)])
                for half in range(2):
                    pp = pd[pdi % 4]
                    pdi += 1
                    k.op("pe", lambda e: e.matmul(pp[:], lhsT=wrT[:, tl * 128:(tl + 1) * 128], rhs=bdn[:, half * 512:(half + 1) * 512], start=True, stop=True),
                         [(wrT, tl), bdn], [pp])
                    k.op("dve", lambda e: e.tensor_copy(out=yacc[:, tl, half * 512:(half + 1) * 512], in_=pp[:]), [pp], [(yacc, (tl, half))])
            units = [(ps_ * n_exp + e_, bk) for e_ in range(n_exp) for bk in range(NBK)]
            emit_gu(*units[0])
            for i, (ei_, bk_) in enumerate(units):
                if i + 1 < len(units):
                    emit_gu(*units[i + 1])
                emit_down(ei_, bk_, t0)
            for tl in range(PT // 128):
                t = t0 + tl
                xt = xa[t % 2]
                k.dma("sp", xt[:], g.x1[t * 128:(t + 1) * 128, :], reads=[(g.x1.tensor, t)], writes=[xt])
                ya = yacc[:, tl, :]
                k.op("dve", lambda e: e.tensor_tensor(out=ya, in0=ya, in1=gt2[:], op=ALU.mult), [(yacc, (tl, 0)), (yacc, (tl, 1)), gt2], [(yacc, (tl, 0)), (yacc, (tl, 1))])
                k.op("dve", lambda e: e.tensor_tensor(out=xt[:], in0=xt[:], in1=ya, op=ALU.add), [xt, (yacc, (tl, 0)), (yacc, (tl, 1))], [xt])
                rs, rskey = rms_tile(g, xt, stat, t, junk, 1.0 / D)
                k.op("dve", lambda e: e.scalar_tensor_tensor(out=xt[:], in0=xt[:], scalar=rs, in1=fg[:], op0=ALU.mult, op1=ALU.mult),
                     [xt, rskey, fg], [xt])
                k.dma("sp", g.out[t * 128:(t + 1) * 128, :], xt[:], reads=[xt], writes=[(g.out.tensor, t)])
        k.barrier()


def _consts():
    c = np.zeros((4, 128, 128), np.float32)
    c[0] = np.eye(128, dtype=np.float32)
    c[1] = 1.0
    c[2] = np.triu(np.ones((128, 128), np.float32))
    return c


def make_in_map(inp, b, names=None):
    f = lambda a: np.ascontiguousarray(np.asarray(a, dtype=np.float32))
    m = {}
    m["x"] = f(inp["x"][b])
    m["c_t"] = f(np.asarray(inp["c"][b]).reshape(8, 128).T)
    m["ada_w"] = f(inp["ada_w"][0])
    m["ada_b"] = f(inp["ada_b"][0]).reshape(1, -1)
    gv = np.stack([np.broadcast_to(np.asarray(inp[n]).reshape(-1), (128, D)) for n in ("norm1_g", "norm2_g", "final_g")])
    m["gvec"] = f(gv)
    m["w_in"] = f(inp["w_in"][0])
    m["consts"] = _consts()
    m["w_out"] = f(inp["w_out"][0])
    m["router_w"] = f(inp["router_w"][0])
    m["router_b"] = f(inp["router_b"][0]).reshape(1, -1)
    m["w_gate_up"] = f(inp["w_gate_up"][0])
    m["w_down"] = f(inp["w_down"][0])
    m["bgu_t"] = f(np.asarray(inp["b_gate_up"][0]).reshape(NE, 16, 128).transpose(2, 0, 1))
    m["b_down"] = f(inp["b_down"][0])
    m["w_q_b"] = f(inp["w_q_b"][0])
    m["w_kv_b"] = f(inp["w_kv_b"][0])
    mv = np.zeros((128, 16), np.float32)
    mv[:, 0:3] = np.asarray(inp["q_norm_g"][0]).reshape(3, 128).T
    mv[:, 3:5] = np.asarray(inp["kv_norm_g"][0]).reshape(2, 128).T
    mv[:, 5:9] = np.asarray(inp["mla_out_g"][0]).reshape(4, 128).T
    pidx = np.arange(128)
    mv[:, 9] = (np.float32(10000.0) ** (-(pidx % 16).astype(np.float32) / np.float32(16))).astype(np.float32)
    mv[:, 10] = np.where((pidx % 32) < 16, -1.0, 1.0)
    m["mla_vec"] = mv
    m["pos_bc"] = np.ascontiguousarray(np.broadcast_to(np.asarray(inp["positions"][b]).astype(np.int32).reshape(1, S), (128, S)))
    gvv = np.zeros((128, 32), np.float32)
    gvv[:, 0:8] = np.asarray(inp["A_log"][0]).reshape(1, 8)
    gvv[:, 8:16] = np.asarray(inp["dt_bias"][0]).reshape(1, 8)
    gvv[0:64, 16] = np.asarray(inp["gdn_norm_g"][0]).reshape(64)
    gvv[64:128, 16] = np.asarray(inp["gdn_norm_g"][0]).reshape(64)
    m["gdn_vec"] = gvv
    m["conv_wt"] = f(np.asarray(inp["conv_w"][0]).reshape(4, 24, 64).transpose(2, 1, 0))
    if names is not None:
        m = {n: v for n, v in m.items() if n in names}
    return m


def input_names(nc):
    return None


_CACHE = {}


def kernel(**inputs):
    if "nc" not in _CACHE:
        _CACHE["nc"] = build({})
    nc, kb = _CACHE["nc"]
    in_maps = [make_in_map(inputs, b) for b in range(8)]
    res = run_bass_kernel_spmd(nc, in_maps, core_ids=list(range(8)))
    out = np.stack([np.asarray(r["out"], dtype=np.float32) for r in res.results], axis=0)
    return out
```
